# Optimizing a Trainium2 kernel written in Bass

```python
import numpy as np
import jax
import jax.numpy as jnp
from jax import lax

D_MODEL = 1024
BATCH = 16
SEQ = 2048
DEPTH = 4

GRID_W = 64
CTX_LEN = 256
NORM_EPS = 1e-6
N_MOD = 6

RET_HEADS = 4
RET_DK = 128
RET_DV = 256
RET_QK = RET_HEADS * RET_DK
RET_V = RET_HEADS * RET_DV
RET_CHUNK = 128
ROPE_BASE = 10000.0

CONV_W = 512
CONV_K = 3

POOL_WINDOWS = (2, 4, 8, 16)
POOL_GROUPS = 4
POOL_GDIM = 128
POOL_W = POOL_GROUPS * POOL_GDIM

N_BRANCH = 3
IN_SPLITS = (RET_QK, RET_QK, RET_V, RET_V, CONV_W, CONV_W, CONV_W, POOL_W, N_BRANCH * D_MODEL)
IN_WIDTH = 2 * RET_QK + 2 * RET_V + 3 * CONV_W + POOL_W + N_BRANCH * D_MODEL

N_GROUPS = 4
EXP_PER_GROUP = 4
N_EXPERTS = N_GROUPS * EXP_PER_GROUP
TOP_K_INNER = 2
D_FF_EXPERT = 512

kernel_name = 'hybrid_retention_conv_pool_hmoe_dit'


def rmsnorm(x, g):
    xf = x.astype(jnp.float32)
    y = xf * lax.rsqrt(jnp.mean(xf * xf, axis=-1, keepdims=True) + NORM_EPS)
    return (y * g.astype(jnp.float32)).astype(x.dtype)


def split_cols(p):
    offsets = np.cumsum(IN_SPLITS)[:-1].tolist()
    return jnp.split(p, offsets, axis=-1)


def to_heads(a, head_dim):
    b, n, w = a.shape
    return a.astype(jnp.float32).reshape(b, n, w // head_dim, head_dim)


def axial_rope(n_tokens):
    rows = n_tokens // GRID_W
    row = jnp.repeat(jnp.arange(rows, dtype=jnp.float32), GRID_W)
    col = jnp.tile(jnp.arange(GRID_W, dtype=jnp.float32), rows)
    n_freq = RET_DK // 4
    inv_freq = ROPE_BASE ** (-jnp.arange(n_freq, dtype=jnp.float32) / n_freq)
    ang = jnp.concatenate([row[:, None] * inv_freq[None, :], col[:, None] * inv_freq[None, :]], axis=-1)
    return jnp.cos(ang), jnp.sin(ang)


def apply_rope(x, cos, sin):
    x1, x2 = jnp.split(x, 2, axis=-1)
    c = cos[None, :, None, :]
    s = sin[None, :, None, :]
    return jnp.concatenate([x1 * c - x2 * s, x1 * s + x2 * c], axis=-1)


def retention_dir(q, k, v, log_gamma, state0):
    b, n_tok, h, dk = q.shape
    dv = v.shape[-1]
    C = RET_CHUNK
    n = n_tok // C
    qc = q.reshape(b, n, C, h, dk)
    kc = k.reshape(b, n, C, h, dk)
    vc = v.reshape(b, n, C, h, dv)
    j = jnp.arange(C, dtype=jnp.float32)
    rel = j[:, None] - j[None, :]
    intra_decay = jnp.where(rel[None] >= 0.0,
                            jnp.exp(log_gamma[:, None, None] * jnp.maximum(rel, 0.0)[None]), 0.0)
    scores = jnp.einsum('bnihd,bnjhd->bnhij', qc, kc) * intra_decay[None, None]
    intra = jnp.einsum('bnhij,bnjhe->bnihe', scores, vc)
    q_decay = jnp.exp(log_gamma[None, :] * (j[:, None] + 1.0))
    k_decay = jnp.exp(log_gamma[None, :] * (C - 1.0 - j[:, None]))
    chunk_decay = jnp.exp(log_gamma * C)[None, :, None, None]
    kv = jnp.einsum('bnjhd,jh,bnjhe->nbhde', kc, k_decay, vc)

    def step(state, inp):
        q_i, kv_i = inp
        out_i = jnp.einsum('bihd,bhde->bihe', q_i, state)
        return state * chunk_decay + kv_i, out_i

    _, inter = lax.scan(step, state0, (jnp.moveaxis(qc, 1, 0), kv))
    inter = jnp.moveaxis(inter, 0, 1) * q_decay[None, None, :, :, None]
    return (intra + inter).reshape(b, n_tok, h, dv)


def ctx_final_states(k, v, log_gamma):
    n_tok = k.shape[1]
    m = jnp.arange(n_tok, dtype=jnp.float32)
    w_fwd = jnp.exp(log_gamma[0][:, None] * (n_tok - 1.0 - m)[None, :])
    w_bwd = jnp.exp(log_gamma[1][:, None] * m[None, :])
    s_fwd = jnp.einsum('blhd,hl,blhe->bhde', k, w_fwd, v)
    s_bwd = jnp.einsum('blhd,hl,blhe->bhde', k, w_bwd, v)
    return s_fwd, s_bwd


def head_norm(y):
    mu = jnp.mean(y, axis=-1, keepdims=True)
    yc = y - mu
    return yc * lax.rsqrt(jnp.mean(yc * yc, axis=-1, keepdims=True) + NORM_EPS)


def short_conv(u, w):
    up = jnp.pad(u, ((0, 0), (1, 1), (0, 0)))
    return w[0] * up[:, :-2] + w[1] * up[:, 1:-1] + w[2] * up[:, 2:]


def multiscale_pool(u, w_group, scale):
    b, n_tok, width = u.shape
    uf = u.astype(jnp.float32)
    cs = jnp.concatenate([jnp.zeros((b, 1, width), jnp.float32), jnp.cumsum(uf, axis=1)], axis=1)
    t = jnp.arange(n_tok)
    outs = []
    for gi, win in enumerate(POOL_WINDOWS):
        sl = slice(gi * POOL_GDIM, (gi + 1) * POOL_GDIM)
        lo = jnp.clip(t - win // 2, 0, n_tok)
        hi = jnp.clip(t + win - win // 2, 0, n_tok)
        cnt = (hi - lo).astype(jnp.float32)
        mean = (cs[:, hi, sl] - cs[:, lo, sl]) / cnt[None, :, None]
        outs.append(mean - uf[:, :, sl])
    pooled = jnp.stack(outs, axis=2)
    mixed = jnp.einsum('blgc,gcd->blgd', pooled, w_group.astype(jnp.float32)).reshape(b, n_tok, width)
    return (mixed * scale.astype(jnp.float32)).astype(u.dtype)


def mix_stream(parts, rope, log_gamma, state_f, state_b, conv_w, pool_w, pool_scale,
               w_ret_out, w_conv_out, w_pool_out, w_o):
    q_in, k_in, v_in, g_in, conv_b, conv_c, conv_x, pool_in, gate_in = parts
    dt = q_in.dtype
    b, n_tok, _ = q_in.shape
    q = to_heads(q_in, RET_DK)
    k = to_heads(k_in, RET_DK) * RET_DK ** -0.5
    v = to_heads(v_in, RET_DV)
    if rope is not None:
        q = apply_rope(q, rope[0], rope[1])
        k = apply_rope(k, rope[0], rope[1])
    flip = lambda a: jnp.flip(a, axis=1)
    o_fwd = retention_dir(q, k, v, log_gamma[0], state_f)
    o_bwd = flip(retention_dir(flip(q), flip(k), flip(v), log_gamma[1], state_b))
    y_ret = head_norm(o_fwd + o_bwd).reshape(b, n_tok, RET_V).astype(dt) * jax.nn.silu(g_in)
    y_conv = conv_b * short_conv(conv_c * conv_x, conv_w)
    y_pool = multiscale_pool(pool_in, pool_w, pool_scale)
    g_ret, g_conv, g_pool = jnp.split(jax.nn.sigmoid(gate_in.astype(jnp.float32)).astype(dt), N_BRANCH, axis=-1)
    merged = g_ret * (y_ret @ w_ret_out) + g_conv * (y_conv @ w_conv_out) + g_pool * (y_pool @ w_pool_out)
    return merged @ w_o


def token_mixers(h, hc, rope, w_in, ret_decay, conv_w, pool_w, pool_scale,
                 w_ret_out, w_conv_out, w_pool_out, w_o, need_ctx):
    log_gamma = jax.nn.log_sigmoid(ret_decay.astype(jnp.float32))
    if need_ctx:
        pc = split_cols(hc @ w_in)
        k_c_in, v_c_in = pc[1], pc[2]
    else:
        k_c_in, v_c_in = jnp.split(hc @ w_in[:, RET_QK:2 * RET_QK + RET_V], [RET_QK], axis=-1)
    k_c = to_heads(k_c_in, RET_DK) * RET_DK ** -0.5
    v_c = to_heads(v_c_in, RET_DV)
    s_fwd, s_bwd = ctx_final_states(k_c, v_c, log_gamma)
    y = mix_stream(split_cols(h @ w_in), rope, log_gamma, s_fwd, s_bwd, conv_w, pool_w, pool_scale,
                   w_ret_out, w_conv_out, w_pool_out, w_o)
    if need_ctx:
        zeros = jnp.zeros_like(s_fwd)
        yc = mix_stream(pc, None, log_gamma, zeros, zeros, conv_w, pool_w, pool_scale,
                        w_ret_out, w_conv_out, w_pool_out, w_o)
    else:
        yc = None
    return y, yc


def hier_moe(h, w_rg, b_rg, w_re, b_re, w1, w3, w2):
    b, n_tok, d = h.shape
    t = h.reshape(-1, d)
    tf = t.astype(jnp.float32)
    g_prob = jax.nn.softmax(tf @ w_rg.astype(jnp.float32) + b_rg.astype(jnp.float32), axis=-1)
    g_top, g_idx = lax.top_k(g_prob, 1)
    e_logits = (tf @ w_re.astype(jnp.float32) + b_re.astype(jnp.float32)).reshape(-1, N_GROUPS, EXP_PER_GROUP)
    e_in_group = jnp.take_along_axis(e_logits, g_idx[:, :, None], axis=1)[:, 0]
    e_top, e_loc = lax.top_k(e_in_group, TOP_K_INNER)
    e_w = jax.nn.softmax(e_top, axis=-1) * g_top
    e_idx = g_idx * EXP_PER_GROUP + e_loc
    combine = jnp.sum(jax.nn.one_hot(e_idx, N_EXPERTS, dtype=jnp.float32) * e_w[..., None], axis=1).astype(t.dtype)
    y = jnp.zeros_like(t)
    for e in range(N_EXPERTS):
        a = jax.nn.silu(t @ w1[e]) * (t @ w3[e])
        y = y + combine[:, e:e + 1] * (a @ w2[e])
    return y.reshape(b, n_tok, d)


def setup_inputs(seed: int = 0) -> dict:
    key = jax.random.key(seed)
    ks = jax.random.split(key, 26)
    f32 = jnp.float32

    def nrm(k, shape, scale):
        return jax.random.normal(k, shape, f32) * scale

    D = D_MODEL
    decay_base = jnp.log(2.0 ** jnp.arange(5, 5 + RET_HEADS, dtype=f32) - 1.0)
    return {
        'x': nrm(ks[0], (BATCH, SEQ, D), 1.0),
        'c': nrm(ks[1], (BATCH, D), 1.0),
        'ctx': nrm(ks[2], (BATCH, CTX_LEN, D), 1.0),
        'c_ctx': nrm(ks[3], (D,), 1.0),
        'w_ada': nrm(ks[4], (DEPTH, D, N_MOD * D), 0.5 * D ** -0.5),
        'b_ada': nrm(ks[5], (DEPTH, N_MOD * D), 0.02),
        'norm1': 1.0 + nrm(ks[6], (DEPTH, D), 0.05),
        'norm2': 1.0 + nrm(ks[7], (DEPTH, D), 0.05),
        'w_in': nrm(ks[8], (DEPTH, D, IN_WIDTH), D ** -0.5),
        'ret_decay': decay_base[None, None, :] + nrm(ks[9], (DEPTH, 2, RET_HEADS), 0.1),
        'conv_w': nrm(ks[10], (DEPTH, CONV_K, CONV_W), CONV_K ** -0.5),
        'pool_w': nrm(ks[11], (DEPTH, POOL_GROUPS, POOL_GDIM, POOL_GDIM), POOL_GDIM ** -0.5),
        'pool_scale': 1.0 + nrm(ks[12], (DEPTH, POOL_W), 0.1),
        'w_ret_out': nrm(ks[13], (DEPTH, RET_V, D), RET_V ** -0.5),
        'w_conv_out': nrm(ks[14], (DEPTH, CONV_W, D), CONV_W ** -0.5),
        'w_pool_out': nrm(ks[15], (DEPTH, POOL_W, D), POOL_W ** -0.5),
        'w_o': nrm(ks[16], (DEPTH, D, D), D ** -0.5),
        'w_rg': nrm(ks[17], (DEPTH, D, N_GROUPS), D ** -0.5),
        'b_rg': nrm(ks[18], (DEPTH, N_GROUPS), 0.01),
        'w_re': nrm(ks[19], (DEPTH, D, N_EXPERTS), D ** -0.5),
        'b_re': nrm(ks[20], (DEPTH, N_EXPERTS), 0.01),
        'w1': nrm(ks[21], (DEPTH, N_EXPERTS, D, D_FF_EXPERT), D ** -0.5),
        'w3': nrm(ks[22], (DEPTH, N_EXPERTS, D, D_FF_EXPERT), D ** -0.5),
        'w2': nrm(ks[23], (DEPTH, N_EXPERTS, D_FF_EXPERT, D), D_FF_EXPERT ** -0.5),
        'final_norm': 1.0 + nrm(ks[24], (D,), 0.05),
    }


def reference(x, c, ctx, c_ctx, w_ada, b_ada, norm1, norm2, w_in, ret_decay, conv_w, pool_w, pool_scale,
              w_ret_out, w_conv_out, w_pool_out, w_o, w_rg, b_rg, w_re, b_re, w1, w3, w2, final_norm):
    rope = axial_rope(x.shape[1])
    xc = ctx
    silu_c = jax.nn.silu(c)
    silu_cc = jax.nn.silu(c_ctx)
    n_ctx = ctx.shape[1]
    for l in range(DEPTH):
        last = l == DEPTH - 1
        mod = (silu_c @ w_ada[l] + b_ada[l])[:, None, :]
        mod_c = (silu_cc @ w_ada[l] + b_ada[l])[None, None, :]
        sh1, sc1, gt1, sh2, sc2, gt2 = jnp.split(mod, N_MOD, axis=-1)
        csh1, csc1, cgt1, csh2, csc2, cgt2 = jnp.split(mod_c, N_MOD, axis=-1)
        h = rmsnorm(x, norm1[l]) * (1.0 + sc1) + sh1
        hc = rmsnorm(xc, norm1[l]) * (1.0 + csc1) + csh1
        y, yc = token_mixers(h, hc, rope, w_in[l], ret_decay[l], conv_w[l], pool_w[l], pool_scale[l],
                             w_ret_out[l], w_conv_out[l], w_pool_out[l], w_o[l], not last)
        x = x + gt1 * y
        h = rmsnorm(x, norm2[l]) * (1.0 + sc2) + sh2
        if last:
            x = x + gt2 * hier_moe(h, w_rg[l], b_rg[l], w_re[l], b_re[l], w1[l], w3[l], w2[l])
        else:
            xc = xc + cgt1 * yc
            hc = rmsnorm(xc, norm2[l]) * (1.0 + csc2) + csh2
            f = hier_moe(jnp.concatenate([hc, h], axis=1), w_rg[l], b_rg[l], w_re[l], b_re[l], w1[l], w3[l], w2[l])
            xc = xc + cgt2 * f[:, :n_ctx]
            x = x + gt2 * f[:, n_ctx:]
    return rmsnorm(x, final_norm)
```

```python
import os
import numpy as np
from contextlib import ExitStack
CUT = int(os.environ.get('K_RET_CUT', '0'))
import concourse.bass as bass
import concourse.mybir as mybir
from concourse.bass_utils import run_bass_kernel_spmd

F32 = mybir.dt.float32
BF16 = mybir.dt.bfloat16
AF = mybir.ActivationFunctionType
ALU = mybir.AluOpType
AX = mybir.AxisListType

D = 1024
KC = 8
NCTX = 256
SEQ = 2048
NT = NCTX + SEQ
NCH = NT // 128
DEPTH = 4
EPS = 1e-6
TILES = [(0, 256), (256, 512), (768, 512), (1280, 512), (1792, 512)]
O_Q, O_K, O_V, O_G, O_CB, O_CC, O_CX, O_PI, O_GT = 0, 512, 1024, 2048, 3072, 3584, 4096, 4608, 5120
LT = 2336


def ucol(t):
    return 8 + t if t < NCTX else t + 24


ALLBUFS = []


class Buf:
    __slots__ = ("w", "r")

    def __init__(self):
        self.w = []
        self.r = []
        ALLBUFS.append(self)


class DSem:
    def __init__(self, sem):
        self.sem = sem
        self.cnt = 0


class T:
    def __init__(self, t):
        self.t = t
        self.b = Buf()

    def __getitem__(self, k):
        return self.t[k]


class Ctx:
    def __init__(self, nc, es):
        self.nc = nc
        self.es = es
        self.engs = {"pe": nc.tensor, "act": nc.scalar, "dve": nc.vector, "pool": nc.gpsimd, "sp": nc.sync}
        self.sem = {k: es.enter_context(nc.semaphore("s_" + k)) for k in self.engs}
        self.cnt = {k: 0 for k in self.engs}
        self.seen = {k: {} for k in self.engs}
        self.dsems = []
        self.nsb = 0

    def sb(self, es, shape, dt, name=None):
        self.nsb += 1
        return T(es.enter_context(self.nc.sbuf_tensor(f"{name or 't'}_{self.nsb}", list(shape), dt)))

    def dsem(self, perm=False, sw=False):
        if sw:
            if not hasattr(self, "swpool_"):
                self.swpool_ = []
                self.swptr = 0
            if self.swptr >= len(self.swpool_):
                s = DSem(self.es.enter_context(self.nc.semaphore(f"dw{len(self.dsems)}")))
                self.dsems.append(s)
                self.swpool_.append(s)
            s = self.swpool_[self.swptr]
            self.swptr += 1
            return s
        if perm:
            s = DSem(self.es.enter_context(self.nc.semaphore(f"dp{len(self.dsems)}")))
            self.dsems.append(s)
            return s
        if not hasattr(self, "pool_"):
            self.pool_ = []
            self.dptr = 0
        if self.dptr >= len(self.pool_):
            s = DSem(self.es.enter_context(self.nc.semaphore(f"d{len(self.dsems)}")))
            self.dsems.append(s)
            self.pool_.append(s)
        s = self.pool_[self.dptr]
        self.dptr += 1
        return s

    def _bufs(self, xs):
        return [x.b if isinstance(x, T) else x for x in xs if x is not None]

    def _wait(self, eng, deps):
        best = {}
        for key, val in deps:
            if best.get(key, 0) < val:
                best[key] = val
        e = self.engs[eng]
        for key, val in best.items():
            if self.seen[eng].get(key, 0) >= val:
                continue
            sem = key.sem if isinstance(key, DSem) else self.sem[key]
            e.wait_ge(sem, val)
            self.seen[eng][key] = val

    def _deps(self, eng, reads, writes, part=False, skipkey=None):
        deps = []
        for b in reads:
            deps += b.w
        for b in writes:
            deps += [d for d in b.w if d[0] is not skipkey]
            deps += b.r
        if eng == "pe":
            deps = [d for d in deps if d[0] != "pe"]
        return deps

    def op(self, eng, fn, r=(), w=(), inc=True, part=False):
        reads, writes = self._bufs(r), self._bufs(w)
        self._wait(eng, self._deps(eng, reads, writes, part))
        ins = fn(self.engs[eng])
        tk = (eng, self.cnt[eng] + 1)
        if inc:
            ins.then_inc(self.sem[eng], 1)
            self.cnt[eng] += 1
        for b in reads:
            if not b.r or b.r[-1] != tk:
                b.r.append(tk)
        for b in writes:
            if part:
                if not b.w or b.w[-1] != tk:
                    b.w.append(tk)
            else:
                b.w = [tk]
                b.r = []
        return ins

    def dma(self, q, out, in_, ds, r=(), w=(), part=False):
        reads, writes = self._bufs(r), self._bufs(w)
        self._wait(q, self._deps(q, reads, writes, part, ds if part else None))
        ins = self.engs[q].dma_start(out=out, in_=in_)
        ds.cnt += 16
        ins.then_inc(ds.sem, 16)
        tk = (ds, ds.cnt)
        for b in reads:
            b.r.append(tk)
        for b in writes:
            if part:
                b.w.append(tk)
            else:
                b.w = [tk]
                b.r = []

    def barrier(self):
        for eng in self.engs:
            deps = [(k, self.cnt[k]) for k in self.engs if k != eng and self.cnt[k] > 0]
            deps += [(d, d.cnt) for d in self.dsems if d.cnt > 0]
            self._wait(eng, deps)
        for b in ALLBUFS:
            b.w = []
            b.r = []

    def reset_dsems(self):
        self.dptr = 0
        self.swptr = 0


class Ring:
    def __init__(self, items):
        self.items = items
        self.i = 0

    def next(self):
        x = self.items[self.i % len(self.items)]
        self.i += 1
        return x


def build(NB=2, n_layers=DEPTH, dbg=False, stop_after=None):
    nc = bass.Bass("TRN2", target_bir_lowering=False)
    es = ExitStack()
    C = Ctx(nc, es)
    last_l = DEPTH - 1

    def dram(name, shape, dt, kind):
        return nc.dram_tensor(name, list(shape), dt, kind=kind).ap()

    xin = dram("xin", [NB, 128, KC, NT], F32, "ExternalInput")
    cT = dram("cT", [128, KC, 3], F32, "ExternalInput")
    w_ada = dram("w_ada", [DEPTH, D, 6 * D], F32, "ExternalInput")
    w_in = dram("w_in", [DEPTH, D, 8192], F32, "ExternalInput")
    w_ret_out = dram("w_ret_out", [DEPTH, 1024, D], F32, "ExternalInput")
    w_conv_out = dram("w_conv_out", [DEPTH, 512, D], F32, "ExternalInput")
    w_pool_out = dram("w_pool_out", [DEPTH, 512, D], F32, "ExternalInput")
    w_o = dram("w_o", [DEPTH, D, D], F32, "ExternalInput")
    pool_w = dram("pool_w", [DEPTH, 4, 128, 128], F32, "ExternalInput")
    w1 = dram("w1", [DEPTH, 16, D, 512], F32, "ExternalInput")
    w3 = dram("w3", [DEPTH, 16, D, 512], F32, "ExternalInput")
    w2 = dram("w2", [DEPTH, 16, 512, D], F32, "ExternalInput")
    smallp = dram("smallp", [128, SMALL_W], F32, "ExternalInput")
    wr = dram("wr", [128, DEPTH * KC * 20], F32, "ExternalInput")
    consts = dram("consts", [128, CONST_W], F32, "ExternalInput")
    ropet = dram("ropet", [128, 4 * 16 * 64], F32, "ExternalInput")
    outT = dram("outT", [NB, 128, KC, SEQ], F32, "ExternalOutput")
    skind = "ExternalOutput" if dbg else "Internal"
    XT = dram("XT", [NB, 128, KC, NT], F32, skind)
    YRT = dram("YRT", [128, KC, NT], BF16, skind)
    MG = dram("MG", [128, KC, NT], F32, skind)
    SBS = dram("SBS", [NCH, 128, 1024], BF16, "Internal")
    b_XT = [[Buf() for _ in TILES] for _ in range(NB)]
    b_YRT = [Buf() for _ in range(NCH)]
    b_MG = [Buf() for _ in TILES]
    b_SBS = [Buf() for _ in range(NCH)]
    b_out = Buf()
    b_in = None

    g = es
    hT = C.sb(g, [128, KC, NT], BF16, "hT")
    CS = C.sb(g, [128, CONST_W], F32, "CS")
    SP_ = C.sb(g, [128, SMALL_W], F32, "SP")
    IDb = C.sb(g, [128, 128], BF16, "IDb")
    MOD = C.sb(g, [128, 48, 3], F32, "MOD")
    GS1 = C.sb(g, [128, KC, 3], F32, "GS1")
    GS2 = C.sb(g, [128, KC, 3], F32, "GS2")
    LG = C.sb(g, [128, 8], F32, "LG")
    GCt = C.sb(g, [128, 8], F32, "GC")
    DBt = C.sb(g, [128, 4], F32, "DB")
    DFt = C.sb(g, [128, 4], F32, "DF")
    DM = C.sb(g, [128, 4, 128], F32, "DM")
    DQF = C.sb(g, [128, 4, 128], F32, "DQF")
    DQB = C.sb(g, [128, 4, 128], F32, "DQB")
    SIC = C.sb(g, [128, KC, 3], F32, "SIC")
    banks = [T(es.enter_context(nc.psum_tensor(f"pb{i}", [128, 512], F32))) for i in range(6)]
    PB = Ring(banks)
    PT = Ring([T(es.enter_context(nc.psum_tensor(f"pt{i}", [128, 1024], BF16))) for i in range(2)])
    ds_misc = C.dsem(perm=True)
    ds_sp = C.dsem(perm=True)

    def cs(name):
        o, n = CONST_OFF[name]
        return CS[:, o:o + n]

    IDf = cs("ident")
    ONES = cs("ones")

    def sp(name, l=None):
        o, n = SMALL_OFF[name]
        if l is None:
            return SP_[:, o:o + n]
        per = n // DEPTH
        return SP_[:, o + l * per:o + (l + 1) * per]

    C.dma("sp", CS[:], consts[:, :], ds_misc, r=[b_in], w=[CS])
    C.dma("sp", SP_[:], smallp[:, :], ds_sp, r=[b_in], w=[SP_])
    C.op("dve", lambda e: e.tensor_copy(out=IDb[:], in_=IDf), r=[CS], w=[IDb])
    ds_c = C.dsem(perm=True)
    C.dma("sp", SIC[:], cT[:, :, :], ds_c, r=[b_in], w=[SIC])
    C.op("act", lambda e: e.activation(out=SIC[:], in_=SIC[:], func=AF.Silu), r=[SIC], w=[SIC])

    def mm_group(out_ap, pairs, bank, reads, fp32=False):
        n = len(pairs)
        for i, (l_, r_) in enumerate(pairs):
            C.op("pe", lambda e, l_=l_, r_=r_, i=i: e.matmul(out_ap, lhsT=l_, rhs=r_, start=(i == 0), stop=(i == n - 1)),
                 r=reads, w=[bank], inc=(i == n - 1), part=False)

    def load_w(es_, src2d, ncols, nk, ds, name):
        wt = C.sb(es_, [128, nk, ncols], BF16, name)
        ds = C.dsem(sw=True)
        v = src2d.rearrange("(kc p) n -> p kc n", p=128)
        for kc in range(nk):
            C.dma("pool", wt[:, kc, :], v[:, kc, :], ds, r=[b_in], w=[wt], part=(kc > 0))
        return wt

    def layer_setup(l):
        with ExitStack() as s:
            wa = [C.sb(s, [128, KC, 512], F32, "wa") for _ in range(2)]
            dsw = [C.dsem() for _ in range(2)]
            wav = w_ada[l].rearrange("(kc p) n -> p kc n", p=128)
            bada = sp("b_ada", l)
            for blk in range(12):
                wt = wa[blk % 2]
                C.dma("sp", wt[:], wav[:, :, blk * 512:(blk + 1) * 512], dsw[blk % 2], r=[b_in], w=[wt])
                for jj in range(4):
                    j = blk * 4 + jj
                    bank = PB.next()
                    mm_group(bank[:, 0:3], [(wt[:, kc, jj * 128:(jj + 1) * 128], SIC[:, kc, :]) for kc in range(KC)],
                             bank, [wt, SIC])
                    C.op("dve", lambda e, j=j, bank=bank: e.tensor_scalar(
                        out=MOD[:, j, :], in0=bank[:, 0:3], scalar1=bada[:, j:j + 1], scalar2=None, op0=ALU.add),
                        r=[bank, SP_], w=[MOD], part=True)
            for (GS, nm, jo) in ((GS1, "norm1", 8), (GS2, "norm2", 32)):
                ng = sp(nm, l)
                for kc in range(KC):
                    C.op("dve", lambda e, GS=GS, kc=kc, jo=jo, ng=ng: e.tensor_scalar(
                        out=GS[:, kc, :], in0=MOD[:, jo + kc, :], scalar1=1.0, scalar2=ng[:, kc:kc + 1],
                        op0=ALU.add, op1=ALU.mult), r=[MOD, SP_], w=[GS], part=True)
            rd = sp("ret_decay", l)
            C.op("act", lambda e: e.activation(out=LG[:], in_=rd, func=AF.Sigmoid), r=[SP_], w=[LG])
            C.op("act", lambda e: e.activation(out=LG[:], in_=LG[:], func=AF.Ln), r=[LG], w=[LG])
            C.op("act", lambda e: e.activation(out=GCt[:], in_=LG[:], func=AF.Exp, scale=128.0), r=[LG], w=[GCt])
            jc = cs("jcol")
            C.op("act", lambda e: e.activation(out=DBt[:], in_=LG[:, 4:8], func=AF.Exp, scale=jc[:, 0:1]), r=[LG, CS], w=[DBt])
            C.op("act", lambda e: e.activation(out=DFt[:], in_=LG[:, 0:4], func=AF.Exp, scale=jc[:, 1:2]), r=[LG, CS], w=[DFt])
            tmp = C.sb(s, [128, 128], F32, "dtmp")
            for h in range(4):
                C.op("act", lambda e, h=h: e.activation(out=DQF[:, h, :], in_=cs("iota1"), func=AF.Exp, scale=LG[:, h:h + 1]),
                     r=[LG, CS], w=[DQF], part=True)
                C.op("act", lambda e, h=h: e.activation(out=DQB[:, h, :], in_=cs("iotac"), func=AF.Exp, scale=LG[:, 4 + h:5 + h]),
                     r=[LG, CS], w=[DQB], part=True)
                C.op("act", lambda e, h=h: e.activation(out=DM[:, h, :], in_=cs("relp"), func=AF.Exp, scale=LG[:, h:h + 1]),
                     r=[LG, CS], w=[DM], part=True)
                C.op("dve", lambda e, h=h: e.tensor_tensor(out=DM[:, h, :], in0=DM[:, h, :], in1=cs("mf"), op=ALU.mult),
                     r=[DM, CS], w=[DM])
                C.op("act", lambda e, h=h: e.activation(out=tmp[:], in_=cs("reln"), func=AF.Exp, scale=LG[:, 4 + h:5 + h]),
                     r=[LG, CS], w=[tmp])
                C.op("dve", lambda e: e.tensor_tensor(out=tmp[:], in0=tmp[:], in1=cs("mb"), op=ALU.mult), r=[tmp, CS], w=[tmp])
                C.op("dve", lambda e, h=h: e.tensor_tensor(out=DM[:, h, :], in0=DM[:, h, :], in1=tmp[:], op=ALU.add),
                     r=[DM, tmp], w=[DM])
            C.barrier()

    def norm_tile(s, l, b, ti, src_ap, src_buf, GS, sh_j, bufs, hf=None, out_fn=None):
        off, n = TILES[ti]
        r = 2 if ti == 0 else b
        sq, rstd, tmp = bufs
        C.op("act", lambda e: e.activation(out=sq[:, :, :n], in_=src_ap, func=AF.Square), r=[src_buf], w=[sq])
        bank = PB.next()
        mm_group(bank[:, :n], [(ONES, sq[:, kc, :n]) for kc in range(KC)], bank, [sq, CS])
        C.op("act", lambda e: e.activation(out=rstd[:, :n], in_=bank[:, :n], func=AF.Sqrt, scale=1.0 / D, bias=cs("eps")[:, 0:1]),
             r=[bank, CS], w=[rstd])
        C.op("dve", lambda e: e.reciprocal(out=rstd[:, :n], in_=rstd[:, :n]), r=[rstd], w=[rstd])
        for kc in range(KC):
            tb = tmp.next()
            C.op("dve", lambda e, kc=kc, tb=tb: e.scalar_tensor_tensor(
                out=tb[:, :n], in0=src_ap[:, kc, :], scalar=GS[:, kc, r:r + 1], in1=rstd[:, :n], op0=ALU.mult, op1=ALU.mult),
                r=[src_buf, GS, rstd], w=[tb])
            if out_fn is not None:
                out_fn(kc, tb, n)
            elif hf is None:
                C.op("act", lambda e, kc=kc, tb=tb: e.activation(
                    out=hT[:, kc, off:off + n], in_=tb[:, :n], func=AF.Identity, bias=MOD[:, sh_j + kc, r:r + 1]),
                    r=[tb, MOD], w=[hT], part=True)
            else:
                C.op("act", lambda e, kc=kc, tb=tb: e.activation(
                    out=hf[:, kc, :n], in_=tb[:, :n], func=AF.Identity, bias=MOD[:, sh_j + kc, r:r + 1]),
                    r=[tb, MOD], w=[hf], part=True)
                C.op("dve", lambda e, kc=kc: e.tensor_copy(out=hT[:, kc, off:off + n], in_=hf[:, kc, :n]),
                     r=[hf], w=[hT], part=True)

    def stage_norm1(l, b, tiles):
        with ExitStack() as s:
            xt = [C.sb(s, [128, KC, 512], F32, "xt") for _ in range(2)]
            dsx = [C.dsem() for _ in range(2)]
            sq = C.sb(s, [128, KC, 512], F32, "sq")
            rstd = C.sb(s, [128, 512], F32, "rstd")
            tmp = Ring([C.sb(s, [128, 512], F32, "ntmp") for _ in range(2)])
            src = xin if l == 0 else XT
            for i, ti in enumerate(tiles):
                off, n = TILES[ti]
                x_ = xt[i % 2]
                C.dma("sp", x_[:, :, :n], src[b, :, :, off:off + n], dsx[i % 2],
                      r=[b_in if l == 0 else b_XT[b][ti]], w=[x_])
                norm_tile(s, l, b, ti, x_[:, :, :n], x_, GS1, 0, (sq, rstd, tmp))
            C.barrier()

    def rope_evac(src_bank, dst, tabs, ci, rt):
        cos_t, sin_t, tab_buf = tabs
        n = ci - 2
        sv = src_bank[:, :].rearrange("p (h t d) -> p h t d", h=4, t=2)
        dv = dst[:, :, :].rearrange("p h (t d) -> p h t d", t=2)
        t1, t2 = rt
        for h in range(4):
            c_ = cos_t[:, n, :]
            s_ = sin_t[:, n, :]
            C.op("dve", lambda e: e.tensor_tensor(out=t1[:, h, :], in0=sv[:, h, 0, :], in1=c_, op=ALU.mult),
                 r=[src_bank, tab_buf], w=[t1], part=True)
            C.op("dve", lambda e: e.tensor_tensor(out=t2[:, h, :], in0=sv[:, h, 1, :], in1=s_, op=ALU.mult),
                 r=[src_bank, tab_buf], w=[t2], part=True)
        C.op("dve", lambda e: e.tensor_tensor(out=dv[:, :, 0, :], in0=t1[:], in1=t2[:], op=ALU.subtract),
             r=[t1, t2], w=[dst], part=True)
        for h in range(4):
            c_ = cos_t[:, n, :]
            s_ = sin_t[:, n, :]
            C.op("dve", lambda e: e.tensor_tensor(out=t1[:, h, :], in0=sv[:, h, 0, :], in1=s_, op=ALU.mult),
                 r=[src_bank, tab_buf], w=[t1], part=True)
            C.op("dve", lambda e: e.tensor_tensor(out=t2[:, h, :], in0=sv[:, h, 1, :], in1=c_, op=ALU.mult),
                 r=[src_bank, tab_buf], w=[t2], part=True)
        C.op("dve", lambda e: e.tensor_tensor(out=dv[:, :, 1, :], in0=t1[:], in1=t2[:], op=ALU.add),
             r=[t1, t2], w=[dst], part=True)

    def stage_ret(l, b):
        last = (l == last_l)
        with ExitStack() as s:
            dsw = C.dsem(sw=True)
            WK = load_w(s, w_in[l, :, O_K:O_K + 512], 512, KC, dsw, "WK")
            WV = load_w(s, w_in[l, :, O_V:O_V + 1024], 1024, KC, dsw, "WV")
            WQ = load_w(s, w_in[l, :, O_Q:O_Q + 512], 512, KC, dsw, "WQ")
            WG = load_w(s, w_in[l, :, O_G:O_G + 1024], 1024, KC, dsw, "WG")
            RT = C.sb(s, [128, 4 * 16 * 64], F32, "RT")
            C.dma("sp", RT[:], ropet[:, :], C.dsem(), r=[b_in], w=[RT])
            rtv = RT[:, :].rearrange("p (k n d) -> p k n d", k=4, n=16)
            tabq = (rtv[:, 0], rtv[:, 1], RT)
            tabk = (rtv[:, 2], rtv[:, 3], RT)
            rt = (C.sb(s, [128, 4, 64], F32, "rt1"), C.sb(s, [128, 4, 64], F32, "rt2"))
            Sb = C.sb(s, [128, 4, 256], F32, "Sb")
            Sf = C.sb(s, [128, 4, 256], F32, "Sf")
            qr = Ring([C.sb(s, [128, 4, 128], BF16, "qr") for _ in range(2)])
            kr = Ring([C.sb(s, [128, 4, 128], BF16, "kr") for _ in range(2)])
            vb = Ring([C.sb(s, [128, 4, 256], BF16, "vb") for _ in range(2)])
            vd = Ring([C.sb(s, [128, 4, 256], BF16, "vd") for _ in range(2)])
            sg = Ring([C.sb(s, [128, 4, 256], F32, "sg") for _ in range(2)])
            sbo = [C.sb(s, [128, 1024], BF16, "sbo") for _ in range(2)]
            ds_sbo = [C.dsem() for _ in range(2)]
            sbi = [C.sb(s, [128, 4, 256], BF16, "sbi") for _ in range(2)]
            ds_sbi = [C.dsem() for _ in range(2)]
            sfb = Ring([C.sb(s, [128, 4, 256], BF16, "sfb") for _ in range(2)])
            qT = Ring([C.sb(s, [128, 4, 128], BF16, "qT") for _ in range(2)])
            qfT = Ring([C.sb(s, [128, 4, 128], BF16, "qfT") for _ in range(2)])
            qbT = Ring([C.sb(s, [128, 4, 128], BF16, "qbT") for _ in range(2)])
            kT = Ring([C.sb(s, [128, 4, 128], BF16, "kT") for _ in range(2)])
            sT = Ring([C.sb(s, [128, 4, 128], BF16, "sT") for _ in range(2)])
            yn = Ring([C.sb(s, [128, 256], F32, "yn") for _ in range(2)])
            yr = Ring([C.sb(s, [128, 1024], BF16, "yr") for _ in range(2)])
            yT = [C.sb(s, [128, KC, 128], BF16, "yT") for _ in range(2)]
            ds_yT = [C.dsem() for _ in range(2)]
            st6 = C.sb(s, [128, 4, 6], F32, "st6")
            mv = C.sb(s, [128, 4, 2], F32, "mv")
            rs = C.sb(s, [128, 4], F32, "rs")

            def proj(ci, W, c0, ncols):
                bank = PB.next()
                mm_group(bank[:, :ncols], [(hT[:, kc, ci * 128:(ci + 1) * 128], W[:, kc, c0:c0 + ncols]) for kc in range(KC)],
                         bank, [hT, W])
                return bank

            def k_evac(ci, kps):
                k_ = kr.next()
                if ci >= 2:
                    rope_evac(kps, k_, tabk, ci, rt)
                else:
                    C.op("act", lambda e: e.activation(out=k_[:, :, :].rearrange("p h d -> p (h d)"), in_=kps[:, :],
                                                       func=AF.Identity, scale=128.0 ** -0.5), r=[kps], w=[k_])
                return k_

            def v_scaled(vps, dec, dst):
                for h in range(4):
                    bk = vps[h // 2]
                    C.op("act", lambda e, h=h, bk=bk: e.activation(
                        out=dst[:, h, :], in_=bk[:, (h % 2) * 256:(h % 2) * 256 + 256], func=AF.Identity, scale=dec[:, h:h + 1]),
                        r=[bk, dec], w=[dst], part=True)

            def state_update(S, k_, vdd, gcol0):
                kv = [PB.next(), PB.next()]
                for h in range(4):
                    bk = kv[h // 2]
                    C.op("pe", lambda e, h=h, bk=bk: e.matmul(bk[:, (h % 2) * 256:(h % 2) * 256 + 256], lhsT=k_[:, h, :],
                                                             rhs=vdd[:, h, :], start=True, stop=True),
                         r=[k_, vdd], w=[bk], part=(h % 2 == 1))
                for h in range(4):
                    bk = kv[h // 2]
                    C.op("dve", lambda e, h=h, bk=bk: e.scalar_tensor_tensor(
                        out=S[:, h, :], in0=S[:, h, :], scalar=GCt[:, gcol0 + h:gcol0 + h + 1],
                        in1=bk[:, (h % 2) * 256:(h % 2) * 256 + 256], op0=ALU.mult, op1=ALU.add),
                        r=[S, GCt, bk], w=[S])

            C.op("dve", lambda e: e.memset(Sb[:], 0.0), w=[Sb])
            C.op("dve", lambda e: e.memset(Sf[:], 0.0), w=[Sf])
            order = [1, 0] + list(range(NCH - 1, 1, -1))
            for i, ci in enumerate(order):
                kps = proj(ci, WK, 0, 512)
                vps = [proj(ci, WV, 0, 512), proj(ci, WV, 512, 512)]
                k_ = k_evac(ci, kps)
                vd_ = vd.next()
                v_scaled(vps, DBt, vd_)
                so = sbo[i % 2]
                C.op("act", lambda e: e.copy(out=so[:], in_=Sb[:, :, :].rearrange("p h d -> p (h d)")), r=[Sb], w=[so])
                C.dma("sp", SBS[ci, :, :], so[:], ds_sbo[i % 2], r=[so], w=[b_SBS[ci]])
                state_update(Sb, k_, vd_, 4)
            sf_cur = sfb.next()
            C.op("dve", lambda e: e.memset(sf_cur[:], 0.0), w=[sf_cur])
            for ci in range(NCH if CUT != 1 else 0):
                only_state = (last and ci < 2) or CUT == 2
                kps = proj(ci, WK, 0, 512)
                vps = [proj(ci, WV, 0, 512), proj(ci, WV, 512, 512)]
                k_ = k_evac(ci, kps)
                vdf = vd.next()
                v_scaled(vps, DFt, vdf)
                if not only_state:
                    si = sbi[ci % 2]
                    C.dma("sp", si[:, :, :].rearrange("p h d -> p (h d)"), SBS[ci, :, :], ds_sbi[ci % 2], r=[b_SBS[ci]], w=[si])
                    qps = proj(ci, WQ, 0, 512)
                    gps = [proj(ci, WG, 0, 512), proj(ci, WG, 512, 512)]
                    q_ = qr.next()
                    if ci >= 2:
                        rope_evac(qps, q_, tabq, ci, rt)
                    else:
                        C.op("act", lambda e: e.copy(out=q_[:, :, :].rearrange("p h d -> p (h d)"), in_=qps[:, :]), r=[qps], w=[q_])
                    vb_ = vb.next()
                    sg_ = sg.next()
                    for j in range(2):
                        C.op("act", lambda e, j=j: e.copy(out=vb_[:, 2 * j:2 * j + 2, :].rearrange("p h d -> p (h d)"), in_=vps[j][:, :]),
                             r=[vps[j]], w=[vb_], part=(j == 1))
                        C.op("act", lambda e, j=j: e.activation(out=sg_[:, 2 * j:2 * j + 2, :].rearrange("p h d -> p (h d)"),
                                                               in_=gps[j][:, :], func=AF.Silu), r=[gps[j]], w=[sg_], part=(j == 1))
                    if CUT == 6:
                        state_update(Sf, k_, vdf, 0)
                        continue
                    tb = PT.next()
                    tbv = tb[:, :]
                    for h in range(4):
                        C.op("pe", lambda e, h=h: e.transpose(out=tbv[:, h * 128:(h + 1) * 128], in_=q_[:, h, :], identity=IDb[:]),
                             r=[q_, IDb], w=[tb], inc=False, part=(h > 0))
                    for h in range(4):
                        C.op("pe", lambda e, h=h: e.transpose(out=tbv[:, 512 + h * 128:512 + (h + 1) * 128], in_=k_[:, h, :], identity=IDb[:]),
                             r=[k_, IDb], w=[tb], inc=(h == 3), part=True)
                    if CUT == 7:
                        state_update(Sf, k_, vdf, 0)
                        continue
                    qT_, qfT_, qbT_, kT_ = qT.next(), qfT.next(), qbT.next(), kT.next()
                    fl = lambda t_: t_[:, :, :].rearrange("p h d -> p (h d)")
                    if CUT != 9:
                        C.op("act", lambda e: e.copy(out=fl(qT_), in_=tbv[:, 0:512]), r=[tb], w=[qT_])
                        C.op("act", lambda e: e.copy(out=fl(kT_), in_=tbv[:, 512:1024]), r=[tb], w=[kT_])
                    if CUT != 8:
                        C.op("dve", lambda e: e.tensor_tensor(out=fl(qfT_), in0=fl(qT_), in1=fl(DQF), op=ALU.mult), r=[qT_, DQF], w=[qfT_])
                        C.op("dve", lambda e: e.tensor_tensor(out=fl(qbT_), in0=fl(qT_), in1=fl(DQB), op=ALU.mult), r=[qT_, DQB], w=[qbT_])
                    if CUT in (8, 9):
                        state_update(Sf, k_, vdf, 0)
                        continue
                    if CUT == 3:
                        state_update(Sf, k_, vdf, 0)
                        continue
                    scb = PB.next()
                    for h in range(4):
                        C.op("pe", lambda e, h=h: e.matmul(scb[:, h * 128:(h + 1) * 128], lhsT=kT_[:, h, :], rhs=qT_[:, h, :],
                                                           start=True, stop=True), r=[kT_, qT_], w=[scb], inc=(h == 3), part=(h > 0))
                    sT_ = sT.next()
                    C.op("dve", lambda e: e.tensor_tensor(out=fl(sT_), in0=scb[:, :], in1=fl(DM), op=ALU.mult), r=[scb, DM], w=[sT_])
                    ob = [PB.next(), PB.next()]
                    for h in range(4):
                        bk = ob[h // 2]
                        oap = bk[:, (h % 2) * 256:(h % 2) * 256 + 256]
                        C.op("pe", lambda e, h=h, oap=oap: e.matmul(oap, lhsT=sT_[:, h, :], rhs=vb_[:, h, :], start=True, stop=False),
                             r=[sT_, vb_], w=[bk], inc=False, part=(h % 2 == 1))
                        C.op("pe", lambda e, h=h, oap=oap: e.matmul(oap, lhsT=qfT_[:, h, :], rhs=sf_cur[:, h, :], start=False, stop=False),
                             r=[qfT_, sf_cur], w=[bk], inc=False, part=True)
                        C.op("pe", lambda e, h=h, oap=oap: e.matmul(oap, lhsT=qbT_[:, h, :], rhs=si[:, h, :], start=False, stop=True),
                             r=[qbT_, si], w=[bk], inc=True, part=True)
                    if CUT == 4:
                        state_update(Sf, k_, vdf, 0)
                        continue
                    for h in range(4):
                        bk = ob[h // 2]
                        C.op("dve", lambda e, h=h, bk=bk: e.bn_stats(out=st6[:, h, :], in_=bk[:, (h % 2) * 256:(h % 2) * 256 + 256]),
                             r=[bk], w=[st6], part=(h > 0))
                    for h in range(4):
                        C.op("dve", lambda e, h=h: e.bn_aggr(out=mv[:, h, :], in_=st6[:, h, :]), r=[st6], w=[mv], part=(h > 0))
                    C.op("act", lambda e: e.activation(out=rs[:], in_=mv[:, :, 1], func=AF.Sqrt, bias=cs("eps")[:, 0:1]), r=[mv, CS], w=[rs])
                    C.op("dve", lambda e: e.reciprocal(out=rs[:], in_=rs[:]), r=[rs], w=[rs])
                    yr_ = yr.next()
                    for h in range(4):
                        bk = ob[h // 2]
                        yn_ = yn.next()
                        C.op("dve", lambda e, h=h, bk=bk, yn_=yn_: e.tensor_scalar(
                            out=yn_[:], in0=bk[:, (h % 2) * 256:(h % 2) * 256 + 256], scalar1=mv[:, h, 0:1], scalar2=rs[:, h:h + 1],
                            op0=ALU.subtract, op1=ALU.mult), r=[bk, mv, rs], w=[yn_])
                        C.op("dve", lambda e, h=h, yn_=yn_: e.tensor_tensor(out=yr_[:, h * 256:(h + 1) * 256], in0=yn_[:], in1=sg_[:, h, :],
                                                                            op=ALU.mult), r=[yn_, sg_], w=[yr_], part=(h > 0))
                    if CUT == 5:
                        state_update(Sf, k_, vdf, 0)
                        continue
                    tb2 = PT.next()
                    tb2v = tb2[:, :]
                    for cc in range(KC):
                        C.op("pe", lambda e, cc=cc: e.transpose(out=tb2v[:, cc * 128:(cc + 1) * 128], in_=yr_[:, cc * 128:(cc + 1) * 128],
                                                                identity=IDb[:]), r=[yr_, IDb], w=[tb2], inc=(cc == KC - 1), part=(cc > 0))
                    yT_ = yT[ci % 2]
                    C.op("act", lambda e: e.copy(out=yT_[:, :, :].rearrange("p c t -> p (c t)"), in_=tb2v[:, :]), r=[tb2], w=[yT_])
                    C.dma("sp", YRT[:, :, ci * 128:(ci + 1) * 128], yT_[:], ds_yT[ci % 2], r=[yT_], w=[b_YRT[ci]])
                state_update(Sf, k_, vdf, 0)
                sf_cur = sfb.next()
                C.op("act", lambda e: e.copy(out=sf_cur[:], in_=Sf[:]), r=[Sf], w=[sf_cur])
            C.barrier()

    def chunks_of(ti):
        off, n = TILES[ti]
        return list(range(off // 128, (off + n) // 128))

    def stage_m1(l, b, tiles):
        with ExitStack() as s:
            dsw = C.dsem(sw=True)
            WGR = load_w(s, w_in[l, :, O_GT:O_GT + 1024], 1024, KC, dsw, "WGR")
            WRO = load_w(s, w_ret_out[l, :, :], 1024, KC, dsw, "WRO")
            yt = [C.sb(s, [128, KC, 512], BF16, "yt") for _ in range(2)]
            ds_yt = [C.dsem() for _ in range(2)]
            mg = [C.sb(s, [128, KC, 512], F32, "mg") for _ in range(2)]
            ds_mg = [C.dsem() for _ in range(2)]
            gs = Ring([C.sb(s, [128, 512], F32, "gs") for _ in range(2)])
            for i, ti in enumerate(tiles):
                off, n = TILES[ti]
                y_ = yt[i % 2]
                m_ = mg[i % 2]
                C.dma("sp", y_[:, :, :n], YRT[:, :, off:off + n], ds_yt[i % 2], r=[b_YRT[c] for c in chunks_of(ti)], w=[y_])
                for cc in range(KC):
                    rb = PB.next()
                    mm_group(rb[:, :n], [(WRO[:, kc, cc * 128:(cc + 1) * 128], y_[:, kc, :n]) for kc in range(KC)], rb, [WRO, y_])
                    gb = PB.next()
                    mm_group(gb[:, :n], [(WGR[:, kc, cc * 128:(cc + 1) * 128], hT[:, kc, off:off + n]) for kc in range(KC)], gb, [WGR, hT])
                    g_ = gs.next()
                    C.op("act", lambda e: e.activation(out=g_[:, :n], in_=gb[:, :n], func=AF.Sigmoid), r=[gb], w=[g_])
                    C.op("dve", lambda e, cc=cc: e.tensor_tensor(out=m_[:, cc, :n], in0=rb[:, :n], in1=g_[:, :n], op=ALU.mult),
                         r=[rb, g_], w=[m_], part=(cc > 0))
                C.dma("sp", MG[:, :, off:off + n], m_[:, :, :n], ds_mg[i % 2], r=[m_], w=[b_MG[ti]])
            C.barrier()

    def stage_cp(l, b, tiles, U, P):
        with ExitStack() as s:
            dsw = C.dsem(sw=True)
            WC = load_w(s, w_in[l, :, O_CC:O_CC + 512], 512, KC, dsw, "WC")
            WX = load_w(s, w_in[l, :, O_CX:O_CX + 512], 512, KC, dsw, "WX")
            WP = load_w(s, w_in[l, :, O_PI:O_PI + 512], 512, KC, dsw, "WP")
            csb = Ring([C.sb(s, [128, 512], F32, "csb") for _ in range(2)])
            C.op("dve", lambda e: e.memset(U[:], 0.0), w=[U])
            C.op("dve", lambda e: e.memset(P[:], 0.0), w=[P])
            for ti in tiles:
                off, n = TILES[ti]
                uc = ucol(off)
                for ch in range(4):
                    cb = PB.next()
                    mm_group(cb[:, :n], [(WC[:, kc, ch * 128:(ch + 1) * 128], hT[:, kc, off:off + n]) for kc in range(KC)], cb, [WC, hT])
                    xb = PB.next()
                    mm_group(xb[:, :n], [(WX[:, kc, ch * 128:(ch + 1) * 128], hT[:, kc, off:off + n]) for kc in range(KC)], xb, [WX, hT])
                    pb = PB.next()
                    mm_group(pb[:, :n], [(WP[:, kc, ch * 128:(ch + 1) * 128], hT[:, kc, off:off + n]) for kc in range(KC)], pb, [WP, hT])
                    c_ = csb.next()
                    C.op("act", lambda e: e.copy(out=c_[:, :n], in_=cb[:, :n]), r=[cb], w=[c_])
                    C.op("dve", lambda e, ch=ch: e.tensor_tensor(out=U[:, ch, uc:uc + n], in0=xb[:, :n], in1=c_[:, :n], op=ALU.mult),
                         r=[xb, c_], w=[U], part=True)
                    C.op("act", lambda e, ch=ch: e.copy(out=P[:, ch, uc:uc + n], in_=pb[:, :n]), r=[pb], w=[P], part=True)
            C.barrier()

    def stage_m2(l, b, tiles, U):
        with ExitStack() as s:
            dsw = C.dsem(sw=True)
            WB = load_w(s, w_in[l, :, O_CB:O_CB + 512], 512, KC, dsw, "WB")
            WGC = load_w(s, w_in[l, :, O_GT + 1024:O_GT + 2048], 1024, KC, dsw, "WGC")
            WCO = load_w(s, w_conv_out[l, :, :], 1024, 4, dsw, "WCO")
            cw = sp("conv_w", l)
            mg = [C.sb(s, [128, KC, 512], F32, "mg2") for _ in range(2)]
            ds_mg = [C.dsem() for _ in range(2)]
            ds_mgo = [C.dsem() for _ in range(2)]
            cv = Ring([C.sb(s, [128, 512], F32, "cv") for _ in range(2)])
            yc = Ring([C.sb(s, [128, 4, 512], BF16, "yc") for _ in range(2)])
            gs = Ring([C.sb(s, [128, 512], F32, "gs2") for _ in range(2)])
            tt = Ring([C.sb(s, [128, 512], F32, "tt2") for _ in range(2)])
            for i, ti in enumerate(tiles):
                off, n = TILES[ti]
                uc = ucol(off)
                m_ = mg[i % 2]
                C.dma("sp", m_[:, :, :n], MG[:, :, off:off + n], ds_mg[i % 2], r=[b_MG[ti]], w=[m_])
                yc_ = yc.next()
                for ch in range(4):
                    bb = PB.next()
                    mm_group(bb[:, :n], [(WB[:, kc, ch * 128:(ch + 1) * 128], hT[:, kc, off:off + n]) for kc in range(KC)], bb, [WB, hT])
                    cv_ = cv.next()
                    C.op("dve", lambda e, ch=ch: e.tensor_scalar(out=cv_[:, :n], in0=U[:, ch, uc - 1:uc - 1 + n], scalar1=cw[:, ch:ch + 1],
                                                                 scalar2=None, op0=ALU.mult), r=[U, SP_], w=[cv_])
                    for k in (1, 2):
                        C.op("dve", lambda e, ch=ch, k=k: e.scalar_tensor_tensor(
                            out=cv_[:, :n], in0=U[:, ch, uc - 1 + k:uc - 1 + k + n], scalar=cw[:, k * 4 + ch:k * 4 + ch + 1],
                            in1=cv_[:, :n], op0=ALU.mult, op1=ALU.add), r=[U, SP_, cv_], w=[cv_])
                    C.op("dve", lambda e, ch=ch: e.tensor_tensor(out=yc_[:, ch, :n], in0=bb[:, :n], in1=cv_[:, :n], op=ALU.mult),
                         r=[bb, cv_], w=[yc_], part=(ch > 0))
                for cc in range(KC):
                    cb = PB.next()
                    mm_group(cb[:, :n], [(WCO[:, ch, cc * 128:(cc + 1) * 128], yc_[:, ch, :n]) for ch in range(4)], cb, [WCO, yc_])
                    gb = PB.next()
                    mm_group(gb[:, :n], [(WGC[:, kc, cc * 128:(cc + 1) * 128], hT[:, kc, off:off + n]) for kc in range(KC)], gb, [WGC, hT])
                    g_ = gs.next()
                    C.op("act", lambda e: e.activation(out=g_[:, :n], in_=gb[:, :n], func=AF.Sigmoid), r=[gb], w=[g_])
                    t_ = tt.next()
                    C.op("dve", lambda e: e.tensor_tensor(out=t_[:, :n], in0=cb[:, :n], in1=g_[:, :n], op=ALU.mult), r=[cb, g_], w=[t_])
                    C.op("dve", lambda e, cc=cc: e.tensor_tensor(out=m_[:, cc, :n], in0=m_[:, cc, :n], in1=t_[:, :n], op=ALU.add),
                         r=[m_, t_], w=[m_])
                C.dma("sp", MG[:, :, off:off + n], m_[:, :, :n], ds_mgo[i % 2], r=[m_], w=[b_MG[ti]])
            C.barrier()

    def stage_m3(l, b, tiles, P):
        with ExitStack() as s:
            dsw = C.dsem(sw=True)
            WGP = load_w(s, w_in[l, :, O_GT + 2048:O_GT + 3072], 1024, KC, dsw, "WGP")
            WPO = load_w(s, w_pool_out[l, :, :], 1024, 4, dsw, "WPO")
            WO = load_w(s, w_o[l, :, :], 1024, KC, dsw, "WO")
            PW = C.sb(s, [128, 4, 128], BF16, "PW")
            dspw = C.dsem(sw=True)
            for gI in range(4):
                C.dma("pool", PW[:, gI, :], pool_w[l, gI, :, :], dspw, r=[b_in], w=[PW], part=(gI > 0))
            et = C.sb(s, [128, 8], F32, "et")
            ive = cs("ive")
            psc = sp("pool_scale", l)
            mg = C.sb(s, [128, KC, 512], F32, "mg3")
            ds_mg = C.dsem()
            xt = C.sb(s, [128, KC, 512], F32, "xt3")
            ds_xt = C.dsem()
            ds_xo = C.dsem()
            wa = C.sb(s, [128, 528], F32, "wa3")
            wb_ = C.sb(s, [128, 528], F32, "wb3")
            pl = Ring([C.sb(s, [128, 512], BF16, "pl") for _ in range(2)])
            yp = Ring([C.sb(s, [128, 4, 512], BF16, "yp") for _ in range(2)])
            gs = Ring([C.sb(s, [128, 512], F32, "gs3") for _ in range(2)])
            tt = Ring([C.sb(s, [128, 512], F32, "tt3") for _ in range(2)])
            mgb = C.sb(s, [128, KC, 512], BF16, "mgb")
            src = xin if l == 0 else XT
            for ti in tiles:
                off, n = TILES[ti]
                uc = ucol(off)
                r = 2 if ti == 0 else b
                C.dma("sp", mg[:, :, :n], MG[:, :, off:off + n], ds_mg, r=[b_MG[ti]], w=[mg])
                C.dma("sp", xt[:, :, :n], src[b, :, :, off:off + n], ds_xt, r=[b_in if l == 0 else b_XT[b][ti]], w=[xt])
                yp_ = yp.next()
                for gI, W in enumerate((2, 4, 8, 16)):
                    hw = W // 2
                    lo = uc - hw
                    ln = n + W - 2
                    C.op("dve", lambda e, gI=gI, lo=lo, ln=ln: e.tensor_tensor(out=wa[:, :ln], in0=P[:, gI, lo:lo + ln], in1=P[:, gI, lo + 1:lo + 1 + ln],
                                                                               op=ALU.add), r=[P], w=[wa])
                    cur, oth = wa, wb_
                    step = 2
                    while step < W:
                        ln2 = ln - step
                        C.op("dve", lambda e, cur=cur, oth=oth, ln2=ln2, step=step: e.tensor_tensor(
                            out=oth[:, :ln2], in0=cur[:, :ln2], in1=cur[:, step:step + ln2], op=ALU.add), r=[cur], w=[oth])
                        cur, oth = oth, cur
                        ln = ln2
                        step *= 2
                    assert ln == n
                    pl_ = pl.next()
                    C.op("dve", lambda e, cur=cur, gI=gI, W=W: e.scalar_tensor_tensor(
                        out=pl_[:, :n], in0=cur[:, :n], scalar=1.0 / W, in1=P[:, gI, uc:uc + n], op0=ALU.mult, op1=ALU.subtract),
                        r=[cur, P], w=[pl_])
                    edges = {0: [(0, 0), (n - 8, 1)], 1: [(0, 2)], 4: [(n - 8, 3)]}.get(ti, [])
                    for (e0, k) in edges:
                        io = (gI * 4 + k) * 8
                        C.op("dve", lambda e, cur=cur, e0=e0, io=io: e.tensor_tensor(out=et[:, 0:8], in0=cur[:, e0:e0 + 8], in1=ive[:, io:io + 8], op=ALU.mult),
                             r=[cur, CS], w=[et])
                        C.op("dve", lambda e, e0=e0, gI=gI: e.tensor_tensor(out=pl_[:, e0:e0 + 8], in0=et[:, 0:8], in1=P[:, gI, uc + e0:uc + e0 + 8], op=ALU.subtract),
                             r=[et, P], w=[pl_], part=True)
                    mb = PB.next()
                    mm_group(mb[:, :n], [(PW[:, gI, :], pl_[:, :n])], mb, [PW, pl_])
                    C.op("act", lambda e, gI=gI: e.activation(out=yp_[:, gI, :n], in_=mb[:, :n], func=AF.Identity, scale=psc[:, gI:gI + 1]),
                         r=[mb, SP_], w=[yp_], part=(gI > 0))
                for cc in range(KC):
                    pb = PB.next()
                    mm_group(pb[:, :n], [(WPO[:, ch, cc * 128:(cc + 1) * 128], yp_[:, ch, :n]) for ch in range(4)], pb, [WPO, yp_])
                    gb = PB.next()
                    mm_group(gb[:, :n], [(WGP[:, kc, cc * 128:(cc + 1) * 128], hT[:, kc, off:off + n]) for kc in range(KC)], gb, [WGP, hT])
                    g_ = gs.next()
                    C.op("act", lambda e: e.activation(out=g_[:, :n], in_=gb[:, :n], func=AF.Sigmoid), r=[gb], w=[g_])
                    t_ = tt.next()
                    C.op("dve", lambda e: e.tensor_tensor(out=t_[:, :n], in0=pb[:, :n], in1=g_[:, :n], op=ALU.mult), r=[pb, g_], w=[t_])
                    C.op("dve", lambda e, cc=cc: e.tensor_tensor(out=mgb[:, cc, :n], in0=mg[:, cc, :n], in1=t_[:, :n], op=ALU.add),
                         r=[mg, t_], w=[mgb], part=(cc > 0))
                for cc in range(KC):
                    yb = PB.next()
                    mm_group(yb[:, :n], [(WO[:, kc, cc * 128:(cc + 1) * 128], mgb[:, kc, :n]) for kc in range(KC)], yb, [WO, mgb])
                    C.op("dve", lambda e, cc=cc: e.scalar_tensor_tensor(
                        out=xt[:, cc, :n], in0=yb[:, :n], scalar=MOD[:, 16 + cc, r:r + 1], in1=xt[:, cc, :n], op0=ALU.mult, op1=ALU.add),
                        r=[yb, MOD, xt], w=[xt])
                C.dma("sp", XT[b, :, :, off:off + n], xt[:, :, :n], ds_xo, r=[xt], w=[b_XT[b][ti]])
            C.barrier()

    def stage_moe(l, b, tiles):
        last = (l == last_l)
        with ExitStack() as s:
            XS = C.sb(s, [128, KC, NT], F32, "XS")
            xsb = [Buf() for _ in TILES]
            ds_xs = [C.dsem() for _ in TILES]
            COMBT = C.sb(s, [16, NT], F32, "COMBT")
            for ti in tiles:
                off, n = TILES[ti]
                C.dma("sp", XS[:, :, off:off + n], XT[b, :, :, off:off + n], ds_xs[ti], r=[b_XT[b][ti]], w=[xsb[ti]])
            wrl = C.sb(s, [128, KC, 20], F32, "wrl")
            C.dma("sp", wrl[:, :, :].rearrange("p k n -> p (k n)"), wr[:, l * KC * 20:(l + 1) * KC * 20], C.dsem(), r=[b_in], w=[wrl])
            brow = sp("b_r", l)
            with ExitStack() as s2:
                hf = C.sb(s2, [128, KC, 512], F32, "hf")
                sq = C.sb(s2, [128, KC, 512], F32, "sq2")
                rstd = C.sb(s2, [128, 512], F32, "rstd2")
                tmp = Ring([C.sb(s2, [128, 512], F32, "ntmp2") for _ in range(2)])
                R = {k: C.sb(s2, shp, F32, "r_" + k) for k, shp in dict(
                    lg=[128, 20], mx=[128, 1], nmx=[128, 1], eg=[128, 4], se=[128, 1], gtop=[128, 1], ohg=[128, 4], ing=[128, 4],
                    m1=[128, 1], oh1=[128, 4], msk=[128, 4], m2=[128, 1], oh2=[128, 4], nm1=[128, 1], e2=[128, 1], den=[128, 1],
                    w1=[128, 1], w2=[128, 1], loc=[128, 4], comb=[128, 16]).items()}
                for ti in tiles:
                    off, n = TILES[ti]
                    norm_tile(s2, l, b, ti, XS[:, :, off:off + n], xsb[ti], GS2, 24, (sq, rstd, tmp), hf=hf)
                    for cj in range(n // 128):
                        tok = off + cj * 128
                        lb = PB.next()
                        mm_group(lb[:, 0:20], [(hf[:, kc, cj * 128:(cj + 1) * 128], wrl[:, kc, :]) for kc in range(KC)], lb, [hf, wrl])
                        V = lambda fn, r, w: C.op("dve", fn, r=r, w=w)
                        A = lambda fn, r, w: C.op("act", fn, r=r, w=w)
                        V(lambda e: e.tensor_tensor(out=R["lg"][:], in0=lb[:, 0:20], in1=brow, op=ALU.add), [lb, SP_], [R["lg"]])
                        lgg = R["lg"][:, 0:4]
                        V(lambda e: e.reduce_max(out=R["mx"][:], in_=lgg, axis=AX.X), [R["lg"]], [R["mx"]])
                        V(lambda e: e.tensor_scalar(out=R["nmx"][:], in0=R["mx"][:], scalar1=-1.0, scalar2=None, op0=ALU.mult), [R["mx"]], [R["nmx"]])
                        A(lambda e: e.activation(out=R["eg"][:], in_=lgg, func=AF.Exp, bias=R["nmx"][:, 0:1]),
                          [R["lg"], R["nmx"]], [R["eg"]])
                        V(lambda e: e.reduce_sum(out=R["se"][:], in_=R["eg"][:], axis=AX.X), [R["eg"]], [R["se"]])
                        V(lambda e: e.reciprocal(out=R["gtop"][:], in_=R["se"][:]), [R["se"]], [R["gtop"]])
                        V(lambda e: e.tensor_scalar(out=R["ohg"][:], in0=lgg, scalar1=R["mx"][:, 0:1], scalar2=None, op0=ALU.is_ge),
                          [R["lg"], R["mx"]], [R["ohg"]])
                        V(lambda e: e.tensor_scalar(out=R["ing"][:], in0=R["lg"][:, 4:8], scalar1=R["ohg"][:, 0:1], scalar2=None, op0=ALU.mult),
                          [R["lg"], R["ohg"]], [R["ing"]])
                        for gI in range(1, 4):
                            V(lambda e, gI=gI: e.scalar_tensor_tensor(out=R["ing"][:], in0=R["lg"][:, 4 + 4 * gI:8 + 4 * gI], scalar=R["ohg"][:, gI:gI + 1],
                                                                      in1=R["ing"][:], op0=ALU.mult, op1=ALU.add), [R["lg"], R["ohg"], R["ing"]], [R["ing"]])
                        V(lambda e: e.reduce_max(out=R["m1"][:], in_=R["ing"][:], axis=AX.X), [R["ing"]], [R["m1"]])
                        V(lambda e: e.tensor_scalar(out=R["oh1"][:], in0=R["ing"][:], scalar1=R["m1"][:, 0:1], scalar2=None, op0=ALU.is_ge),
                          [R["ing"], R["m1"]], [R["oh1"]])
                        V(lambda e: e.scalar_tensor_tensor(out=R["msk"][:], in0=R["oh1"][:], scalar=-1e30, in1=R["ing"][:], op0=ALU.mult, op1=ALU.add),
                          [R["oh1"], R["ing"]], [R["msk"]])
                        V(lambda e: e.reduce_max(out=R["m2"][:], in_=R["msk"][:], axis=AX.X), [R["msk"]], [R["m2"]])
                        V(lambda e: e.tensor_scalar(out=R["oh2"][:], in0=R["msk"][:], scalar1=R["m2"][:, 0:1], scalar2=None, op0=ALU.is_ge),
                          [R["msk"], R["m2"]], [R["oh2"]])
                        V(lambda e: e.tensor_scalar(out=R["nm1"][:], in0=R["m1"][:], scalar1=-1.0, scalar2=None, op0=ALU.mult), [R["m1"]], [R["nm1"]])
                        A(lambda e: e.activation(out=R["e2"][:], in_=R["m2"][:], func=AF.Exp, bias=R["nm1"][:, 0:1]), [R["m2"], R["nm1"]], [R["e2"]])
                        V(lambda e: e.tensor_scalar(out=R["den"][:], in0=R["e2"][:], scalar1=1.0, scalar2=None, op0=ALU.add), [R["e2"]], [R["den"]])
                        V(lambda e: e.reciprocal(out=R["w1"][:], in_=R["den"][:]), [R["den"]], [R["w1"]])
                        V(lambda e: e.tensor_tensor(out=R["w1"][:], in0=R["w1"][:], in1=R["gtop"][:], op=ALU.mult), [R["w1"], R["gtop"]], [R["w1"]])
                        V(lambda e: e.tensor_tensor(out=R["w2"][:], in0=R["w1"][:], in1=R["e2"][:], op=ALU.mult), [R["w1"], R["e2"]], [R["w2"]])
                        V(lambda e: e.tensor_scalar(out=R["loc"][:], in0=R["oh1"][:], scalar1=R["w1"][:, 0:1], scalar2=None, op0=ALU.mult),
                          [R["oh1"], R["w1"]], [R["loc"]])
                        V(lambda e: e.scalar_tensor_tensor(out=R["loc"][:], in0=R["oh2"][:], scalar=R["w2"][:, 0:1], in1=R["loc"][:], op0=ALU.mult, op1=ALU.add),
                          [R["oh2"], R["w2"], R["loc"]], [R["loc"]])
                        for gI in range(4):
                            C.op("dve", lambda e, gI=gI: e.tensor_scalar(out=R["comb"][:, 4 * gI:4 * gI + 4], in0=R["loc"][:], scalar1=R["ohg"][:, gI:gI + 1],
                                                                         scalar2=None, op0=ALU.mult), r=[R["loc"], R["ohg"]], w=[R["comb"]], part=(gI > 0))
                        tb = PB.next()
                        C.op("pe", lambda e: e.transpose(out=tb[0:16, 0:128], in_=R["comb"][:], identity=IDf), r=[R["comb"], CS], w=[tb])
                        C.op("act", lambda e, tok=tok: e.copy(out=COMBT[:, tok:tok + 128], in_=tb[0:16, 0:128]), r=[tb], w=[COMBT], part=True)
                C.barrier()
            with ExitStack() as s3:
                w1e = [C.sb(s3, [128, KC, 512], BF16, "w1e") for _ in range(2)]
                w3e = [C.sb(s3, [128, KC, 512], BF16, "w3e") for _ in range(2)]
                w2e = [C.sb(s3, [128, 4, 1024], BF16, "w2e") for _ in range(2)]
                ds_e = [[C.dsem(sw=True) for _ in range(3)] for _ in range(2)]
                sel = Ring([C.sb(s3, [16, 128], F32, "sel") for _ in range(2)])
                cbs = Ring([C.sb(s3, [128, 512], F32, "cbs") for _ in range(2)])
                s1 = Ring([C.sb(s3, [128, 512], F32, "s1") for _ in range(2)])
                s2_ = Ring([C.sb(s3, [128, 512], F32, "s2") for _ in range(2)])
                act = Ring([C.sb(s3, [128, 4, 512], BF16, "act") for _ in range(2)])

                def load_e(e_):
                    k = e_ % 2
                    v1 = w1[l, e_].rearrange("(kc p) n -> p kc n", p=128)
                    v3 = w3[l, e_].rearrange("(kc p) n -> p kc n", p=128)
                    v2 = w2[l, e_].rearrange("(kc p) n -> p kc n", p=128)
                    for kc in range(KC):
                        C.dma("pool", w1e[k][:, kc, :], v1[:, kc, :], ds_e[k][0], r=[b_in], w=[w1e[k]], part=(kc > 0))
                        C.dma("pool", w3e[k][:, kc, :], v3[:, kc, :], ds_e[k][1], r=[b_in], w=[w3e[k]], part=(kc > 0))
                    for fc in range(4):
                        C.dma("pool", w2e[k][:, fc, :], v2[:, fc, :], ds_e[k][2], r=[b_in], w=[w2e[k]], part=(fc > 0))

                load_e(0)
                for e_ in range(16):
                    if e_ + 1 < 16:
                        load_e(e_ + 1)
                    k = e_ % 2
                    se_ = sel.next()
                    C.op("dve", lambda e, e_=e_: e.tensor_copy(out=se_[:], in_=IDf[0:16, e_:e_ + 1].to_broadcast([16, 128])), r=[CS], w=[se_])
                    for ti in tiles:
                        off, n = TILES[ti]
                        r = 2 if ti == 0 else b
                        cb = PB.next()
                        mm_group(cb[:, :n], [(se_[:], COMBT[:, off:off + n])], cb, [se_, COMBT])
                        cb_ = cbs.next()
                        C.op("act", lambda e: e.copy(out=cb_[:, :n], in_=cb[:, :n]), r=[cb], w=[cb_])
                        a_ = act.next()
                        for fc in range(4):
                            z1 = PB.next()
                            mm_group(z1[:, :n], [(w1e[k][:, kc, fc * 128:(fc + 1) * 128], hT[:, kc, off:off + n]) for kc in range(KC)], z1, [w1e[k], hT])
                            z3 = PB.next()
                            mm_group(z3[:, :n], [(w3e[k][:, kc, fc * 128:(fc + 1) * 128], hT[:, kc, off:off + n]) for kc in range(KC)], z3, [w3e[k], hT])
                            a1 = s1.next()
                            C.op("act", lambda e: e.activation(out=a1[:, :n], in_=z1[:, :n], func=AF.Silu), r=[z1], w=[a1])
                            a2 = s2_.next()
                            C.op("dve", lambda e: e.tensor_tensor(out=a2[:, :n], in0=a1[:, :n], in1=cb_[:, :n], op=ALU.mult), r=[a1, cb_], w=[a2])
                            C.op("dve", lambda e, fc=fc: e.tensor_tensor(out=a_[:, fc, :n], in0=z3[:, :n], in1=a2[:, :n], op=ALU.mult),
                                 r=[z3, a2], w=[a_], part=(fc > 0))
                        for cc in range(KC):
                            yb = PB.next()
                            mm_group(yb[:, :n], [(w2e[k][:, fc, cc * 128:(cc + 1) * 128], a_[:, fc, :n]) for fc in range(4)], yb, [w2e[k], a_])
                            C.op("dve", lambda e, cc=cc: e.scalar_tensor_tensor(
                                out=XS[:, cc, off:off + n], in0=yb[:, :n], scalar=MOD[:, 40 + cc, r:r + 1], in1=XS[:, cc, off:off + n],
                                op0=ALU.mult, op1=ALU.add), r=[yb, MOD, xsb[ti]], w=[xsb[ti]])
                C.barrier()
            if not last:
                for ti in tiles:
                    off, n = TILES[ti]
                    C.dma("sp", XT[b, :, :, off:off + n], XS[:, :, off:off + n], ds_xs[ti], r=[xsb[ti]], w=[b_XT[b][ti]])
            else:
                with ExitStack() as s4:
                    sq = C.sb(s4, [128, KC, 512], F32, "sq4")
                    rstd = C.sb(s4, [128, 512], F32, "rstd4")
                    tmp = Ring([C.sb(s4, [128, 512], F32, "ntmp4") for _ in range(2)])
                    ot = [C.sb(s4, [128, KC, 512], F32, "ot") for _ in range(2)]
                    ds_o = [C.dsem() for _ in range(2)]
                    fn_ = sp("final_norm")
                    FN = C.sb(s4, [128, KC, 3], F32, "FN")
                    for kc in range(KC):
                        C.op("dve", lambda e, kc=kc: e.tensor_copy(out=FN[:, kc, :], in_=fn_[:, kc:kc + 1].to_broadcast([128, 3])), r=[SP_], w=[FN], part=True)
                    for i, ti in enumerate(t for t in tiles if t > 0):
                        off, n = TILES[ti]
                        o_ = ot[i % 2]

                        def out_fn(kc, tb, n, o_=o_):
                            C.op("act", lambda e: e.copy(out=o_[:, kc, :n], in_=tb[:, :n]), r=[tb], w=[o_], part=True)
                        norm_tile(s4, l, b, ti, XS[:, :, off:off + n], xsb[ti], FN, 0, (sq, rstd, tmp), out_fn=out_fn)
                        C.dma("sp", outT[b, :, :, off - NCTX:off - NCTX + n], o_[:, :, :n], ds_o[i % 2], r=[o_], w=[b_out], part=True)
            C.barrier()

    stages = 0

    def done():
        nonlocal stages
        C.reset_dsems()
        stages += 1
        return stop_after is not None and stages >= stop_after

    def program():
        for l in range(n_layers):
            last = (l == last_l)
            layer_setup(l)
            C.reset_dsems()
            tiles_all = list(range(5))
            tiles_out = [1, 2, 3, 4] if last else tiles_all
            for b in range(NB):
                stage_norm1(l, b, tiles_all)
                if done(): return
                stage_ret(l, b)
                if done(): return
                stage_m1(l, b, tiles_out)
                if done(): return
                with ExitStack() as su:
                    U = C.sb(su, [128, 4, LT], BF16, "U")
                    P = C.sb(su, [128, 4, LT], BF16, "P")
                    stage_cp(l, b, tiles_out, U, P)
                    C.reset_dsems()
                    stage_m2(l, b, tiles_out, U)
                    if done(): return
                    stage_m3(l, b, tiles_out, P)
                if done(): return
                if last:
                    pass
                stage_moe(l, b, tiles_out)
                if done(): return

    program()
    C.barrier()
    if dbg:
        dh = dram("dbg_hT", [128, KC, NT], BF16, "ExternalOutput")
        C.dma("sp", dh[:, :, :], hT[:], ds_misc, r=[hT], w=[b_out])
    C.barrier()
    es.close()
    return nc


def _const_layout():
    off = {}
    o = 0
    for name, n in (("ident", 128), ("ones", 128), ("relp", 128), ("reln", 128), ("mf", 128), ("mb", 128),
                    ("iota1", 128), ("iotac", 128), ("jcol", 2), ("eps", 1), ("ive", 128)):
        off[name] = (o, n)
        o += n
    return off, o


CONST_OFF, CONST_W = _const_layout()


def _small_layout():
    off = {}
    o = 0
    for name, n in (("b_ada", DEPTH * 48), ("norm1", DEPTH * 8), ("norm2", DEPTH * 8), ("ret_decay", DEPTH * 8),
                    ("conv_w", DEPTH * 12), ("pool_scale", DEPTH * 4), ("b_r", DEPTH * 20), ("final_norm", 8)):
        off[name] = (o, n)
        o += n
    return off, o


SMALL_OFF, SMALL_W = _small_layout()


def _consts():
    c = np.zeros((128, CONST_W), np.float32)
    j = np.arange(128, dtype=np.float32)
    rel = j[None, :] - j[:, None]
    put = lambda name, v: c.__setitem__((slice(None), slice(CONST_OFF[name][0], CONST_OFF[name][0] + CONST_OFF[name][1])), v)
    put("ident", np.eye(128, dtype=np.float32))
    put("ones", np.ones((128, 128), np.float32))
    put("relp", np.maximum(rel, 0))
    put("reln", np.maximum(-rel, 0))
    put("mf", (rel >= 0).astype(np.float32))
    put("mb", (rel <= 0).astype(np.float32))
    put("iota1", np.broadcast_to(j[None, :] + 1.0, (128, 128)))
    put("iotac", np.broadcast_to(128.0 - j[None, :], (128, 128)))
    put("jcol", np.stack([j, 127.0 - j], axis=1))
    put("eps", np.full((128, 1), EPS, np.float32))
    pos = np.arange(SEQ)
    row = (pos // 64).astype(np.float32)
    col = (pos % 64).astype(np.float32)
    inv = (10000.0 ** (-np.arange(32, dtype=np.float32) / 32)).astype(np.float32)
    ang = np.concatenate([row[:, None] * inv[None, :], col[:, None] * inv[None, :]], axis=-1).astype(np.float32)
    cos, sin = np.cos(ang), np.sin(ang)
    sc = np.float32(128.0 ** -0.5)
    tab = np.stack([cos, sin, cos * sc, sin * sc], axis=0).reshape(4, 16, 128, 64).transpose(2, 0, 1, 3)
    ropet = np.ascontiguousarray(tab.reshape(128, 4 * 16 * 64)).astype(np.float32)
    iv = np.zeros((4, NT), np.float32)
    for gi, win in enumerate((2, 4, 8, 16)):
        for (o, L) in ((0, NCTX), (NCTX, SEQ)):
            t = np.arange(L)
            lo = np.clip(t - win // 2, 0, L)
            hi = np.clip(t + win - win // 2, 0, L)
            iv[gi, o:o + L] = 1.0 / (hi - lo)
    ive = np.zeros((4, 4, 8), np.float32)
    for gi in range(4):
        ive[gi, 0] = iv[gi, 0:8]
        ive[gi, 1] = iv[gi, NCTX - 8:NCTX]
        ive[gi, 2] = iv[gi, NCTX:NCTX + 8]
        ive[gi, 3] = iv[gi, NT - 8:NT]
    put("ive", np.broadcast_to(ive.reshape(1, 128), (128, 128)))
    return c, ropet


def _fm(v):
    return np.ascontiguousarray(v.reshape(-1, 128).T)


def _small(inp):
    s = np.zeros((128, SMALL_W), np.float32)

    def put(name, v):
        o, n = SMALL_OFF[name]
        assert v.shape == (128, n), (name, v.shape, n)
        s[:, o:o + n] = v
    put("b_ada", np.concatenate([_fm(inp["b_ada"][l]) for l in range(DEPTH)], axis=1))
    put("norm1", np.concatenate([_fm(inp["norm1"][l]) for l in range(DEPTH)], axis=1))
    put("norm2", np.concatenate([_fm(inp["norm2"][l]) for l in range(DEPTH)], axis=1))
    put("ret_decay", np.broadcast_to(inp["ret_decay"].reshape(1, DEPTH * 8), (128, DEPTH * 8)))
    cw = inp["conv_w"].reshape(DEPTH, 3, 4, 128).transpose(3, 0, 1, 2).reshape(128, DEPTH * 12)
    put("conv_w", cw)
    put("pool_scale", inp["pool_scale"].reshape(DEPTH, 4, 128).transpose(2, 0, 1).reshape(128, DEPTH * 4))
    br = np.concatenate([inp["b_rg"], inp["b_re"]], axis=1)
    put("b_r", np.broadcast_to(br.reshape(1, DEPTH * 20), (128, DEPTH * 20)))
    put("final_norm", _fm(inp["final_norm"]))
    return s


def _prep(inputs, NB, core):
    x, ctx = inputs["x"], inputs["ctx"]
    bs = slice(core * NB, (core + 1) * NB)
    seq = np.concatenate([ctx[bs], x[bs]], axis=1)
    xin = np.ascontiguousarray(seq.reshape(NB, NT, KC, 128).transpose(0, 3, 2, 1))
    crow = np.concatenate([inputs["c"][bs], inputs["c_ctx"][None, :]] if NB == 2 else
                          [inputs["c"][bs], inputs["c"][bs], inputs["c_ctx"][None, :]], axis=0)
    cT = np.ascontiguousarray(crow.reshape(3, KC, 128).transpose(2, 1, 0))
    return xin, cT


def _shared(inputs):
    c, ropet = _consts()
    wrc = np.concatenate([inputs["w_rg"], inputs["w_re"]], axis=2)
    wr = np.ascontiguousarray(wrc.reshape(DEPTH, KC, 128, 20).transpose(2, 0, 1, 3).reshape(128, DEPTH * KC * 20))
    d = {"consts": c, "ropet": ropet, "wr": wr, "smallp": _small(inputs)}
    for k in ("w_ada", "w_in", "w_ret_out", "w_conv_out", "w_pool_out", "w_o", "pool_w", "w1", "w3", "w2"):
        d[k] = np.ascontiguousarray(inputs[k], dtype=np.float32)
    return d


def kernel(**inputs):
    inputs = {k: np.asarray(v, dtype=np.float32) for k, v in inputs.items()}
    n = 8
    NB = 2
    nc = build(NB=NB)
    shared = _shared(inputs)
    in_maps = []
    for core in range(n):
        xin, cT = _prep(inputs, NB, core)
        m = dict(shared)
        m["xin"] = xin
        m["cT"] = cT
        in_maps.append(m)
    res = run_bass_kernel_spmd(nc, in_maps, core_ids=list(range(n)))
    outs = []
    for r in res.results:
        o = np.asarray(r["outT"])
        outs.append(o.transpose(0, 3, 2, 1).reshape(NB, SEQ, D))
    return np.ascontiguousarray(np.concatenate(outs, axis=0)).astype(np.float32)
```

```python
import os
import numpy as np
from contextlib import ExitStack
CUT = int(os.environ.get('K_RET_CUT', '0'))
import concourse.bass as bass
import concourse.mybir as mybir
from concourse.bass_utils import run_bass_kernel_spmd

F32 = mybir.dt.float32
BF16 = mybir.dt.bfloat16
AF = mybir.ActivationFunctionType
ALU = mybir.AluOpType
AX = mybir.AxisListType

D = 1024
KC = 8
NCTX = 256
SEQ = 2048
NT = NCTX + SEQ
NCH = NT // 128
DEPTH = 4
EPS = 1e-6
TILES = [(0, 256), (256, 512), (768, 512), (1280, 512), (1792, 512)]
O_Q, O_K, O_V, O_G, O_CB, O_CC, O_CX, O_PI, O_GT = 0, 512, 1024, 2048, 3072, 3584, 4096, 4608, 5120
LT = 2336


def ucol(t):
    return 8 + t if t < NCTX else t + 24


ALLBUFS = []


class Buf:
    __slots__ = ("w", "r")

    def __init__(self):
        self.w = []
        self.r = []
        ALLBUFS.append(self)


class DSem:
    def __init__(self, sem):
        self.sem = sem
        self.cnt = 0


class T:
    def __init__(self, t):
        self.t = t
        self.b = Buf()

    def __getitem__(self, k):
        return self.t[k]


class Ctx:
    def __init__(self, nc, es):
        self.nc = nc
        self.es = es
        self.engs = {"pe": nc.tensor, "act": nc.scalar, "dve": nc.vector, "pool": nc.gpsimd, "sp": nc.sync}
        self.sem = {k: es.enter_context(nc.semaphore("s_" + k)) for k in self.engs}
        self.cnt = {k: 0 for k in self.engs}
        self.seen = {k: {} for k in self.engs}
        self.dsems = []
        self.nsb = 0

    def sb(self, es, shape, dt, name=None):
        self.nsb += 1
        return T(es.enter_context(self.nc.sbuf_tensor(f"{name or 't'}_{self.nsb}", list(shape), dt)))

    def dsem(self, perm=False, sw=False):
        if sw:
            if not hasattr(self, "swpool_"):
                self.swpool_ = []
                self.swptr = 0
            if self.swptr >= len(self.swpool_):
                s = DSem(self.es.enter_context(self.nc.semaphore(f"dw{len(self.dsems)}")))
                self.dsems.append(s)
                self.swpool_.append(s)
            s = self.swpool_[self.swptr]
            self.swptr += 1
            return s
        if perm:
            s = DSem(self.es.enter_context(self.nc.semaphore(f"dp{len(self.dsems)}")))
            self.dsems.append(s)
            return s
        if not hasattr(self, "pool_"):
            self.pool_ = []
            self.dptr = 0
        if self.dptr >= len(self.pool_):
            s = DSem(self.es.enter_context(self.nc.semaphore(f"d{len(self.dsems)}")))
            self.dsems.append(s)
            self.pool_.append(s)
        s = self.pool_[self.dptr]
        self.dptr += 1
        return s

    def _bufs(self, xs):
        return [x.b if isinstance(x, T) else x for x in xs if x is not None]

    def _wait(self, eng, deps):
        best = {}
        for key, val in deps:
            if best.get(key, 0) < val:
                best[key] = val
        e = self.engs[eng]
        for key, val in best.items():
            if self.seen[eng].get(key, 0) >= val:
                continue
            sem = key.sem if isinstance(key, DSem) else self.sem[key]
            e.wait_ge(sem, val)
            self.seen[eng][key] = val

    def _deps(self, eng, reads, writes, part=False, skipkey=None):
        deps = []
        for b in reads:
            deps += b.w
        for b in writes:
            deps += [d for d in b.w if d[0] is not skipkey]
            deps += b.r
        if eng == "pe":
            deps = [d for d in deps if d[0] != "pe"]
        return deps

    def op(self, eng, fn, r=(), w=(), inc=True, part=False):
        reads, writes = self._bufs(r), self._bufs(w)
        self._wait(eng, self._deps(eng, reads, writes, part))
        ins = fn(self.engs[eng])
        tk = (eng, self.cnt[eng] + 1)
        if inc:
            ins.then_inc(self.sem[eng], 1)
            self.cnt[eng] += 1
        for b in reads:
            if not b.r or b.r[-1] != tk:
                b.r.append(tk)
        for b in writes:
            if part:
                if not b.w or b.w[-1] != tk:
                    b.w.append(tk)
            else:
                b.w = [tk]
                b.r = []
        return ins

    def dma(self, q, out, in_, ds, r=(), w=(), part=False):
        reads, writes = self._bufs(r), self._bufs(w)
        self._wait(q, self._deps(q, reads, writes, part, ds if part else None))
        ins = self.engs[q].dma_start(out=out, in_=in_)
        ds.cnt += 16
        ins.then_inc(ds.sem, 16)
        tk = (ds, ds.cnt)
        for b in reads:
            b.r.append(tk)
        for b in writes:
            if part:
                b.w.append(tk)
            else:
                b.w = [tk]
                b.r = []

    def barrier(self):
        for eng in self.engs:
            deps = [(k, self.cnt[k]) for k in self.engs if k != eng and self.cnt[k] > 0]
            deps += [(d, d.cnt) for d in self.dsems if d.cnt > 0]
            self._wait(eng, deps)
        for b in ALLBUFS:
            b.w = []
            b.r = []

    def reset_dsems(self):
        self.dptr = 0
        self.swptr = 0


class Ring:
    def __init__(self, items):
        self.items = items
        self.i = 0

    def next(self):
        x = self.items[self.i % len(self.items)]
        self.i += 1
        return x


def build(NB=2, n_layers=DEPTH, dbg=False, stop_after=None):
    nc = bass.Bass("TRN2", target_bir_lowering=False)
    es = ExitStack()
    C = Ctx(nc, es)
    last_l = DEPTH - 1

    def dram(name, shape, dt, kind):
        return nc.dram_tensor(name, list(shape), dt, kind=kind).ap()

    xin = dram("xin", [NB, 128, KC, NT], F32, "ExternalInput")
    cT = dram("cT", [128, KC, 3], F32, "ExternalInput")
    w_ada = dram("w_ada", [DEPTH, D, 6 * D], F32, "ExternalInput")
    w_in = dram("w_in", [DEPTH, D, 8192], F32, "ExternalInput")
    w_ret_out = dram("w_ret_out", [DEPTH, 1024, D], F32, "ExternalInput")
    w_conv_out = dram("w_conv_out", [DEPTH, 512, D], F32, "ExternalInput")
    w_pool_out = dram("w_pool_out", [DEPTH, 512, D], F32, "ExternalInput")
    w_o = dram("w_o", [DEPTH, D, D], F32, "ExternalInput")
    pool_w = dram("pool_w", [DEPTH, 4, 128, 128], F32, "ExternalInput")
    w1 = dram("w1", [DEPTH, 16, D, 512], F32, "ExternalInput")
    w3 = dram("w3", [DEPTH, 16, D, 512], F32, "ExternalInput")
    w2 = dram("w2", [DEPTH, 16, 512, D], F32, "ExternalInput")
    smallp = dram("smallp", [128, SMALL_W], F32, "ExternalInput")
    wr = dram("wr", [128, DEPTH * KC * 20], F32, "ExternalInput")
    consts = dram("consts", [128, CONST_W], F32, "ExternalInput")
    ropet = dram("ropet", [128, 4 * 16 * 64], F32, "ExternalInput")
    outT = dram("outT", [NB, 128, KC, SEQ], F32, "ExternalOutput")
    skind = "ExternalOutput" if dbg else "Internal"
    XT = dram("XT", [NB, 128, KC, NT], F32, skind)
    YRT = dram("YRT", [128, KC, NT], BF16, skind)
    MG = dram("MG", [128, KC, NT], F32, skind)
    SBS = dram("SBS", [NCH, 128, 1024], BF16, "Internal")
    b_XT = [[Buf() for _ in TILES] for _ in range(NB)]
    b_YRT = [Buf() for _ in range(NCH)]
    b_MG = [Buf() for _ in TILES]
    b_SBS = [Buf() for _ in range(NCH)]
    b_out = Buf()
    b_in = None

    g = es
    hT = C.sb(g, [128, KC, NT], BF16, "hT")
    CS = C.sb(g, [128, CONST_W], F32, "CS")
    SP_ = C.sb(g, [128, SMALL_W], F32, "SP")
    IDb = C.sb(g, [128, 128], BF16, "IDb")
    ONESb = C.sb(g, [128, 128], BF16, "ONESb")
    MOD = C.sb(g, [128, 48, 3], F32, "MOD")
    GS1 = C.sb(g, [128, KC, 3], F32, "GS1")
    GS2 = C.sb(g, [128, KC, 3], F32, "GS2")
    LG = C.sb(g, [128, 8], F32, "LG")
    GCt = C.sb(g, [128, 8], F32, "GC")
    DBt = C.sb(g, [128, 4], F32, "DB")
    DFt = C.sb(g, [128, 4], F32, "DF")
    DM = C.sb(g, [128, 4, 128], F32, "DM")
    DQF = C.sb(g, [128, 4, 128], F32, "DQF")
    DQB = C.sb(g, [128, 4, 128], F32, "DQB")
    SIC = C.sb(g, [128, KC, 3], F32, "SIC")
    banks = [T(es.enter_context(nc.psum_tensor(f"pb{i}", [128, 512], F32))) for i in range(6)]
    PB = Ring(banks)
    PT = Ring([T(es.enter_context(nc.psum_tensor(f"pt{i}", [128, 1024], BF16))) for i in range(2)])
    ds_misc = C.dsem(perm=True)
    ds_sp = C.dsem(perm=True)

    def cs(name):
        o, n = CONST_OFF[name]
        return CS[:, o:o + n]

    IDf = cs("ident")
    ONES = cs("ones")

    def sp(name, l=None):
        o, n = SMALL_OFF[name]
        if l is None:
            return SP_[:, o:o + n]
        per = n // DEPTH
        return SP_[:, o + l * per:o + (l + 1) * per]

    C.dma("sp", CS[:], consts[:, :], ds_misc, r=[b_in], w=[CS])
    C.dma("sp", SP_[:], smallp[:, :], ds_sp, r=[b_in], w=[SP_])
    C.op("dve", lambda e: e.tensor_copy(out=IDb[:], in_=IDf), r=[CS], w=[IDb])
    C.op("dve", lambda e: e.tensor_copy(out=ONESb[:], in_=ONES), r=[CS], w=[ONESb])
    ds_c = C.dsem(perm=True)
    C.dma("sp", SIC[:], cT[:, :, :], ds_c, r=[b_in], w=[SIC])
    C.op("act", lambda e: e.activation(out=SIC[:], in_=SIC[:], func=AF.Silu), r=[SIC], w=[SIC])
    SICb = C.sb(g, [128, KC, 3], BF16, "SICb")
    C.op("dve", lambda e: e.tensor_copy(out=SICb[:], in_=SIC[:]), r=[SIC], w=[SICb])

    def mm_group(out_ap, pairs, bank, reads, fp32=False):
        n = len(pairs)
        for i, (l_, r_) in enumerate(pairs):
            C.op("pe", lambda e, l_=l_, r_=r_, i=i: e.matmul(out_ap, lhsT=l_, rhs=r_, start=(i == 0), stop=(i == n - 1)),
                 r=reads, w=[bank], inc=(i == n - 1), part=False)

    def load_w(es_, src2d, ncols, nk, ds, name):
        wt = C.sb(es_, [128, nk, ncols], BF16, name)
        ds = C.dsem(sw=True)
        v = src2d.rearrange("(kc p) n -> p kc n", p=128)
        for kc in range(nk):
            C.dma("pool", wt[:, kc, :], v[:, kc, :], ds, r=[b_in], w=[wt], part=(kc > 0))
        return wt

    def layer_setup(l):
        with ExitStack() as s:
            wa = [C.sb(s, [128, KC, 512], BF16, "wa") for _ in range(2)]
            dsw = [C.dsem(sw=True) for _ in range(2)]
            wav = w_ada[l].rearrange("(kc p) n -> p kc n", p=128)
            bada = sp("b_ada", l)
            for blk in range(12):
                wt = wa[blk % 2]
                C.dma("pool", wt[:], wav[:, :, blk * 512:(blk + 1) * 512], dsw[blk % 2], r=[b_in], w=[wt])
                for jj in range(4):
                    j = blk * 4 + jj
                    bank = PB.next()
                    mm_group(bank[:, 0:3], [(wt[:, kc, jj * 128:(jj + 1) * 128], SICb[:, kc, :]) for kc in range(KC)],
                             bank, [wt, SICb])
                    C.op("dve", lambda e, j=j, bank=bank: e.tensor_scalar(
                        out=MOD[:, j, :], in0=bank[:, 0:3], scalar1=bada[:, j:j + 1], scalar2=None, op0=ALU.add),
                        r=[bank, SP_], w=[MOD], part=True)
            for (GS, nm, jo) in ((GS1, "norm1", 8), (GS2, "norm2", 32)):
                ng = sp(nm, l)
                for kc in range(KC):
                    C.op("dve", lambda e, GS=GS, kc=kc, jo=jo, ng=ng: e.tensor_scalar(
                        out=GS[:, kc, :], in0=MOD[:, jo + kc, :], scalar1=1.0, scalar2=ng[:, kc:kc + 1],
                        op0=ALU.add, op1=ALU.mult), r=[MOD, SP_], w=[GS], part=True)
            rd = sp("ret_decay", l)
            C.op("act", lambda e: e.activation(out=LG[:], in_=rd, func=AF.Sigmoid), r=[SP_], w=[LG])
            C.op("act", lambda e: e.activation(out=LG[:], in_=LG[:], func=AF.Ln), r=[LG], w=[LG])
            C.op("act", lambda e: e.activation(out=GCt[:], in_=LG[:], func=AF.Exp, scale=128.0), r=[LG], w=[GCt])
            jc = cs("jcol")
            C.op("act", lambda e: e.activation(out=DBt[:], in_=LG[:, 4:8], func=AF.Exp, scale=jc[:, 0:1]), r=[LG, CS], w=[DBt])
            C.op("act", lambda e: e.activation(out=DFt[:], in_=LG[:, 0:4], func=AF.Exp, scale=jc[:, 1:2]), r=[LG, CS], w=[DFt])
            tmp = C.sb(s, [128, 128], F32, "dtmp")
            for h in range(4):
                C.op("act", lambda e, h=h: e.activation(out=DQF[:, h, :], in_=cs("iota1"), func=AF.Exp, scale=LG[:, h:h + 1]),
                     r=[LG, CS], w=[DQF], part=True)
                C.op("act", lambda e, h=h: e.activation(out=DQB[:, h, :], in_=cs("iotac"), func=AF.Exp, scale=LG[:, 4 + h:5 + h]),
                     r=[LG, CS], w=[DQB], part=True)
                C.op("act", lambda e, h=h: e.activation(out=DM[:, h, :], in_=cs("relp"), func=AF.Exp, scale=LG[:, h:h + 1]),
                     r=[LG, CS], w=[DM], part=True)
                C.op("dve", lambda e, h=h: e.tensor_tensor(out=DM[:, h, :], in0=DM[:, h, :], in1=cs("mf"), op=ALU.mult),
                     r=[DM, CS], w=[DM])
                C.op("act", lambda e, h=h: e.activation(out=tmp[:], in_=cs("reln"), func=AF.Exp, scale=LG[:, 4 + h:5 + h]),
                     r=[LG, CS], w=[tmp])
                C.op("dve", lambda e: e.tensor_tensor(out=tmp[:], in0=tmp[:], in1=cs("mb"), op=ALU.mult), r=[tmp, CS], w=[tmp])
                C.op("dve", lambda e, h=h: e.tensor_tensor(out=DM[:, h, :], in0=DM[:, h, :], in1=tmp[:], op=ALU.add),
                     r=[DM, tmp], w=[DM])
            C.barrier()

    def norm_tile(s, l, b, ti, src_ap, src_buf, GS, sh_j, bufs, hf=None, out_fn=None):
        off, n = TILES[ti]
        r = 2 if ti == 0 else b
        sq, rstd, tmp = bufs
        sbl = src_buf if isinstance(src_buf, list) else [src_buf] * KC
        C.op("act", lambda e: e.activation(out=sq[:, :, :n], in_=src_ap, func=AF.Square), r=sbl, w=[sq])
        bank = PB.next()
        mm_group(bank[:, :n], [(ONESb[:], sq[:, kc, :n]) for kc in range(KC)], bank, [sq, ONESb])
        C.op("act", lambda e: e.activation(out=rstd[:, :n], in_=bank[:, :n], func=AF.Sqrt, scale=1.0 / D, bias=cs("eps")[:, 0:1]),
             r=[bank, CS], w=[rstd])
        C.op("dve", lambda e: e.reciprocal(out=rstd[:, :n], in_=rstd[:, :n]), r=[rstd], w=[rstd])
        for kc in range(KC):
            tb = tmp.next()
            C.op("dve", lambda e, kc=kc, tb=tb: e.scalar_tensor_tensor(
                out=tb[:, :n], in0=src_ap[:, kc, :], scalar=GS[:, kc, r:r + 1], in1=rstd[:, :n], op0=ALU.mult, op1=ALU.mult),
                r=[sbl[kc], GS, rstd], w=[tb])
            if out_fn is not None:
                out_fn(kc, tb, n)
            elif hf is None:
                C.op("act", lambda e, kc=kc, tb=tb: e.activation(
                    out=hT[:, kc, off:off + n], in_=tb[:, :n], func=AF.Identity, bias=MOD[:, sh_j + kc, r:r + 1]),
                    r=[tb, MOD], w=[hT], part=True)
            else:
                C.op("act", lambda e, kc=kc, tb=tb: e.activation(
                    out=hf[:, kc, :n], in_=tb[:, :n], func=AF.Identity, bias=MOD[:, sh_j + kc, r:r + 1]),
                    r=[tb, MOD], w=[hf], part=True)
                C.op("dve", lambda e, kc=kc: e.tensor_copy(out=hT[:, kc, off:off + n], in_=hf[:, kc, :n]),
                     r=[hf], w=[hT], part=True)

    def stage_norm1(l, b, tiles):
        with ExitStack() as s:
            xt = [C.sb(s, [128, KC, 512], F32, "xt") for _ in range(2)]
            dsx = [C.dsem() for _ in range(2)]
            sq = C.sb(s, [128, KC, 512], BF16, "sq")
            rstd = C.sb(s, [128, 512], F32, "rstd")
            tmp = Ring([C.sb(s, [128, 512], F32, "ntmp") for _ in range(2)])
            src = xin if l == 0 else XT
            for i, ti in enumerate(tiles):
                off, n = TILES[ti]
                x_ = xt[i % 2]
                C.dma("sp", x_[:, :, :n], src[b, :, :, off:off + n], dsx[i % 2],
                      r=[b_in if l == 0 else b_XT[b][ti]], w=[x_])
                norm_tile(s, l, b, ti, x_[:, :, :n], x_, GS1, 0, (sq, rstd, tmp))
            C.barrier()

    def rope_evac(src_bank, dst, tabs, ci, rt, rt2):
        cos_t, sin_t, tab_buf = tabs
        n = ci - 2
        sv = src_bank[:, :].rearrange("p (h t d) -> p h t d", h=4, t=2)
        dv = dst[:, :, :].rearrange("p h (t d) -> p h t d", t=2)
        t1, t2 = rt
        c_ = cos_t[:, n, :].unsqueeze(1).to_broadcast([128, 4, 64])
        s_ = sin_t[:, n, :].unsqueeze(1).to_broadcast([128, 4, 64])
        C.op("dve", lambda e: e.tensor_tensor(out=t1[:], in0=sv[:, :, 0, :], in1=c_, op=ALU.mult), r=[src_bank, tab_buf], w=[t1])
        C.op("dve", lambda e: e.tensor_tensor(out=t2[:], in0=sv[:, :, 1, :], in1=s_, op=ALU.mult), r=[src_bank, tab_buf], w=[t2])
        C.op("dve", lambda e: e.tensor_tensor(out=dv[:, :, 0, :], in0=t1[:], in1=t2[:], op=ALU.subtract), r=[t1, t2], w=[dst], part=True)
        t3, t4 = rt2
        C.op("dve", lambda e: e.tensor_tensor(out=t3[:], in0=sv[:, :, 0, :], in1=s_, op=ALU.mult), r=[src_bank, tab_buf], w=[t3])
        C.op("dve", lambda e: e.tensor_tensor(out=t4[:], in0=sv[:, :, 1, :], in1=c_, op=ALU.mult), r=[src_bank, tab_buf], w=[t4])
        C.op("dve", lambda e: e.tensor_tensor(out=dv[:, :, 1, :], in0=t3[:], in1=t4[:], op=ALU.add), r=[t3, t4], w=[dst], part=True)

    def stage_ret(l, b):
        last = (l == last_l)
        with ExitStack() as s:
            dsw = C.dsem(sw=True)
            WK = load_w(s, w_in[l, :, O_K:O_K + 512], 512, KC, dsw, "WK")
            WV = load_w(s, w_in[l, :, O_V:O_V + 1024], 1024, KC, dsw, "WV")
            WQ = load_w(s, w_in[l, :, O_Q:O_Q + 512], 512, KC, dsw, "WQ")
            WG = load_w(s, w_in[l, :, O_G:O_G + 1024], 1024, KC, dsw, "WG")
            RT = C.sb(s, [128, 4 * 16 * 64], F32, "RT")
            C.dma("sp", RT[:], ropet[:, :], C.dsem(), r=[b_in], w=[RT])
            rtv = RT[:, :].rearrange("p (k n d) -> p k n d", k=4, n=16)
            tabq = (rtv[:, 0], rtv[:, 1], RT)
            tabk = (rtv[:, 2], rtv[:, 3], RT)
            rt = (C.sb(s, [128, 4, 64], F32, "rt1"), C.sb(s, [128, 4, 64], F32, "rt2"))
            rt2 = (C.sb(s, [128, 4, 64], F32, "rt3"), C.sb(s, [128, 4, 64], F32, "rt4"))
            Sb = C.sb(s, [128, 4, 256], F32, "Sb")
            Sf = C.sb(s, [128, 4, 256], F32, "Sf")
            qr = Ring([C.sb(s, [128, 4, 128], BF16, "qr") for _ in range(2)])
            kr = Ring([C.sb(s, [128, 4, 128], BF16, "kr") for _ in range(2)])
            vb = Ring([C.sb(s, [128, 4, 256], BF16, "vb") for _ in range(2)])
            vd = Ring([C.sb(s, [128, 4, 256], BF16, "vd") for _ in range(2)])
            sg = Ring([C.sb(s, [128, 4, 256], F32, "sg") for _ in range(2)])
            sbo = [C.sb(s, [128, 1024], BF16, "sbo") for _ in range(2)]
            ds_sbo = [C.dsem() for _ in range(2)]
            sbi = [C.sb(s, [128, 4, 256], BF16, "sbi") for _ in range(2)]
            ds_sbi = [C.dsem() for _ in range(2)]
            sfb = Ring([C.sb(s, [128, 4, 256], BF16, "sfb") for _ in range(2)])
            qT = Ring([C.sb(s, [128, 4, 128], BF16, "qT") for _ in range(2)])
            qfT = Ring([C.sb(s, [128, 4, 128], BF16, "qfT") for _ in range(2)])
            qbT = Ring([C.sb(s, [128, 4, 128], BF16, "qbT") for _ in range(2)])
            kT = Ring([C.sb(s, [128, 4, 128], BF16, "kT") for _ in range(2)])
            sT = Ring([C.sb(s, [128, 4, 128], BF16, "sT") for _ in range(2)])
            yn = Ring([C.sb(s, [128, 256], F32, "yn") for _ in range(2)])
            yr = Ring([C.sb(s, [128, 1024], BF16, "yr") for _ in range(2)])
            yT = [C.sb(s, [128, KC, 128], BF16, "yT") for _ in range(2)]
            ds_yT = [C.dsem() for _ in range(2)]
            st6 = C.sb(s, [128, 4, 6], F32, "st6")
            mv = C.sb(s, [128, 4, 2], F32, "mv")
            rs = C.sb(s, [128, 4], F32, "rs")

            def proj(ci, W, c0, ncols):
                bank = PB.next()
                mm_group(bank[:, :ncols], [(hT[:, kc, ci * 128:(ci + 1) * 128], W[:, kc, c0:c0 + ncols]) for kc in range(KC)],
                         bank, [hT, W])
                return bank

            def k_evac(ci, kps):
                k_ = kr.next()
                if ci >= 2:
                    rope_evac(kps, k_, tabk, ci, rt, rt2)
                else:
                    C.op("act", lambda e: e.activation(out=k_[:, :, :].rearrange("p h d -> p (h d)"), in_=kps[:, :],
                                                       func=AF.Identity, scale=128.0 ** -0.5), r=[kps], w=[k_])
                return k_

            def v_scaled(vps, dec, dst):
                for h in range(4):
                    bk = vps[h // 2]
                    C.op("act", lambda e, h=h, bk=bk: e.activation(
                        out=dst[:, h, :], in_=bk[:, (h % 2) * 256:(h % 2) * 256 + 256], func=AF.Identity, scale=dec[:, h:h + 1]),
                        r=[bk, dec], w=[dst], part=True)

            Sbh = [Buf() for _ in range(4)]
            Sfh = [Buf() for _ in range(4)]

            def state_update(S, k_, vdd, gcol0):
                SH = Sbh if S is Sb else Sfh
                kv = [PB.next(), PB.next()]
                for h in range(4):
                    bk = kv[h // 2]
                    C.op("pe", lambda e, h=h, bk=bk: e.matmul(bk[:, (h % 2) * 256:(h % 2) * 256 + 256], lhsT=k_[:, h, :],
                                                             rhs=vdd[:, h, :], start=True, stop=True),
                         r=[k_, vdd], w=[bk], part=(h % 2 == 1))
                for h in range(4):
                    bk = kv[h // 2]
                    C.op("dve", lambda e, h=h, bk=bk: e.scalar_tensor_tensor(
                        out=S[:, h, :], in0=S[:, h, :], scalar=GCt[:, gcol0 + h:gcol0 + h + 1],
                        in1=bk[:, (h % 2) * 256:(h % 2) * 256 + 256], op0=ALU.mult, op1=ALU.add),
                        r=[SH[h], GCt, bk], w=[SH[h]])

            C.op("dve", lambda e: e.memset(Sb[:], 0.0), w=Sbh)
            C.op("dve", lambda e: e.memset(Sf[:], 0.0), w=Sfh)
            order = [1, 0] + list(range(NCH - 1, 1, -1))
            fl = lambda t_: t_[:, :, :].rearrange("p h d -> p (h d)")

            def front1(ci):
                kps = proj(ci, WK, 0, 512)
                vps = [proj(ci, WV, 0, 512), proj(ci, WV, 512, 512)]
                k_ = k_evac(ci, kps)
                vd_ = vd.next()
                v_scaled(vps, DBt, vd_)
                return k_, vd_

            def back1(i, ci, k_, vd_):
                so = sbo[i % 2]
                C.op("act", lambda e: e.copy(out=so[:], in_=fl(Sb)), r=Sbh, w=[so])
                C.dma("sp", SBS[ci, :, :], so[:], ds_sbo[i % 2], r=[so], w=[b_SBS[ci]])
                state_update(Sb, k_, vd_, 4)

            cur = front1(order[0])
            for i, ci in enumerate(order):
                nxt = front1(order[i + 1]) if i + 1 < len(order) else None
                back1(i, ci, *cur)
                cur = nxt

            def front2(ci):
                only_state = last and ci < 2
                kps = proj(ci, WK, 0, 512)
                vps = [proj(ci, WV, 0, 512), proj(ci, WV, 512, 512)]
                k_ = k_evac(ci, kps)
                vdf = vd.next()
                v_scaled(vps, DFt, vdf)
                H = dict(k_=k_, vdf=vdf, only_state=only_state)
                if only_state:
                    return H
                si = sbi[ci % 2]
                C.dma("sp", fl(si), SBS[ci, :, :], ds_sbi[ci % 2], r=[b_SBS[ci]], w=[si])
                qps = proj(ci, WQ, 0, 512)
                gps = [proj(ci, WG, 0, 512), proj(ci, WG, 512, 512)]
                q_ = qr.next()
                if ci >= 2:
                    rope_evac(qps, q_, tabq, ci, rt, rt2)
                else:
                    C.op("act", lambda e: e.copy(out=fl(q_), in_=qps[:, :]), r=[qps], w=[q_])
                vb_ = vb.next()
                sg_ = sg.next()
                for j in range(2):
                    C.op("act", lambda e, j=j: e.copy(out=vb_[:, 2 * j:2 * j + 2, :].rearrange("p h d -> p (h d)"), in_=vps[j][:, :]),
                         r=[vps[j]], w=[vb_], part=(j == 1))
                    C.op("act", lambda e, j=j: e.activation(out=sg_[:, 2 * j:2 * j + 2, :].rearrange("p h d -> p (h d)"),
                                                           in_=gps[j][:, :], func=AF.Silu), r=[gps[j]], w=[sg_], part=(j == 1))
                tb = PT.next()
                tbv = tb[:, :]
                for h in range(4):
                    C.op("pe", lambda e, h=h: e.transpose(out=tbv[:, h * 128:(h + 1) * 128], in_=q_[:, h, :], identity=IDb[:]),
                         r=[q_, IDb], w=[tb], inc=False, part=(h > 0))
                for h in range(4):
                    C.op("pe", lambda e, h=h: e.transpose(out=tbv[:, 512 + h * 128:512 + (h + 1) * 128], in_=k_[:, h, :], identity=IDb[:]),
                         r=[k_, IDb], w=[tb], inc=(h == 3), part=True)
                qT_, qfT_, qbT_, kT_ = qT.next(), qfT.next(), qbT.next(), kT.next()
                C.op("act", lambda e: e.copy(out=fl(qT_), in_=tbv[:, 0:512]), r=[tb], w=[qT_])
                C.op("act", lambda e: e.copy(out=fl(kT_), in_=tbv[:, 512:1024]), r=[tb], w=[kT_])
                C.op("dve", lambda e: e.tensor_tensor(out=fl(qfT_), in0=fl(qT_), in1=fl(DQF), op=ALU.mult), r=[qT_, DQF], w=[qfT_])
                C.op("dve", lambda e: e.tensor_tensor(out=fl(qbT_), in0=fl(qT_), in1=fl(DQB), op=ALU.mult), r=[qT_, DQB], w=[qbT_])
                scb = PB.next()
                for h in range(4):
                    C.op("pe", lambda e, h=h: e.matmul(scb[:, h * 128:(h + 1) * 128], lhsT=kT_[:, h, :], rhs=qT_[:, h, :],
                                                       start=True, stop=True), r=[kT_, qT_], w=[scb], inc=(h == 3), part=(h > 0))
                sT_ = sT.next()
                C.op("dve", lambda e: e.tensor_tensor(out=fl(sT_), in0=scb[:, :], in1=fl(DM), op=ALU.mult), r=[scb, DM], w=[sT_])
                H.update(si=si, vb_=vb_, sg_=sg_, qfT_=qfT_, qbT_=qbT_, sT_=sT_)
                return H

            def back2(ci, H, sf_cur):
                k_, vdf = H["k_"], H["vdf"]
                if not H["only_state"]:
                    si, vb_, sg_, qfT_, qbT_, sT_ = (H[k] for k in ("si", "vb_", "sg_", "qfT_", "qbT_", "sT_"))
                    ob = [PB.next(), PB.next()]
                    for h in range(4):
                        bk = ob[h // 2]
                        oap = bk[:, (h % 2) * 256:(h % 2) * 256 + 256]
                        C.op("pe", lambda e, h=h, oap=oap: e.matmul(oap, lhsT=sT_[:, h, :], rhs=vb_[:, h, :], start=True, stop=False),
                             r=[sT_, vb_], w=[bk], inc=False, part=(h % 2 == 1))
                        C.op("pe", lambda e, h=h, oap=oap: e.matmul(oap, lhsT=qfT_[:, h, :], rhs=sf_cur[:, h, :], start=False, stop=False),
                             r=[qfT_, sf_cur], w=[bk], inc=False, part=True)
                        C.op("pe", lambda e, h=h, oap=oap: e.matmul(oap, lhsT=qbT_[:, h, :], rhs=si[:, h, :], start=False, stop=True),
                             r=[qbT_, si], w=[bk], inc=True, part=True)
                    for h in range(4):
                        bk = ob[h // 2]
                        C.op("dve", lambda e, h=h, bk=bk: e.bn_stats(out=st6[:, h, :], in_=bk[:, (h % 2) * 256:(h % 2) * 256 + 256]),
                             r=[bk], w=[st6], part=(h > 0))
                    for h in range(4):
                        C.op("dve", lambda e, h=h: e.bn_aggr(out=mv[:, h, :], in_=st6[:, h, :]), r=[st6], w=[mv], part=(h > 0))
                    C.op("act", lambda e: e.activation(out=rs[:], in_=mv[:, :, 1], func=AF.Sqrt, bias=cs("eps")[:, 0:1]), r=[mv, CS], w=[rs])
                    C.op("dve", lambda e: e.reciprocal(out=rs[:], in_=rs[:]), r=[rs], w=[rs])
                    yr_ = yr.next()
                    for h in range(4):
                        bk = ob[h // 2]
                        yn_ = yn.next()
                        C.op("dve", lambda e, h=h, bk=bk, yn_=yn_: e.tensor_scalar(
                            out=yn_[:], in0=bk[:, (h % 2) * 256:(h % 2) * 256 + 256], scalar1=mv[:, h, 0:1], scalar2=rs[:, h:h + 1],
                            op0=ALU.subtract, op1=ALU.mult), r=[bk, mv, rs], w=[yn_])
                        C.op("dve", lambda e, h=h, yn_=yn_: e.tensor_tensor(out=yr_[:, h * 256:(h + 1) * 256], in0=yn_[:], in1=sg_[:, h, :],
                                                                            op=ALU.mult), r=[yn_, sg_], w=[yr_], part=(h > 0))
                    tb2 = PT.next()
                    tb2v = tb2[:, :]
                    for cc in range(KC):
                        C.op("pe", lambda e, cc=cc: e.transpose(out=tb2v[:, cc * 128:(cc + 1) * 128], in_=yr_[:, cc * 128:(cc + 1) * 128],
                                                                identity=IDb[:]), r=[yr_, IDb], w=[tb2], inc=(cc == KC - 1), part=(cc > 0))
                    yT_ = yT[ci % 2]
                    C.op("act", lambda e: e.copy(out=yT_[:, :, :].rearrange("p c t -> p (c t)"), in_=tb2v[:, :]), r=[tb2], w=[yT_])
                    C.dma("sp", YRT[:, :, ci * 128:(ci + 1) * 128], yT_[:], ds_yT[ci % 2], r=[yT_], w=[b_YRT[ci]])
                state_update(Sf, k_, vdf, 0)
                sf_new = sfb.next()
                C.op("act", lambda e: e.copy(out=sf_new[:], in_=Sf[:]), r=Sfh, w=[sf_new])
                return sf_new

            sf_cur = sfb.next()
            C.op("dve", lambda e: e.memset(sf_cur[:], 0.0), w=[sf_cur])
            cur = front2(0)
            for ci in range(NCH):
                nxt = front2(ci + 1) if ci + 1 < NCH else None
                sf_cur = back2(ci, cur, sf_cur)
                cur = nxt
            C.barrier()

    def chunks_of(ti):
        off, n = TILES[ti]
        return list(range(off // 128, (off + n) // 128))

    def stage_m1(l, b, tiles):
        with ExitStack() as s:
            dsw = C.dsem(sw=True)
            WRO = load_w(s, w_ret_out[l, :, :], 1024, KC, dsw, "WRO")
            WGR = load_w(s, w_in[l, :, O_GT:O_GT + 1024], 1024, KC, dsw, "WGR")
            yt = [C.sb(s, [128, KC, 512], BF16, "yt") for _ in range(2)]
            ds_yt = [C.dsem() for _ in range(2)]
            mg = [C.sb(s, [128, KC, 512], F32, "mg") for _ in range(2)]
            ds_mg = [C.dsem() for _ in range(2)]
            gs = Ring([C.sb(s, [128, 512], F32, "gs") for _ in range(2)])
            for i, ti in enumerate(tiles):
                off, n = TILES[ti]
                y_ = yt[i % 2]
                m_ = mg[i % 2]
                C.dma("sp", y_[:, :, :n], YRT[:, :, off:off + n], ds_yt[i % 2], r=[b_YRT[c] for c in chunks_of(ti)], w=[y_])
                for cc in range(KC):
                    rb = PB.next()
                    mm_group(rb[:, :n], [(WRO[:, kc, cc * 128:(cc + 1) * 128], y_[:, kc, :n]) for kc in range(KC)], rb, [WRO, y_])
                    gb = PB.next()
                    mm_group(gb[:, :n], [(WGR[:, kc, cc * 128:(cc + 1) * 128], hT[:, kc, off:off + n]) for kc in range(KC)], gb, [WGR, hT])
                    g_ = gs.next()
                    C.op("act", lambda e: e.activation(out=g_[:, :n], in_=gb[:, :n], func=AF.Sigmoid), r=[gb], w=[g_])
                    C.op("dve", lambda e, cc=cc: e.tensor_tensor(out=m_[:, cc, :n], in0=rb[:, :n], in1=g_[:, :n], op=ALU.mult),
                         r=[rb, g_], w=[m_], part=(cc > 0))
                C.dma("sp", MG[:, :, off:off + n], m_[:, :, :n], ds_mg[i % 2], r=[m_], w=[b_MG[ti]])
            C.barrier()

    def stage_cp(l, b, tiles, U, P):
        with ExitStack() as s:
            dsw = C.dsem(sw=True)
            WC = load_w(s, w_in[l, :, O_CC:O_CC + 512], 512, KC, dsw, "WC")
            WX = load_w(s, w_in[l, :, O_CX:O_CX + 512], 512, KC, dsw, "WX")
            WP = load_w(s, w_in[l, :, O_PI:O_PI + 512], 512, KC, dsw, "WP")
            csb = Ring([C.sb(s, [128, 512], F32, "csb") for _ in range(2)])
            C.op("dve", lambda e: e.memset(U[:], 0.0), w=[U])
            C.op("dve", lambda e: e.memset(P[:], 0.0), w=[P])
            for ti in tiles:
                off, n = TILES[ti]
                uc = ucol(off)
                for ch in range(4):
                    cb = PB.next()
                    mm_group(cb[:, :n], [(WC[:, kc, ch * 128:(ch + 1) * 128], hT[:, kc, off:off + n]) for kc in range(KC)], cb, [WC, hT])
                    xb = PB.next()
                    mm_group(xb[:, :n], [(WX[:, kc, ch * 128:(ch + 1) * 128], hT[:, kc, off:off + n]) for kc in range(KC)], xb, [WX, hT])
                    pb = PB.next()
                    mm_group(pb[:, :n], [(WP[:, kc, ch * 128:(ch + 1) * 128], hT[:, kc, off:off + n]) for kc in range(KC)], pb, [WP, hT])
                    c_ = csb.next()
                    C.op("act", lambda e: e.copy(out=c_[:, :n], in_=cb[:, :n]), r=[cb], w=[c_])
                    C.op("dve", lambda e, ch=ch: e.tensor_tensor(out=U[:, ch, uc:uc + n], in0=xb[:, :n], in1=c_[:, :n], op=ALU.mult),
                         r=[xb, c_], w=[U], part=True)
                    C.op("act", lambda e, ch=ch: e.copy(out=P[:, ch, uc:uc + n], in_=pb[:, :n]), r=[pb], w=[P], part=True)
            C.barrier()

    def stage_m2(l, b, tiles, U):
        with ExitStack() as s:
            dsw = C.dsem(sw=True)
            WB = load_w(s, w_in[l, :, O_CB:O_CB + 512], 512, KC, dsw, "WB")
            WGC = load_w(s, w_in[l, :, O_GT + 1024:O_GT + 2048], 1024, KC, dsw, "WGC")
            WCO = load_w(s, w_conv_out[l, :, :], 1024, 4, dsw, "WCO")
            cw = sp("conv_w", l)
            mg = [C.sb(s, [128, KC, 512], F32, "mg2") for _ in range(2)]
            mgc = [[Buf() for _ in range(KC)] for _ in range(2)]
            ds_mg = [C.dsem() for _ in range(2)]
            ds_mgo = [C.dsem() for _ in range(2)]
            cv = Ring([C.sb(s, [128, 512], F32, "cv") for _ in range(2)])
            yc = Ring([C.sb(s, [128, 4, 512], BF16, "yc") for _ in range(2)])
            gs = Ring([C.sb(s, [128, 512], F32, "gs2") for _ in range(2)])
            tt = Ring([C.sb(s, [128, 512], F32, "tt2") for _ in range(2)])
            for i, ti in enumerate(tiles):
                off, n = TILES[ti]
                uc = ucol(off)
                m_ = mg[i % 2]
                mc_ = mgc[i % 2]
                C.dma("sp", m_[:, :, :n], MG[:, :, off:off + n], ds_mg[i % 2], r=[b_MG[ti]], w=mc_)
                yc_ = yc.next()
                for ch in range(4):
                    bb = PB.next()
                    mm_group(bb[:, :n], [(WB[:, kc, ch * 128:(ch + 1) * 128], hT[:, kc, off:off + n]) for kc in range(KC)], bb, [WB, hT])
                    cv_ = cv.next()
                    C.op("dve", lambda e, ch=ch: e.tensor_scalar(out=cv_[:, :n], in0=U[:, ch, uc - 1:uc - 1 + n], scalar1=cw[:, ch:ch + 1],
                                                                 scalar2=None, op0=ALU.mult), r=[U, SP_], w=[cv_])
                    for k in (1, 2):
                        C.op("dve", lambda e, ch=ch, k=k: e.scalar_tensor_tensor(
                            out=cv_[:, :n], in0=U[:, ch, uc - 1 + k:uc - 1 + k + n], scalar=cw[:, k * 4 + ch:k * 4 + ch + 1],
                            in1=cv_[:, :n], op0=ALU.mult, op1=ALU.add), r=[U, SP_, cv_], w=[cv_])
                    C.op("dve", lambda e, ch=ch: e.tensor_tensor(out=yc_[:, ch, :n], in0=bb[:, :n], in1=cv_[:, :n], op=ALU.mult),
                         r=[bb, cv_], w=[yc_], part=(ch > 0))
                for cc in range(KC):
                    cb = PB.next()
                    mm_group(cb[:, :n], [(WCO[:, ch, cc * 128:(cc + 1) * 128], yc_[:, ch, :n]) for ch in range(4)], cb, [WCO, yc_])
                    gb = PB.next()
                    mm_group(gb[:, :n], [(WGC[:, kc, cc * 128:(cc + 1) * 128], hT[:, kc, off:off + n]) for kc in range(KC)], gb, [WGC, hT])
                    g_ = gs.next()
                    C.op("act", lambda e: e.activation(out=g_[:, :n], in_=gb[:, :n], func=AF.Sigmoid), r=[gb], w=[g_])
                    t_ = tt.next()
                    C.op("dve", lambda e: e.tensor_tensor(out=t_[:, :n], in0=cb[:, :n], in1=g_[:, :n], op=ALU.mult), r=[cb, g_], w=[t_])
                    C.op("dve", lambda e, cc=cc: e.tensor_tensor(out=m_[:, cc, :n], in0=m_[:, cc, :n], in1=t_[:, :n], op=ALU.add),
                         r=[mc_[cc], t_], w=[mc_[cc]])
                C.dma("sp", MG[:, :, off:off + n], m_[:, :, :n], ds_mgo[i % 2], r=mc_, w=[b_MG[ti]])
            C.barrier()

    def stage_m3(l, b, tiles, P):
        with ExitStack() as s:
            dsw = C.dsem(sw=True)
            WGP = load_w(s, w_in[l, :, O_GT + 2048:O_GT + 3072], 1024, KC, dsw, "WGP")
            WPO = load_w(s, w_pool_out[l, :, :], 1024, 4, dsw, "WPO")
            WO = load_w(s, w_o[l, :, :], 1024, KC, dsw, "WO")
            PW = C.sb(s, [128, 4, 128], BF16, "PW")
            dspw = C.dsem(sw=True)
            for gI in range(4):
                C.dma("pool", PW[:, gI, :], pool_w[l, gI, :, :], dspw, r=[b_in], w=[PW], part=(gI > 0))
            et = C.sb(s, [128, 8], F32, "et")
            ive = cs("ive")
            psc = sp("pool_scale", l)
            mg = C.sb(s, [128, KC, 512], F32, "mg3")
            ds_mg = C.dsem()
            xt = C.sb(s, [128, KC, 512], F32, "xt3")
            xtc = [Buf() for _ in range(KC)]
            ds_xt = C.dsem()
            ds_xo = C.dsem()
            wa = C.sb(s, [128, 528], F32, "wa3")
            wb_ = C.sb(s, [128, 528], F32, "wb3")
            pl = Ring([C.sb(s, [128, 512], BF16, "pl") for _ in range(2)])
            yp = Ring([C.sb(s, [128, 4, 512], BF16, "yp") for _ in range(2)])
            gs = Ring([C.sb(s, [128, 512], F32, "gs3") for _ in range(2)])
            tt = Ring([C.sb(s, [128, 512], F32, "tt3") for _ in range(2)])
            mgb = C.sb(s, [128, KC, 512], BF16, "mgb")
            src = xin if l == 0 else XT
            for ti in tiles:
                off, n = TILES[ti]
                uc = ucol(off)
                r = 2 if ti == 0 else b
                C.dma("sp", mg[:, :, :n], MG[:, :, off:off + n], ds_mg, r=[b_MG[ti]], w=[mg])
                C.dma("sp", xt[:, :, :n], src[b, :, :, off:off + n], ds_xt, r=[b_in if l == 0 else b_XT[b][ti]], w=xtc)
                yp_ = yp.next()
                for gI, W in enumerate((2, 4, 8, 16)):
                    hw = W // 2
                    lo = uc - hw
                    ln = n + W - 2
                    C.op("dve", lambda e, gI=gI, lo=lo, ln=ln: e.tensor_tensor(out=wa[:, :ln], in0=P[:, gI, lo:lo + ln], in1=P[:, gI, lo + 1:lo + 1 + ln],
                                                                               op=ALU.add), r=[P], w=[wa])
                    cur, oth = wa, wb_
                    step = 2
                    while step < W:
                        ln2 = ln - step
                        C.op("dve", lambda e, cur=cur, oth=oth, ln2=ln2, step=step: e.tensor_tensor(
                            out=oth[:, :ln2], in0=cur[:, :ln2], in1=cur[:, step:step + ln2], op=ALU.add), r=[cur], w=[oth])
                        cur, oth = oth, cur
                        ln = ln2
                        step *= 2
                    assert ln == n
                    pl_ = pl.next()
                    C.op("dve", lambda e, cur=cur, gI=gI, W=W: e.scalar_tensor_tensor(
                        out=pl_[:, :n], in0=cur[:, :n], scalar=1.0 / W, in1=P[:, gI, uc:uc + n], op0=ALU.mult, op1=ALU.subtract),
                        r=[cur, P], w=[pl_])
                    edges = {0: [(0, 0), (n - 8, 1)], 1: [(0, 2)], 4: [(n - 8, 3)]}.get(ti, [])
                    for (e0, k) in edges:
                        io = (gI * 4 + k) * 8
                        C.op("dve", lambda e, cur=cur, e0=e0, io=io: e.tensor_tensor(out=et[:, 0:8], in0=cur[:, e0:e0 + 8], in1=ive[:, io:io + 8], op=ALU.mult),
                             r=[cur, CS], w=[et])
                        C.op("dve", lambda e, e0=e0, gI=gI: e.tensor_tensor(out=pl_[:, e0:e0 + 8], in0=et[:, 0:8], in1=P[:, gI, uc + e0:uc + e0 + 8], op=ALU.subtract),
                             r=[et, P], w=[pl_], part=True)
                    mb = PB.next()
                    mm_group(mb[:, :n], [(PW[:, gI, :], pl_[:, :n])], mb, [PW, pl_])
                    C.op("act", lambda e, gI=gI: e.activation(out=yp_[:, gI, :n], in_=mb[:, :n], func=AF.Identity, scale=psc[:, gI:gI + 1]),
                         r=[mb, SP_], w=[yp_], part=(gI > 0))
                for cc in range(KC):
                    pb = PB.next()
                    mm_group(pb[:, :n], [(WPO[:, ch, cc * 128:(cc + 1) * 128], yp_[:, ch, :n]) for ch in range(4)], pb, [WPO, yp_])
                    gb = PB.next()
                    mm_group(gb[:, :n], [(WGP[:, kc, cc * 128:(cc + 1) * 128], hT[:, kc, off:off + n]) for kc in range(KC)], gb, [WGP, hT])
                    g_ = gs.next()
                    C.op("act", lambda e: e.activation(out=g_[:, :n], in_=gb[:, :n], func=AF.Sigmoid), r=[gb], w=[g_])
                    t_ = tt.next()
                    C.op("dve", lambda e: e.tensor_tensor(out=t_[:, :n], in0=pb[:, :n], in1=g_[:, :n], op=ALU.mult), r=[pb, g_], w=[t_])
                    C.op("dve", lambda e, cc=cc: e.tensor_tensor(out=mgb[:, cc, :n], in0=mg[:, cc, :n], in1=t_[:, :n], op=ALU.add),
                         r=[mg, t_], w=[mgb], part=(cc > 0))
                for cc in range(KC):
                    yb = PB.next()
                    mm_group(yb[:, :n], [(WO[:, kc, cc * 128:(cc + 1) * 128], mgb[:, kc, :n]) for kc in range(KC)], yb, [WO, mgb])
                    C.op("dve", lambda e, cc=cc: e.scalar_tensor_tensor(
                        out=xt[:, cc, :n], in0=yb[:, :n], scalar=MOD[:, 16 + cc, r:r + 1], in1=xt[:, cc, :n], op0=ALU.mult, op1=ALU.add),
                        r=[yb, MOD, xtc[cc]], w=[xtc[cc]])
                C.dma("sp", XT[b, :, :, off:off + n], xt[:, :, :n], ds_xo, r=xtc, w=[b_XT[b][ti]])
            C.barrier()

    def stage_moe(l, b, tiles):
        last = (l == last_l)
        with ExitStack() as s:
            XS = C.sb(s, [128, KC, NT], F32, "XS")
            xsb = [[Buf() for _ in range(KC)] for _ in TILES]
            ds_xs = [C.dsem() for _ in TILES]
            COMBT = C.sb(s, [16, NT], F32, "COMBT")
            for ti in tiles:
                off, n = TILES[ti]
                C.dma("sp", XS[:, :, off:off + n], XT[b, :, :, off:off + n], ds_xs[ti], r=[b_XT[b][ti]], w=xsb[ti])
            wrl = C.sb(s, [128, KC, 20], F32, "wrl")
            C.dma("sp", wrl[:, :, :].rearrange("p k n -> p (k n)"), wr[:, l * KC * 20:(l + 1) * KC * 20], C.dsem(), r=[b_in], w=[wrl])
            brow = sp("b_r", l)
            with ExitStack() as s2:
                hf = C.sb(s2, [128, KC, 512], F32, "hf")
                sq = C.sb(s2, [128, KC, 512], BF16, "sq2")
                rstd = C.sb(s2, [128, 512], F32, "rstd2")
                tmp = Ring([C.sb(s2, [128, 512], F32, "ntmp2") for _ in range(2)])
                R = {k: C.sb(s2, shp, F32, "r_" + k) for k, shp in dict(
                    lg=[128, 4, 20], mx=[128, 4], eg=[128, 4, 4], se=[128, 4], gtop=[128, 4], ohg=[128, 4, 4], prod=[128, 4, 4, 4],
                    ing=[128, 4, 4], m1=[128, 4], oh1=[128, 4, 4], msk=[128, 4, 4], m2=[128, 4], oh2=[128, 4, 4], d21=[128, 4],
                    e2=[128, 4], den=[128, 4], w1=[128, 4], w2=[128, 4], loc=[128, 4, 4], tmp2=[128, 4, 4], comb=[128, 4, 16]).items()}
                V = lambda fn, r, w: C.op("dve", fn, r=r, w=w)
                A = lambda fn, r, w: C.op("act", fn, r=r, w=w)
                for ti in tiles:
                    off, n = TILES[ti]
                    norm_tile(s2, l, b, ti, XS[:, :, off:off + n], xsb[ti], GS2, 24, (sq, rstd, tmp), hf=hf)
                    c = n // 128
                    lb = PB.next()
                    for cj in range(c):
                        for kc in range(KC):
                            C.op("pe", lambda e, cj=cj, kc=kc: e.matmul(lb[:, cj * 20:(cj + 1) * 20], lhsT=hf[:, kc, cj * 128:(cj + 1) * 128],
                                                                      rhs=wrl[:, kc, :], start=(kc == 0), stop=(kc == KC - 1)),
                                 r=[hf, wrl], w=[lb], inc=(kc == KC - 1), part=(cj > 0 or kc > 0))
                    lg, mx, eg, se, gtop, ohg, prod, ing = (R[k] for k in ("lg", "mx", "eg", "se", "gtop", "ohg", "prod", "ing"))
                    m1, oh1, msk, m2, oh2, d21, e2, den, w1_, w2_, loc, tmp2, comb = (R[k] for k in (
                        "m1", "oh1", "msk", "m2", "oh2", "d21", "e2", "den", "w1", "w2", "loc", "tmp2", "comb"))
                    bc3 = lambda t_: t_[:, :c].unsqueeze(2).to_broadcast([128, c, 4])
                    V(lambda e: e.tensor_tensor(out=lg[:, :c, :], in0=lb[:, 0:c * 20].rearrange("p (c k) -> p c k", c=c),
                                                in1=brow.unsqueeze(1).to_broadcast([128, c, 20]), op=ALU.add), [lb, SP_], [lg])
                    lgg = lg[:, :c, 0:4]
                    V(lambda e: e.reduce_max(out=mx[:, :c], in_=lgg, axis=AX.X), [lg], [mx])
                    V(lambda e: e.tensor_tensor(out=eg[:, :c, :], in0=lgg, in1=bc3(mx), op=ALU.subtract), [lg, mx], [eg])
                    A(lambda e: e.activation(out=eg[:, :c, :], in_=eg[:, :c, :], func=AF.Exp), [eg], [eg])
                    V(lambda e: e.reduce_sum(out=se[:, :c], in_=eg[:, :c, :], axis=AX.X), [eg], [se])
                    V(lambda e: e.reciprocal(out=gtop[:, :c], in_=se[:, :c]), [se], [gtop])
                    V(lambda e: e.tensor_tensor(out=ohg[:, :c, :], in0=lgg, in1=bc3(mx), op=ALU.is_ge), [lg, mx], [ohg])
                    V(lambda e: e.tensor_tensor(out=prod[:, :c, :, :], in0=lg[:, :c, 4:20].rearrange("p c (g k) -> p c g k", g=4),
                                                in1=ohg[:, :c, :].unsqueeze(3).to_broadcast([128, c, 4, 4]), op=ALU.mult), [lg, ohg], [prod])
                    V(lambda e: e.tensor_tensor(out=ing[:, :c, :], in0=prod[:, :c, 0, :], in1=prod[:, :c, 1, :], op=ALU.add), [prod], [ing])
                    for gI in (2, 3):
                        V(lambda e, gI=gI: e.tensor_tensor(out=ing[:, :c, :], in0=ing[:, :c, :], in1=prod[:, :c, gI, :], op=ALU.add), [prod, ing], [ing])
                    V(lambda e: e.reduce_max(out=m1[:, :c], in_=ing[:, :c, :], axis=AX.X), [ing], [m1])
                    V(lambda e: e.tensor_tensor(out=oh1[:, :c, :], in0=ing[:, :c, :], in1=bc3(m1), op=ALU.is_ge), [ing, m1], [oh1])
                    V(lambda e: e.scalar_tensor_tensor(out=msk[:, :c, :], in0=oh1[:, :c, :], scalar=-1e30, in1=ing[:, :c, :], op0=ALU.mult, op1=ALU.add),
                      [oh1, ing], [msk])
                    V(lambda e: e.reduce_max(out=m2[:, :c], in_=msk[:, :c, :], axis=AX.X), [msk], [m2])
                    V(lambda e: e.tensor_tensor(out=oh2[:, :c, :], in0=msk[:, :c, :], in1=bc3(m2), op=ALU.is_ge), [msk, m2], [oh2])
                    V(lambda e: e.tensor_tensor(out=d21[:, :c], in0=m2[:, :c], in1=m1[:, :c], op=ALU.subtract), [m1, m2], [d21])
                    A(lambda e: e.activation(out=e2[:, :c], in_=d21[:, :c], func=AF.Exp), [d21], [e2])
                    V(lambda e: e.tensor_scalar(out=den[:, :c], in0=e2[:, :c], scalar1=1.0, scalar2=None, op0=ALU.add), [e2], [den])
                    V(lambda e: e.reciprocal(out=w1_[:, :c], in_=den[:, :c]), [den], [w1_])
                    V(lambda e: e.tensor_tensor(out=w1_[:, :c], in0=w1_[:, :c], in1=gtop[:, :c], op=ALU.mult), [w1_, gtop], [w1_])
                    V(lambda e: e.tensor_tensor(out=w2_[:, :c], in0=w1_[:, :c], in1=e2[:, :c], op=ALU.mult), [w1_, e2], [w2_])
                    V(lambda e: e.tensor_tensor(out=loc[:, :c, :], in0=oh1[:, :c, :], in1=bc3(w1_), op=ALU.mult), [oh1, w1_], [loc])
                    V(lambda e: e.tensor_tensor(out=tmp2[:, :c, :], in0=oh2[:, :c, :], in1=bc3(w2_), op=ALU.mult), [oh2, w2_], [tmp2])
                    V(lambda e: e.tensor_tensor(out=loc[:, :c, :], in0=loc[:, :c, :], in1=tmp2[:, :c, :], op=ALU.add), [loc, tmp2], [loc])
                    for gI in range(4):
                        C.op("dve", lambda e, gI=gI: e.tensor_tensor(out=comb[:, :c, 4 * gI:4 * gI + 4], in0=loc[:, :c, :],
                                                                     in1=ohg[:, :c, gI:gI + 1].to_broadcast([128, c, 4]), op=ALU.mult),
                             r=[loc, ohg], w=[comb], part=(gI > 0))
                    tb = PB.next()
                    for cj in range(c):
                        C.op("pe", lambda e, cj=cj: e.transpose(out=tb[0:16, cj * 128:(cj + 1) * 128], in_=comb[:, cj, :], identity=IDf),
                             r=[comb, CS], w=[tb], inc=(cj == c - 1), part=(cj > 0))
                    C.op("act", lambda e: e.copy(out=COMBT[:, off:off + n], in_=tb[0:16, 0:n]), r=[tb], w=[COMBT], part=True)
                C.barrier()
            with ExitStack() as s3:
                w1e = [C.sb(s3, [128, KC, 512], BF16, "w1e") for _ in range(2)]
                w3e = [C.sb(s3, [128, KC, 512], BF16, "w3e") for _ in range(2)]
                w2e = [C.sb(s3, [128, 4, 1024], BF16, "w2e") for _ in range(2)]
                ds_e = [[C.dsem(sw=True) for _ in range(3)] for _ in range(2)]
                sel = Ring([C.sb(s3, [16, 128], F32, "sel") for _ in range(2)])
                cbs = Ring([C.sb(s3, [128, 512], F32, "cbs") for _ in range(2)])
                s1 = Ring([C.sb(s3, [128, 512], F32, "s1") for _ in range(2)])
                s2_ = Ring([C.sb(s3, [128, 512], F32, "s2") for _ in range(2)])
                act = Ring([C.sb(s3, [128, 4, 512], BF16, "act") for _ in range(2)])

                def load_e(e_):
                    k = e_ % 2
                    v1 = w1[l, e_].rearrange("(kc p) n -> p kc n", p=128)
                    v3 = w3[l, e_].rearrange("(kc p) n -> p kc n", p=128)
                    v2 = w2[l, e_].rearrange("(kc p) n -> p kc n", p=128)
                    for kc in range(KC):
                        C.dma("pool", w1e[k][:, kc, :], v1[:, kc, :], ds_e[k][0], r=[b_in], w=[w1e[k]], part=(kc > 0))
                        C.dma("pool", w3e[k][:, kc, :], v3[:, kc, :], ds_e[k][1], r=[b_in], w=[w3e[k]], part=(kc > 0))
                    for fc in range(4):
                        C.dma("pool", w2e[k][:, fc, :], v2[:, fc, :], ds_e[k][2], r=[b_in], w=[w2e[k]], part=(fc > 0))

                load_e(0)
                for e_ in range(16):
                    if e_ + 1 < 16:
                        load_e(e_ + 1)
                    k = e_ % 2
                    se_ = sel.next()
                    C.op("dve", lambda e, e_=e_: e.tensor_copy(out=se_[:], in_=IDf[0:16, e_:e_ + 1].to_broadcast([16, 128])), r=[CS], w=[se_])
                    for ti in tiles:
                        off, n = TILES[ti]
                        r = 2 if ti == 0 else b
                        cb = PB.next()
                        mm_group(cb[:, :n], [(se_[:], COMBT[:, off:off + n])], cb, [se_, COMBT])
                        cb_ = cbs.next()
                        C.op("act", lambda e: e.copy(out=cb_[:, :n], in_=cb[:, :n]), r=[cb], w=[cb_])
                        a_ = act.next()
                        for fc in range(4):
                            z1 = PB.next()
                            mm_group(z1[:, :n], [(w1e[k][:, kc, fc * 128:(fc + 1) * 128], hT[:, kc, off:off + n]) for kc in range(KC)], z1, [w1e[k], hT])
                            z3 = PB.next()
                            mm_group(z3[:, :n], [(w3e[k][:, kc, fc * 128:(fc + 1) * 128], hT[:, kc, off:off + n]) for kc in range(KC)], z3, [w3e[k], hT])
                            a1 = s1.next()
                            C.op("act", lambda e: e.activation(out=a1[:, :n], in_=z1[:, :n], func=AF.Silu), r=[z1], w=[a1])
                            a2 = s2_.next()
                            C.op("dve", lambda e: e.tensor_tensor(out=a2[:, :n], in0=a1[:, :n], in1=cb_[:, :n], op=ALU.mult), r=[a1, cb_], w=[a2])
                            C.op("dve", lambda e, fc=fc: e.tensor_tensor(out=a_[:, fc, :n], in0=z3[:, :n], in1=a2[:, :n], op=ALU.mult),
                                 r=[z3, a2], w=[a_], part=(fc > 0))
                        for cc in range(KC):
                            yb = PB.next()
                            mm_group(yb[:, :n], [(w2e[k][:, fc, cc * 128:(cc + 1) * 128], a_[:, fc, :n]) for fc in range(4)], yb, [w2e[k], a_])
                            C.op("dve", lambda e, cc=cc: e.scalar_tensor_tensor(
                                out=XS[:, cc, off:off + n], in0=yb[:, :n], scalar=MOD[:, 40 + cc, r:r + 1], in1=XS[:, cc, off:off + n],
                                op0=ALU.mult, op1=ALU.add), r=[yb, MOD, xsb[ti][cc]], w=[xsb[ti][cc]])
                C.barrier()
            if not last:
                for ti in tiles:
                    off, n = TILES[ti]
                    C.dma("sp", XT[b, :, :, off:off + n], XS[:, :, off:off + n], ds_xs[ti], r=xsb[ti], w=[b_XT[b][ti]])
            else:
                with ExitStack() as s4:
                    sq = C.sb(s4, [128, KC, 512], BF16, "sq4")
                    rstd = C.sb(s4, [128, 512], F32, "rstd4")
                    tmp = Ring([C.sb(s4, [128, 512], F32, "ntmp4") for _ in range(2)])
                    ot = [C.sb(s4, [128, KC, 512], F32, "ot") for _ in range(2)]
                    ds_o = [C.dsem() for _ in range(2)]
                    fn_ = sp("final_norm")
                    FN = C.sb(s4, [128, KC, 3], F32, "FN")
                    for kc in range(KC):
                        C.op("dve", lambda e, kc=kc: e.tensor_copy(out=FN[:, kc, :], in_=fn_[:, kc:kc + 1].to_broadcast([128, 3])), r=[SP_], w=[FN], part=True)
                    for i, ti in enumerate(t for t in tiles if t > 0):
                        off, n = TILES[ti]
                        o_ = ot[i % 2]

                        def out_fn(kc, tb, n, o_=o_):
                            C.op("act", lambda e: e.copy(out=o_[:, kc, :n], in_=tb[:, :n]), r=[tb], w=[o_], part=True)
                        norm_tile(s4, l, b, ti, XS[:, :, off:off + n], xsb[ti], FN, 0, (sq, rstd, tmp), out_fn=out_fn)
                        C.dma("sp", outT[b, :, :, off - NCTX:off - NCTX + n], o_[:, :, :n], ds_o[i % 2], r=[o_], w=[b_out], part=True)
            C.barrier()

    stages = 0

    def done():
        nonlocal stages
        C.reset_dsems()
        stages += 1
        return stop_after is not None and stages >= stop_after

    def program():
        for l in range(n_layers):
            last = (l == last_l)
            layer_setup(l)
            C.reset_dsems()
            tiles_all = list(range(5))
            tiles_out = [1, 2, 3, 4] if last else tiles_all
            for b in range(NB):
                stage_norm1(l, b, tiles_all)
                if done(): return
                stage_ret(l, b)
                if done(): return
                stage_m1(l, b, tiles_out)
                if done(): return
                with ExitStack() as su:
                    U = C.sb(su, [128, 4, LT], BF16, "U")
                    P = C.sb(su, [128, 4, LT], BF16, "P")
                    stage_cp(l, b, tiles_out, U, P)
                    C.reset_dsems()
                    stage_m2(l, b, tiles_out, U)
                    if done(): return
                    stage_m3(l, b, tiles_out, P)
                if done(): return
                if last:
                    pass
                stage_moe(l, b, tiles_out)
                if done(): return

    program()
    C.barrier()
    if dbg:
        dh = dram("dbg_hT", [128, KC, NT], BF16, "ExternalOutput")
        C.dma("sp", dh[:, :, :], hT[:], ds_misc, r=[hT], w=[b_out])
    C.barrier()
    es.close()
    return nc


def _const_layout():
    off = {}
    o = 0
    for name, n in (("ident", 128), ("ones", 128), ("relp", 128), ("reln", 128), ("mf", 128), ("mb", 128),
                    ("iota1", 128), ("iotac", 128), ("jcol", 2), ("eps", 1), ("ive", 128)):
        off[name] = (o, n)
        o += n
    return off, o


CONST_OFF, CONST_W = _const_layout()


def _small_layout():
    off = {}
    o = 0
    for name, n in (("b_ada", DEPTH * 48), ("norm1", DEPTH * 8), ("norm2", DEPTH * 8), ("ret_decay", DEPTH * 8),
                    ("conv_w", DEPTH * 12), ("pool_scale", DEPTH * 4), ("b_r", DEPTH * 20), ("final_norm", 8)):
        off[name] = (o, n)
        o += n
    return off, o


SMALL_OFF, SMALL_W = _small_layout()


def _consts():
    c = np.zeros((128, CONST_W), np.float32)
    j = np.arange(128, dtype=np.float32)
    rel = j[None, :] - j[:, None]
    put = lambda name, v: c.__setitem__((slice(None), slice(CONST_OFF[name][0], CONST_OFF[name][0] + CONST_OFF[name][1])), v)
    put("ident", np.eye(128, dtype=np.float32))
    put("ones", np.ones((128, 128), np.float32))
    put("relp", np.maximum(rel, 0))
    put("reln", np.maximum(-rel, 0))
    put("mf", (rel >= 0).astype(np.float32))
    put("mb", (rel <= 0).astype(np.float32))
    put("iota1", np.broadcast_to(j[None, :] + 1.0, (128, 128)))
    put("iotac", np.broadcast_to(128.0 - j[None, :], (128, 128)))
    put("jcol", np.stack([j, 127.0 - j], axis=1))
    put("eps", np.full((128, 1), EPS, np.float32))
    pos = np.arange(SEQ)
    row = (pos // 64).astype(np.float32)
    col = (pos % 64).astype(np.float32)
    inv = (10000.0 ** (-np.arange(32, dtype=np.float32) / 32)).astype(np.float32)
    ang = np.concatenate([row[:, None] * inv[None, :], col[:, None] * inv[None, :]], axis=-1).astype(np.float32)
    cos, sin = np.cos(ang), np.sin(ang)
    sc = np.float32(128.0 ** -0.5)
    tab = np.stack([cos, sin, cos * sc, sin * sc], axis=0).reshape(4, 16, 128, 64).transpose(2, 0, 1, 3)
    ropet = np.ascontiguousarray(tab.reshape(128, 4 * 16 * 64)).astype(np.float32)
    iv = np.zeros((4, NT), np.float32)
    for gi, win in enumerate((2, 4, 8, 16)):
        for (o, L) in ((0, NCTX), (NCTX, SEQ)):
            t = np.arange(L)
            lo = np.clip(t - win // 2, 0, L)
            hi = np.clip(t + win - win // 2, 0, L)
            iv[gi, o:o + L] = 1.0 / (hi - lo)
    ive = np.zeros((4, 4, 8), np.float32)
    for gi in range(4):
        ive[gi, 0] = iv[gi, 0:8]
        ive[gi, 1] = iv[gi, NCTX - 8:NCTX]
        ive[gi, 2] = iv[gi, NCTX:NCTX + 8]
        ive[gi, 3] = iv[gi, NT - 8:NT]
    put("ive", np.broadcast_to(ive.reshape(1, 128), (128, 128)))
    return c, ropet


def _fm(v):
    return np.ascontiguousarray(v.reshape(-1, 128).T)


def _small(inp):
    s = np.zeros((128, SMALL_W), np.float32)

    def put(name, v):
        o, n = SMALL_OFF[name]
        assert v.shape == (128, n), (name, v.shape, n)
        s[:, o:o + n] = v
    put("b_ada", np.concatenate([_fm(inp["b_ada"][l]) for l in range(DEPTH)], axis=1))
    put("norm1", np.concatenate([_fm(inp["norm1"][l]) for l in range(DEPTH)], axis=1))
    put("norm2", np.concatenate([_fm(inp["norm2"][l]) for l in range(DEPTH)], axis=1))
    put("ret_decay", np.broadcast_to(inp["ret_decay"].reshape(1, DEPTH * 8), (128, DEPTH * 8)))
    cw = inp["conv_w"].reshape(DEPTH, 3, 4, 128).transpose(3, 0, 1, 2).reshape(128, DEPTH * 12)
    put("conv_w", cw)
    put("pool_scale", inp["pool_scale"].reshape(DEPTH, 4, 128).transpose(2, 0, 1).reshape(128, DEPTH * 4))
    br = np.concatenate([inp["b_rg"], inp["b_re"]], axis=1)
    put("b_r", np.broadcast_to(br.reshape(1, DEPTH * 20), (128, DEPTH * 20)))
    put("final_norm", _fm(inp["final_norm"]))
    return s


def _prep(inputs, NB, core):
    x, ctx = inputs["x"], inputs["ctx"]
    bs = slice(core * NB, (core + 1) * NB)
    seq = np.concatenate([ctx[bs], x[bs]], axis=1)
    xin = np.ascontiguousarray(seq.reshape(NB, NT, KC, 128).transpose(0, 3, 2, 1))
    crow = np.concatenate([inputs["c"][bs], inputs["c_ctx"][None, :]] if NB == 2 else
                          [inputs["c"][bs], inputs["c"][bs], inputs["c_ctx"][None, :]], axis=0)
    cT = np.ascontiguousarray(crow.reshape(3, KC, 128).transpose(2, 1, 0))
    return xin, cT


def _shared(inputs):
    c, ropet = _consts()
    wrc = np.concatenate([inputs["w_rg"], inputs["w_re"]], axis=2)
    wr = np.ascontiguousarray(wrc.reshape(DEPTH, KC, 128, 20).transpose(2, 0, 1, 3).reshape(128, DEPTH * KC * 20))
    d = {"consts": c, "ropet": ropet, "wr": wr, "smallp": _small(inputs)}
    for k in ("w_ada", "w_in", "w_ret_out", "w_conv_out", "w_pool_out", "w_o", "pool_w", "w1", "w3", "w2"):
        d[k] = np.ascontiguousarray(inputs[k], dtype=np.float32)
    return d


def kernel(**inputs):
    inputs = {k: np.asarray(v, dtype=np.float32) for k, v in inputs.items()}
    n = 8
    NB = 2
    nc = build(NB=NB)
    shared = _shared(inputs)
    in_maps = []
    for core in range(n):
        xin, cT = _prep(inputs, NB, core)
        m = dict(shared)
        m["xin"] = xin
        m["cT"] = cT
        in_maps.append(m)
    res = run_bass_kernel_spmd(nc, in_maps, core_ids=list(range(n)))
    outs = []
    for r in res.results:
        o = np.asarray(r["outT"])
        outs.append(o.transpose(0, 3, 2, 1).reshape(NB, SEQ, D))
    return np.ascontiguousarray(np.concatenate(outs, axis=0)).astype(np.float32)
```

```python
import os
import numpy as np
from contextlib import ExitStack
CUT = int(os.environ.get('K_RET_CUT', '0'))
import concourse.bass as bass
import concourse.mybir as mybir
from concourse.bass_utils import run_bass_kernel_spmd

F32 = mybir.dt.float32
BF16 = mybir.dt.bfloat16
AF = mybir.ActivationFunctionType
ALU = mybir.AluOpType
AX = mybir.AxisListType

D = 1024
KC = 8
NCTX = 256
SEQ = 2048
NT = NCTX + SEQ
NCH = NT // 128
DEPTH = 4
EPS = 1e-6
TILES = [(0, 256), (256, 512), (768, 512), (1280, 512), (1792, 512)]
O_Q, O_K, O_V, O_G, O_CB, O_CC, O_CX, O_PI, O_GT = 0, 512, 1024, 2048, 3072, 3584, 4096, 4608, 5120
LT = 2336


def ucol(t):
    return 8 + t if t < NCTX else t + 24


ALLBUFS = []


class Buf:
    __slots__ = ("w", "r")

    def __init__(self):
        self.w = []
        self.r = []
        ALLBUFS.append(self)


class DSem:
    def __init__(self, sem):
        self.sem = sem
        self.cnt = 0


class T:
    def __init__(self, t):
        self.t = t
        self.b = Buf()

    def __getitem__(self, k):
        return self.t[k]


class Sub(T):
    def __init__(self, ap, b):
        self.t = ap
        self.b = b


class Ctx:
    def __init__(self, nc, es):
        self.nc = nc
        self.es = es
        self.engs = {"pe": nc.tensor, "act": nc.scalar, "dve": nc.vector, "pool": nc.gpsimd, "sp": nc.sync}
        self.sem = {k: es.enter_context(nc.semaphore("s_" + k)) for k in self.engs}
        self.cnt = {k: 0 for k in self.engs}
        self.seen = {k: {} for k in self.engs}
        self.dsems = []
        self.nsb = 0

    def sb(self, es, shape, dt, name=None):
        self.nsb += 1
        return T(es.enter_context(self.nc.sbuf_tensor(f"{name or 't'}_{self.nsb}", list(shape), dt)))

    def dsem(self, perm=False, sw=False):
        if sw:
            if not hasattr(self, "swpool_"):
                self.swpool_ = []
                self.swptr = 0
            if self.swptr >= len(self.swpool_):
                s = DSem(self.es.enter_context(self.nc.semaphore(f"dw{len(self.dsems)}")))
                self.dsems.append(s)
                self.swpool_.append(s)
            s = self.swpool_[self.swptr]
            self.swptr += 1
            return s
        if perm:
            s = DSem(self.es.enter_context(self.nc.semaphore(f"dp{len(self.dsems)}")))
            self.dsems.append(s)
            return s
        if not hasattr(self, "pool_"):
            self.pool_ = []
            self.dptr = 0
        if self.dptr >= len(self.pool_):
            s = DSem(self.es.enter_context(self.nc.semaphore(f"d{len(self.dsems)}")))
            self.dsems.append(s)
            self.pool_.append(s)
        s = self.pool_[self.dptr]
        self.dptr += 1
        return s

    def _bufs(self, xs):
        return [x.b if isinstance(x, T) else x for x in xs if x is not None]

    def _wait(self, eng, deps):
        best = {}
        for key, val in deps:
            if best.get(key, 0) < val:
                best[key] = val
        e = self.engs[eng]
        for key, val in best.items():
            if self.seen[eng].get(key, 0) >= val:
                continue
            sem = key.sem if isinstance(key, DSem) else self.sem[key]
            e.wait_ge(sem, val)
            self.seen[eng][key] = val

    def _deps(self, eng, reads, writes, part=False, skipkey=None):
        deps = []
        for b in reads:
            deps += b.w
        for b in writes:
            deps += [d for d in b.w if d[0] is not skipkey]
            deps += b.r
        if eng == "pe":
            deps = [d for d in deps if d[0] != "pe"]
        return deps

    def op(self, eng, fn, r=(), w=(), inc=True, part=False):
        reads, writes = self._bufs(r), self._bufs(w)
        self._wait(eng, self._deps(eng, reads, writes, part))
        ins = fn(self.engs[eng])
        tk = (eng, self.cnt[eng] + 1)
        if inc:
            ins.then_inc(self.sem[eng], 1)
            self.cnt[eng] += 1
        for b in reads:
            if not b.r or b.r[-1] != tk:
                b.r.append(tk)
        for b in writes:
            if part:
                if not b.w or b.w[-1] != tk:
                    b.w.append(tk)
            else:
                b.w = [tk]
                b.r = []
        return ins

    def dma(self, q, out, in_, ds, r=(), w=(), part=False):
        reads, writes = self._bufs(r), self._bufs(w)
        self._wait(q, self._deps(q, reads, writes, part, ds if part else None))
        ins = self.engs[q].dma_start(out=out, in_=in_)
        ds.cnt += 16
        ins.then_inc(ds.sem, 16)
        tk = (ds, ds.cnt)
        for b in reads:
            b.r.append(tk)
        for b in writes:
            if part:
                b.w.append(tk)
            else:
                b.w = [tk]
                b.r = []

    def barrier(self):
        for eng in self.engs:
            deps = [(k, self.cnt[k]) for k in self.engs if k != eng and self.cnt[k] > 0]
            deps += [(d, d.cnt) for d in self.dsems if d.cnt > 0]
            self._wait(eng, deps)
        for b in ALLBUFS:
            b.w = []
            b.r = []

    def reset_dsems(self):
        self.dptr = 0
        self.swptr = 0


class Ring:
    def __init__(self, items):
        self.items = items
        self.i = 0

    def next(self):
        x = self.items[self.i % len(self.items)]
        self.i += 1
        return x


def build(NB=2, n_layers=DEPTH, dbg=False, stop_after=None):
    nc = bass.Bass("TRN2", target_bir_lowering=False)
    es = ExitStack()
    C = Ctx(nc, es)
    last_l = DEPTH - 1

    def dram(name, shape, dt, kind):
        return nc.dram_tensor(name, list(shape), dt, kind=kind).ap()

    xin = dram("xin", [NB, 128, KC, NT], F32, "ExternalInput")
    cT = dram("cT", [128, KC, 3], F32, "ExternalInput")
    w_ada = dram("w_ada", [DEPTH, D, 6 * D], F32, "ExternalInput")
    w_in = dram("w_in", [DEPTH, D, 8192], F32, "ExternalInput")
    w_ret_out = dram("w_ret_out", [DEPTH, 1024, D], F32, "ExternalInput")
    w_conv_out = dram("w_conv_out", [DEPTH, 512, D], F32, "ExternalInput")
    w_pool_out = dram("w_pool_out", [DEPTH, 512, D], F32, "ExternalInput")
    w_o = dram("w_o", [DEPTH, D, D], F32, "ExternalInput")
    pool_w = dram("pool_w", [DEPTH, 4, 128, 128], F32, "ExternalInput")
    w1 = dram("w1", [DEPTH, 16, D, 512], F32, "ExternalInput")
    w3 = dram("w3", [DEPTH, 16, D, 512], F32, "ExternalInput")
    w2 = dram("w2", [DEPTH, 16, 512, D], F32, "ExternalInput")
    smallp = dram("smallp", [128, SMALL_W], F32, "ExternalInput")
    wr = dram("wr", [128, DEPTH * KC * 20], F32, "ExternalInput")
    consts = dram("consts", [128, CONST_W], F32, "ExternalInput")
    ropet = dram("ropet", [128, 4 * 16 * 64], F32, "ExternalInput")
    outT = dram("outT", [NB, 128, KC, SEQ], F32, "ExternalOutput")
    skind = "ExternalOutput" if dbg else "Internal"
    XT = dram("XT", [NB, 128, KC, NT], F32, skind)
    YRT = dram("YRT", [128, KC, NT], BF16, skind)
    MG = dram("MG", [128, KC, NT], F32, skind)
    SBS = dram("SBS", [NCH, 128, 1024], BF16, "Internal")
    KVS = dram("KVS", [NCH, 128, 1536], BF16, "Internal")
    b_KV = [Buf() for _ in range(NCH)]
    b_XT = [[Buf() for _ in TILES] for _ in range(NB)]
    b_YRT = [Buf() for _ in range(NCH)]
    b_MG = [Buf() for _ in TILES]
    b_SBS = [Buf() for _ in range(NCH)]
    b_out = Buf()
    b_in = None

    g = es
    hT = C.sb(g, [128, KC, NT], BF16, "hT")
    CS = C.sb(g, [128, CONST_W], F32, "CS")
    SP_ = C.sb(g, [128, SMALL_W], F32, "SP")
    IDb = C.sb(g, [128, 128], BF16, "IDb")
    ONESb = C.sb(g, [128, 128], BF16, "ONESb")
    MOD = C.sb(g, [128, 48, 3], F32, "MOD")
    GS1 = C.sb(g, [128, KC, 3], F32, "GS1")
    GS2 = C.sb(g, [128, KC, 3], F32, "GS2")
    LG = C.sb(g, [128, 8], F32, "LG")
    GCt = C.sb(g, [128, 8], F32, "GC")
    DBt = C.sb(g, [128, 4], F32, "DB")
    DFt = C.sb(g, [128, 4], F32, "DF")
    DM = C.sb(g, [128, 4, 128], F32, "DM")
    DQF = C.sb(g, [128, 4, 128], F32, "DQF")
    DQB = C.sb(g, [128, 4, 128], F32, "DQB")
    SIC = C.sb(g, [128, KC, 3], F32, "SIC")
    banks = [T(es.enter_context(nc.psum_tensor(f"pb{i}", [128, 512], F32))) for i in range(6)]
    PB = Ring(banks)
    PT = Ring([T(es.enter_context(nc.psum_tensor(f"pt{i}", [128, 1024], BF16))) for i in range(2)])
    ds_misc = C.dsem(perm=True)
    ds_sp = C.dsem(perm=True)

    def cs(name):
        o, n = CONST_OFF[name]
        return CS[:, o:o + n]

    IDf = cs("ident")
    ONES = cs("ones")

    def sp(name, l=None):
        o, n = SMALL_OFF[name]
        if l is None:
            return SP_[:, o:o + n]
        per = n // DEPTH
        return SP_[:, o + l * per:o + (l + 1) * per]

    C.dma("sp", CS[:], consts[:, :], ds_misc, r=[b_in], w=[CS])
    C.dma("sp", SP_[:], smallp[:, :], ds_sp, r=[b_in], w=[SP_])
    C.op("dve", lambda e: e.tensor_copy(out=IDb[:], in_=IDf), r=[CS], w=[IDb])
    C.op("dve", lambda e: e.tensor_copy(out=ONESb[:], in_=ONES), r=[CS], w=[ONESb])
    ds_c = C.dsem(perm=True)
    C.dma("sp", SIC[:], cT[:, :, :], ds_c, r=[b_in], w=[SIC])
    C.op("act", lambda e: e.activation(out=SIC[:], in_=SIC[:], func=AF.Silu), r=[SIC], w=[SIC])
    SICb = C.sb(g, [128, KC, 3], BF16, "SICb")
    C.op("dve", lambda e: e.tensor_copy(out=SICb[:], in_=SIC[:]), r=[SIC], w=[SICb])

    def mm_group(out_ap, pairs, bank, reads, fp32=False):
        n = len(pairs)
        for i, (l_, r_) in enumerate(pairs):
            C.op("pe", lambda e, l_=l_, r_=r_, i=i: e.matmul(out_ap, lhsT=l_, rhs=r_, start=(i == 0), stop=(i == n - 1)),
                 r=reads, w=[bank], inc=(i == n - 1), part=False)

    def load_w(es_, src2d, ncols, nk, ds, name):
        wt = C.sb(es_, [128, nk, ncols], BF16, name)
        ds = C.dsem(sw=True)
        v = src2d.rearrange("(kc p) n -> p kc n", p=128)
        for kc in range(nk):
            C.dma("pool", wt[:, kc, :], v[:, kc, :], ds, r=[b_in], w=[wt], part=(kc > 0))
        return wt

    def layer_setup(l):
        with ExitStack() as s:
            wa = [C.sb(s, [128, KC, 512], BF16, "wa") for _ in range(2)]
            dsw = [C.dsem(sw=True) for _ in range(2)]
            wav = w_ada[l].rearrange("(kc p) n -> p kc n", p=128)
            bada = sp("b_ada", l)
            for blk in range(12):
                wt = wa[blk % 2]
                C.dma("pool", wt[:], wav[:, :, blk * 512:(blk + 1) * 512], dsw[blk % 2], r=[b_in], w=[wt])
                for jj in range(4):
                    j = blk * 4 + jj
                    bank = PB.next()
                    mm_group(bank[:, 0:3], [(wt[:, kc, jj * 128:(jj + 1) * 128], SICb[:, kc, :]) for kc in range(KC)],
                             bank, [wt, SICb])
                    C.op("dve", lambda e, j=j, bank=bank: e.tensor_scalar(
                        out=MOD[:, j, :], in0=bank[:, 0:3], scalar1=bada[:, j:j + 1], scalar2=None, op0=ALU.add),
                        r=[bank, SP_], w=[MOD], part=True)
            for (GS, nm, jo) in ((GS1, "norm1", 8), (GS2, "norm2", 32)):
                ng = sp(nm, l)
                for kc in range(KC):
                    C.op("dve", lambda e, GS=GS, kc=kc, jo=jo, ng=ng: e.tensor_scalar(
                        out=GS[:, kc, :], in0=MOD[:, jo + kc, :], scalar1=1.0, scalar2=ng[:, kc:kc + 1],
                        op0=ALU.add, op1=ALU.mult), r=[MOD, SP_], w=[GS], part=True)
            rd = sp("ret_decay", l)
            C.op("act", lambda e: e.activation(out=LG[:], in_=rd, func=AF.Sigmoid), r=[SP_], w=[LG])
            C.op("act", lambda e: e.activation(out=LG[:], in_=LG[:], func=AF.Ln), r=[LG], w=[LG])
            C.op("act", lambda e: e.activation(out=GCt[:], in_=LG[:], func=AF.Exp, scale=128.0), r=[LG], w=[GCt])
            jc = cs("jcol")
            C.op("act", lambda e: e.activation(out=DBt[:], in_=LG[:, 4:8], func=AF.Exp, scale=jc[:, 0:1]), r=[LG, CS], w=[DBt])
            C.op("act", lambda e: e.activation(out=DFt[:], in_=LG[:, 0:4], func=AF.Exp, scale=jc[:, 1:2]), r=[LG, CS], w=[DFt])
            tmp = C.sb(s, [128, 128], F32, "dtmp")
            for h in range(4):
                C.op("act", lambda e, h=h: e.activation(out=DQF[:, h, :], in_=cs("iota1"), func=AF.Exp, scale=LG[:, h:h + 1]),
                     r=[LG, CS], w=[DQF], part=True)
                C.op("act", lambda e, h=h: e.activation(out=DQB[:, h, :], in_=cs("iotac"), func=AF.Exp, scale=LG[:, 4 + h:5 + h]),
                     r=[LG, CS], w=[DQB], part=True)
                C.op("act", lambda e, h=h: e.activation(out=DM[:, h, :], in_=cs("relp"), func=AF.Exp, scale=LG[:, h:h + 1]),
                     r=[LG, CS], w=[DM], part=True)
                C.op("dve", lambda e, h=h: e.tensor_tensor(out=DM[:, h, :], in0=DM[:, h, :], in1=cs("mf"), op=ALU.mult),
                     r=[DM, CS], w=[DM])
                C.op("act", lambda e, h=h: e.activation(out=tmp[:], in_=cs("reln"), func=AF.Exp, scale=LG[:, 4 + h:5 + h]),
                     r=[LG, CS], w=[tmp])
                C.op("dve", lambda e: e.tensor_tensor(out=tmp[:], in0=tmp[:], in1=cs("mb"), op=ALU.mult), r=[tmp, CS], w=[tmp])
                C.op("dve", lambda e, h=h: e.tensor_tensor(out=DM[:, h, :], in0=DM[:, h, :], in1=tmp[:], op=ALU.add),
                     r=[DM, tmp], w=[DM])
            C.barrier()

    def norm_tile(s, l, b, ti, src_ap, src_buf, GS, sh_j, bufs, hf=None, out_fn=None):
        off, n = TILES[ti]
        r = 2 if ti == 0 else b
        sq, rstd, tmp = bufs
        sbl = src_buf if isinstance(src_buf, list) else [src_buf] * KC
        C.op("act", lambda e: e.activation(out=sq[:, :, :n], in_=src_ap, func=AF.Square), r=sbl, w=[sq])
        bank = PB.next()
        mm_group(bank[:, :n], [(ONESb[:], sq[:, kc, :n]) for kc in range(KC)], bank, [sq, ONESb])
        C.op("act", lambda e: e.activation(out=rstd[:, :n], in_=bank[:, :n], func=AF.Sqrt, scale=1.0 / D, bias=cs("eps")[:, 0:1]),
             r=[bank, CS], w=[rstd])
        C.op("dve", lambda e: e.reciprocal(out=rstd[:, :n], in_=rstd[:, :n]), r=[rstd], w=[rstd])
        for kc in range(KC):
            tb = tmp.next()
            C.op("dve", lambda e, kc=kc, tb=tb: e.scalar_tensor_tensor(
                out=tb[:, :n], in0=src_ap[:, kc, :], scalar=GS[:, kc, r:r + 1], in1=rstd[:, :n], op0=ALU.mult, op1=ALU.mult),
                r=[sbl[kc], GS, rstd], w=[tb])
            if out_fn is not None:
                out_fn(kc, tb, n)
            elif hf is None:
                C.op("act", lambda e, kc=kc, tb=tb: e.activation(
                    out=hT[:, kc, off:off + n], in_=tb[:, :n], func=AF.Identity, bias=MOD[:, sh_j + kc, r:r + 1]),
                    r=[tb, MOD], w=[hT], part=True)
            else:
                C.op("act", lambda e, kc=kc, tb=tb: e.activation(
                    out=hf[:, kc, :n], in_=tb[:, :n], func=AF.Identity, bias=MOD[:, sh_j + kc, r:r + 1]),
                    r=[tb, MOD], w=[hf], part=True)
                C.op("dve", lambda e, kc=kc: e.tensor_copy(out=hT[:, kc, off:off + n], in_=hf[:, kc, :n]),
                     r=[hf], w=[hT], part=True)

    def stage_norm1(l, b, tiles):
        with ExitStack() as s:
            xt = [C.sb(s, [128, KC, 512], F32, "xt") for _ in range(2)]
            dsx = [C.dsem() for _ in range(2)]
            sq = C.sb(s, [128, KC, 512], BF16, "sq")
            rstd = C.sb(s, [128, 512], F32, "rstd")
            tmp = Ring([C.sb(s, [128, 512], F32, "ntmp") for _ in range(2)])
            src = xin if l == 0 else XT
            for i, ti in enumerate(tiles):
                off, n = TILES[ti]
                x_ = xt[i % 2]
                C.dma("sp", x_[:, :, :n], src[b, :, :, off:off + n], dsx[i % 2],
                      r=[b_in if l == 0 else b_XT[b][ti]], w=[x_])
                norm_tile(s, l, b, ti, x_[:, :, :n], x_, GS1, 0, (sq, rstd, tmp))
            C.barrier()

    def rope_evac(src_bank, dst, tabs, ci, rt, rt2):
        cos_t, sin_t, tab_buf = tabs
        n = ci - 2
        sv = src_bank[:, :].rearrange("p (h t d) -> p h t d", h=4, t=2)
        dv = dst[:, :, :].rearrange("p h (t d) -> p h t d", t=2)
        t1, t2 = rt
        c_ = cos_t[:, n, :].unsqueeze(1).to_broadcast([128, 4, 64])
        s_ = sin_t[:, n, :].unsqueeze(1).to_broadcast([128, 4, 64])
        C.op("dve", lambda e: e.tensor_tensor(out=t1[:], in0=sv[:, :, 0, :], in1=c_, op=ALU.mult), r=[src_bank, tab_buf], w=[t1])
        C.op("dve", lambda e: e.tensor_tensor(out=t2[:], in0=sv[:, :, 1, :], in1=s_, op=ALU.mult), r=[src_bank, tab_buf], w=[t2])
        C.op("dve", lambda e: e.tensor_tensor(out=dv[:, :, 0, :], in0=t1[:], in1=t2[:], op=ALU.subtract), r=[t1, t2], w=[dst], part=True)
        t3, t4 = rt2
        C.op("dve", lambda e: e.tensor_tensor(out=t3[:], in0=sv[:, :, 0, :], in1=s_, op=ALU.mult), r=[src_bank, tab_buf], w=[t3])
        C.op("dve", lambda e: e.tensor_tensor(out=t4[:], in0=sv[:, :, 1, :], in1=c_, op=ALU.mult), r=[src_bank, tab_buf], w=[t4])
        C.op("dve", lambda e: e.tensor_tensor(out=dv[:, :, 1, :], in0=t3[:], in1=t4[:], op=ALU.add), r=[t3, t4], w=[dst], part=True)

    def stage_ret(l, b):
        last = (l == last_l)
        with ExitStack() as s:
            dsw = C.dsem(sw=True)
            WK = load_w(s, w_in[l, :, O_K:O_K + 512], 512, KC, dsw, "WK")
            WV = load_w(s, w_in[l, :, O_V:O_V + 1024], 1024, KC, dsw, "WV")
            WQ = load_w(s, w_in[l, :, O_Q:O_Q + 512], 512, KC, dsw, "WQ")
            WG = load_w(s, w_in[l, :, O_G:O_G + 1024], 1024, KC, dsw, "WG")
            RT = C.sb(s, [128, 4 * 16 * 64], F32, "RT")
            C.dma("sp", RT[:], ropet[:, :], C.dsem(), r=[b_in], w=[RT])
            rtv = RT[:, :].rearrange("p (k n d) -> p k n d", k=4, n=16)
            tabq = (rtv[:, 0], rtv[:, 1], RT)
            tabk = (rtv[:, 2], rtv[:, 3], RT)
            rt = (C.sb(s, [128, 4, 64], F32, "rt1"), C.sb(s, [128, 4, 64], F32, "rt2"))
            rt2 = (C.sb(s, [128, 4, 64], F32, "rt3"), C.sb(s, [128, 4, 64], F32, "rt4"))
            Sb = C.sb(s, [128, 4, 256], F32, "Sb")
            Sf = C.sb(s, [128, 4, 256], F32, "Sf")
            qr = Ring([C.sb(s, [128, 4, 128], BF16, "qr") for _ in range(2)])
            kvt = [C.sb(s, [128, 1536], BF16, "kvt") for _ in range(2)]
            ds_kvo = [C.dsem() for _ in range(2)]
            ds_kvi = [C.dsem() for _ in range(2)]

            def kv_views(t_):
                return (Sub(t_[:, 0:512].rearrange("p (h d) -> p h d", h=4), t_.b),
                        Sub(t_[:, 512:1536].rearrange("p (h d) -> p h d", h=4), t_.b))
            vd = Ring([C.sb(s, [128, 4, 256], BF16, "vd") for _ in range(2)])
            sg = Ring([C.sb(s, [128, 4, 256], F32, "sg") for _ in range(2)])
            sbo = [C.sb(s, [128, 1024], BF16, "sbo") for _ in range(2)]
            ds_sbo = [C.dsem() for _ in range(2)]
            sbi = [C.sb(s, [128, 4, 256], BF16, "sbi") for _ in range(2)]
            ds_sbi = [C.dsem() for _ in range(2)]
            sfb = Ring([C.sb(s, [128, 4, 256], BF16, "sfb") for _ in range(2)])
            qT = Ring([C.sb(s, [128, 4, 128], BF16, "qT") for _ in range(2)])
            qfT = Ring([C.sb(s, [128, 4, 128], BF16, "qfT") for _ in range(2)])
            qbT = Ring([C.sb(s, [128, 4, 128], BF16, "qbT") for _ in range(2)])
            kT = Ring([C.sb(s, [128, 4, 128], BF16, "kT") for _ in range(2)])
            sT = Ring([C.sb(s, [128, 4, 128], BF16, "sT") for _ in range(2)])
            yn = Ring([C.sb(s, [128, 256], F32, "yn") for _ in range(2)])
            yr = Ring([C.sb(s, [128, 1024], BF16, "yr") for _ in range(2)])
            yT = [C.sb(s, [128, KC, 128], BF16, "yT") for _ in range(2)]
            ds_yT = [C.dsem() for _ in range(2)]
            st6 = C.sb(s, [128, 4, 6], F32, "st6")
            mv = C.sb(s, [128, 4, 2], F32, "mv")
            rs = C.sb(s, [128, 4], F32, "rs")

            def proj(ci, W, c0, ncols):
                bank = PB.next()
                mm_group(bank[:, :ncols], [(hT[:, kc, ci * 128:(ci + 1) * 128], W[:, kc, c0:c0 + ncols]) for kc in range(KC)],
                         bank, [hT, W])
                return bank

            def k_evac(ci, kps, k_):
                if ci >= 2:
                    rope_evac(kps, k_, tabk, ci, rt, rt2)
                else:
                    C.op("act", lambda e: e.activation(out=k_[:, :, :].rearrange("p h d -> p (h d)"), in_=kps[:, :],
                                                       func=AF.Identity, scale=128.0 ** -0.5), r=[kps], w=[k_], part=True)
                return k_

            def v_scaled(vps, dec, dst):
                for h in range(4):
                    bk = vps[h // 2]
                    C.op("act", lambda e, h=h, bk=bk: e.activation(
                        out=dst[:, h, :], in_=bk[:, (h % 2) * 256:(h % 2) * 256 + 256], func=AF.Identity, scale=dec[:, h:h + 1]),
                        r=[bk, dec], w=[dst], part=True)

            Sbh = [Buf() for _ in range(4)]
            Sfh = [Buf() for _ in range(4)]

            def state_update(S, k_, vdd, gcol0):
                SH = Sbh if S is Sb else Sfh
                kv = [PB.next(), PB.next()]
                for h in range(4):
                    bk = kv[h // 2]
                    C.op("pe", lambda e, h=h, bk=bk: e.matmul(bk[:, (h % 2) * 256:(h % 2) * 256 + 256], lhsT=k_[:, h, :],
                                                             rhs=vdd[:, h, :], start=True, stop=True),
                         r=[k_, vdd], w=[bk], part=(h % 2 == 1))
                for h in range(4):
                    bk = kv[h // 2]
                    C.op("dve", lambda e, h=h, bk=bk: e.scalar_tensor_tensor(
                        out=S[:, h, :], in0=S[:, h, :], scalar=GCt[:, gcol0 + h:gcol0 + h + 1],
                        in1=bk[:, (h % 2) * 256:(h % 2) * 256 + 256], op0=ALU.mult, op1=ALU.add),
                        r=[SH[h], GCt, bk], w=[SH[h]])

            C.op("dve", lambda e: e.memset(Sb[:], 0.0), w=Sbh)
            C.op("dve", lambda e: e.memset(Sf[:], 0.0), w=Sfh)
            order = [1, 0] + list(range(NCH - 1, 1, -1))
            fl = lambda t_: t_[:, :, :].rearrange("p h d -> p (h d)")

            def front1(i, ci):
                kps = proj(ci, WK, 0, 512)
                vps = [proj(ci, WV, 0, 512), proj(ci, WV, 512, 512)]
                t_ = kvt[i % 2]
                C.op("act", lambda e: e.copy(out=t_[:, 512:1024], in_=vps[0][:, :]), r=[vps[0]], w=[t_])
                C.op("act", lambda e: e.copy(out=t_[:, 1024:1536], in_=vps[1][:, :]), r=[vps[1]], w=[t_], part=True)
                k_, _v = kv_views(t_)
                k_evac(ci, kps, k_)
                vd_ = vd.next()
                v_scaled(vps, DBt, vd_)
                C.dma("sp", KVS[ci, :, :], t_[:], ds_kvo[i % 2], r=[t_], w=[b_KV[ci]])
                return k_, vd_

            def back1(i, ci, k_, vd_):
                so = sbo[i % 2]
                C.op("act", lambda e: e.copy(out=so[:], in_=fl(Sb)), r=Sbh, w=[so])
                C.dma("sp", SBS[ci, :, :], so[:], ds_sbo[i % 2], r=[so], w=[b_SBS[ci]])
                state_update(Sb, k_, vd_, 4)

            cur = front1(0, order[0])
            for i, ci in enumerate(order):
                nxt = front1(i + 1, order[i + 1]) if i + 1 < len(order) else None
                back1(i, ci, *cur)
                cur = nxt

            def front2(ci):
                only_state = last and ci < 2
                t_ = kvt[ci % 2]
                C.dma("sp", t_[:], KVS[ci, :, :], ds_kvi[ci % 2], r=[b_KV[ci]], w=[t_])
                k_, vb_ = kv_views(t_)
                vdf = vd.next()
                for h in range(4):
                    C.op("act", lambda e, h=h: e.activation(out=vdf[:, h, :], in_=vb_[:, h, :], func=AF.Identity, scale=DFt[:, h:h + 1]),
                         r=[t_, DFt], w=[vdf], part=(h > 0))
                H = dict(k_=k_, vdf=vdf, only_state=only_state)
                if only_state:
                    return H
                si = sbi[ci % 2]
                C.dma("sp", fl(si), SBS[ci, :, :], ds_sbi[ci % 2], r=[b_SBS[ci]], w=[si])
                qps = proj(ci, WQ, 0, 512)
                gps = [proj(ci, WG, 0, 512), proj(ci, WG, 512, 512)]
                q_ = qr.next()
                if ci >= 2:
                    rope_evac(qps, q_, tabq, ci, rt, rt2)
                else:
                    C.op("act", lambda e: e.copy(out=fl(q_), in_=qps[:, :]), r=[qps], w=[q_])
                sg_ = sg.next()
                for j in range(2):
                    C.op("act", lambda e, j=j: e.activation(out=sg_[:, 2 * j:2 * j + 2, :].rearrange("p h d -> p (h d)"),
                                                           in_=gps[j][:, :], func=AF.Silu), r=[gps[j]], w=[sg_], part=(j == 1))
                tb = PT.next()
                tbv = tb[:, :]
                for h in range(4):
                    C.op("pe", lambda e, h=h: e.transpose(out=tbv[:, h * 128:(h + 1) * 128], in_=q_[:, h, :], identity=IDb[:]),
                         r=[q_, IDb], w=[tb], inc=False, part=(h > 0))
                for h in range(4):
                    C.op("pe", lambda e, h=h: e.transpose(out=tbv[:, 512 + h * 128:512 + (h + 1) * 128], in_=k_[:, h, :], identity=IDb[:]),
                         r=[k_, IDb], w=[tb], inc=(h == 3), part=True)
                qT_, qfT_, qbT_, kT_ = qT.next(), qfT.next(), qbT.next(), kT.next()
                C.op("act", lambda e: e.copy(out=fl(qT_), in_=tbv[:, 0:512]), r=[tb], w=[qT_])
                C.op("act", lambda e: e.copy(out=fl(kT_), in_=tbv[:, 512:1024]), r=[tb], w=[kT_])
                C.op("dve", lambda e: e.tensor_tensor(out=fl(qfT_), in0=fl(qT_), in1=fl(DQF), op=ALU.mult), r=[qT_, DQF], w=[qfT_])
                C.op("dve", lambda e: e.tensor_tensor(out=fl(qbT_), in0=fl(qT_), in1=fl(DQB), op=ALU.mult), r=[qT_, DQB], w=[qbT_])
                scb = PB.next()
                for h in range(4):
                    C.op("pe", lambda e, h=h: e.matmul(scb[:, h * 128:(h + 1) * 128], lhsT=kT_[:, h, :], rhs=qT_[:, h, :],
                                                       start=True, stop=True), r=[kT_, qT_], w=[scb], inc=(h == 3), part=(h > 0))
                sT_ = sT.next()
                C.op("dve", lambda e: e.tensor_tensor(out=fl(sT_), in0=scb[:, :], in1=fl(DM), op=ALU.mult), r=[scb, DM], w=[sT_])
                H.update(si=si, vb_=vb_, sg_=sg_, qfT_=qfT_, qbT_=qbT_, sT_=sT_)
                return H

            def back2(ci, H, sf_cur):
                k_, vdf = H["k_"], H["vdf"]
                if not H["only_state"]:
                    si, vb_, sg_, qfT_, qbT_, sT_ = (H[k] for k in ("si", "vb_", "sg_", "qfT_", "qbT_", "sT_"))
                    ob = [PB.next(), PB.next()]
                    for h in range(4):
                        bk = ob[h // 2]
                        oap = bk[:, (h % 2) * 256:(h % 2) * 256 + 256]
                        C.op("pe", lambda e, h=h, oap=oap: e.matmul(oap, lhsT=sT_[:, h, :], rhs=vb_[:, h, :], start=True, stop=False),
                             r=[sT_, vb_], w=[bk], inc=False, part=(h % 2 == 1))
                        C.op("pe", lambda e, h=h, oap=oap: e.matmul(oap, lhsT=qfT_[:, h, :], rhs=sf_cur[:, h, :], start=False, stop=False),
                             r=[qfT_, sf_cur], w=[bk], inc=False, part=True)
                        C.op("pe", lambda e, h=h, oap=oap: e.matmul(oap, lhsT=qbT_[:, h, :], rhs=si[:, h, :], start=False, stop=True),
                             r=[qbT_, si], w=[bk], inc=True, part=True)
                    for h in range(4):
                        bk = ob[h // 2]
                        C.op("dve", lambda e, h=h, bk=bk: e.bn_stats(out=st6[:, h, :], in_=bk[:, (h % 2) * 256:(h % 2) * 256 + 256]),
                             r=[bk], w=[st6], part=(h > 0))
                    for h in range(4):
                        C.op("dve", lambda e, h=h: e.bn_aggr(out=mv[:, h, :], in_=st6[:, h, :]), r=[st6], w=[mv], part=(h > 0))
                    C.op("act", lambda e: e.activation(out=rs[:], in_=mv[:, :, 1], func=AF.Sqrt, bias=cs("eps")[:, 0:1]), r=[mv, CS], w=[rs])
                    C.op("dve", lambda e: e.reciprocal(out=rs[:], in_=rs[:]), r=[rs], w=[rs])
                    yr_ = yr.next()
                    for h in range(4):
                        bk = ob[h // 2]
                        yn_ = yn.next()
                        C.op("dve", lambda e, h=h, bk=bk, yn_=yn_: e.tensor_scalar(
                            out=yn_[:], in0=bk[:, (h % 2) * 256:(h % 2) * 256 + 256], scalar1=mv[:, h, 0:1], scalar2=rs[:, h:h + 1],
                            op0=ALU.subtract, op1=ALU.mult), r=[bk, mv, rs], w=[yn_])
                        C.op("dve", lambda e, h=h, yn_=yn_: e.tensor_tensor(out=yr_[:, h * 256:(h + 1) * 256], in0=yn_[:], in1=sg_[:, h, :],
                                                                            op=ALU.mult), r=[yn_, sg_], w=[yr_], part=(h > 0))
                    tb2 = PT.next()
                    tb2v = tb2[:, :]
                    for cc in range(KC):
                        C.op("pe", lambda e, cc=cc: e.transpose(out=tb2v[:, cc * 128:(cc + 1) * 128], in_=yr_[:, cc * 128:(cc + 1) * 128],
                                                                identity=IDb[:]), r=[yr_, IDb], w=[tb2], inc=(cc == KC - 1), part=(cc > 0))
                    yT_ = yT[ci % 2]
                    C.op("act", lambda e: e.copy(out=yT_[:, :, :].rearrange("p c t -> p (c t)"), in_=tb2v[:, :]), r=[tb2], w=[yT_])
                    C.dma("sp", YRT[:, :, ci * 128:(ci + 1) * 128], yT_[:], ds_yT[ci % 2], r=[yT_], w=[b_YRT[ci]])
                state_update(Sf, k_, vdf, 0)
                sf_new = sfb.next()
                C.op("act", lambda e: e.copy(out=sf_new[:], in_=Sf[:]), r=Sfh, w=[sf_new])
                return sf_new

            sf_cur = sfb.next()
            C.op("dve", lambda e: e.memset(sf_cur[:], 0.0), w=[sf_cur])
            cur = front2(0)
            for ci in range(NCH):
                nxt = front2(ci + 1) if ci + 1 < NCH else None
                sf_cur = back2(ci, cur, sf_cur)
                cur = nxt
            C.barrier()

    def chunks_of(ti):
        off, n = TILES[ti]
        return list(range(off // 128, (off + n) // 128))

    def stage_m1(l, b, tiles):
        with ExitStack() as s:
            dsw = C.dsem(sw=True)
            WRO = load_w(s, w_ret_out[l, :, :], 1024, KC, dsw, "WRO")
            WGR = load_w(s, w_in[l, :, O_GT:O_GT + 1024], 1024, KC, dsw, "WGR")
            yt = [C.sb(s, [128, KC, 512], BF16, "yt") for _ in range(2)]
            ds_yt = [C.dsem() for _ in range(2)]
            mg = [C.sb(s, [128, KC, 512], F32, "mg") for _ in range(2)]
            ds_mg = [C.dsem() for _ in range(2)]
            gs = Ring([C.sb(s, [128, 512], F32, "gs") for _ in range(2)])
            for i, ti in enumerate(tiles):
                off, n = TILES[ti]
                y_ = yt[i % 2]
                m_ = mg[i % 2]
                C.dma("sp", y_[:, :, :n], YRT[:, :, off:off + n], ds_yt[i % 2], r=[b_YRT[c] for c in chunks_of(ti)], w=[y_])
                for cc in range(KC):
                    rb = PB.next()
                    mm_group(rb[:, :n], [(WRO[:, kc, cc * 128:(cc + 1) * 128], y_[:, kc, :n]) for kc in range(KC)], rb, [WRO, y_])
                    gb = PB.next()
                    mm_group(gb[:, :n], [(WGR[:, kc, cc * 128:(cc + 1) * 128], hT[:, kc, off:off + n]) for kc in range(KC)], gb, [WGR, hT])
                    g_ = gs.next()
                    C.op("act", lambda e: e.activation(out=g_[:, :n], in_=gb[:, :n], func=AF.Sigmoid), r=[gb], w=[g_])
                    C.op("dve", lambda e, cc=cc: e.tensor_tensor(out=m_[:, cc, :n], in0=rb[:, :n], in1=g_[:, :n], op=ALU.mult),
                         r=[rb, g_], w=[m_], part=(cc > 0))
                C.dma("sp", MG[:, :, off:off + n], m_[:, :, :n], ds_mg[i % 2], r=[m_], w=[b_MG[ti]])
            C.barrier()

    def stage_cp(l, b, tiles, U, P):
        with ExitStack() as s:
            dsw = C.dsem(sw=True)
            WC = load_w(s, w_in[l, :, O_CC:O_CC + 512], 512, KC, dsw, "WC")
            WX = load_w(s, w_in[l, :, O_CX:O_CX + 512], 512, KC, dsw, "WX")
            WP = load_w(s, w_in[l, :, O_PI:O_PI + 512], 512, KC, dsw, "WP")
            csb = Ring([C.sb(s, [128, 512], F32, "csb") for _ in range(2)])
            C.op("dve", lambda e: e.memset(U[:], 0.0), w=[U])
            C.op("dve", lambda e: e.memset(P[:], 0.0), w=[P])
            for ti in tiles:
                off, n = TILES[ti]
                uc = ucol(off)
                for ch in range(4):
                    cb = PB.next()
                    mm_group(cb[:, :n], [(WC[:, kc, ch * 128:(ch + 1) * 128], hT[:, kc, off:off + n]) for kc in range(KC)], cb, [WC, hT])
                    xb = PB.next()
                    mm_group(xb[:, :n], [(WX[:, kc, ch * 128:(ch + 1) * 128], hT[:, kc, off:off + n]) for kc in range(KC)], xb, [WX, hT])
                    pb = PB.next()
                    mm_group(pb[:, :n], [(WP[:, kc, ch * 128:(ch + 1) * 128], hT[:, kc, off:off + n]) for kc in range(KC)], pb, [WP, hT])
                    c_ = csb.next()
                    C.op("act", lambda e: e.copy(out=c_[:, :n], in_=cb[:, :n]), r=[cb], w=[c_])
                    C.op("dve", lambda e, ch=ch: e.tensor_tensor(out=U[:, ch, uc:uc + n], in0=xb[:, :n], in1=c_[:, :n], op=ALU.mult),
                         r=[xb, c_], w=[U], part=True)
                    C.op("act", lambda e, ch=ch: e.copy(out=P[:, ch, uc:uc + n], in_=pb[:, :n]), r=[pb], w=[P], part=True)
            C.barrier()

    def stage_m2(l, b, tiles, U):
        with ExitStack() as s:
            dsw = C.dsem(sw=True)
            WB = load_w(s, w_in[l, :, O_CB:O_CB + 512], 512, KC, dsw, "WB")
            WGC = load_w(s, w_in[l, :, O_GT + 1024:O_GT + 2048], 1024, KC, dsw, "WGC")
            WCO = load_w(s, w_conv_out[l, :, :], 1024, 4, dsw, "WCO")
            cw = sp("conv_w", l)
            mg = [C.sb(s, [128, KC, 512], F32, "mg2") for _ in range(2)]
            mgc = [[Buf() for _ in range(KC)] for _ in range(2)]
            ds_mg = [C.dsem() for _ in range(2)]
            ds_mgo = [C.dsem() for _ in range(2)]
            cv = Ring([C.sb(s, [128, 512], F32, "cv") for _ in range(2)])
            yc = Ring([C.sb(s, [128, 4, 512], BF16, "yc") for _ in range(2)])
            gs = Ring([C.sb(s, [128, 512], F32, "gs2") for _ in range(2)])
            tt = Ring([C.sb(s, [128, 512], F32, "tt2") for _ in range(2)])
            for i, ti in enumerate(tiles):
                off, n = TILES[ti]
                uc = ucol(off)
                m_ = mg[i % 2]
                mc_ = mgc[i % 2]
                C.dma("sp", m_[:, :, :n], MG[:, :, off:off + n], ds_mg[i % 2], r=[b_MG[ti]], w=mc_)
                yc_ = yc.next()
                for ch in range(4):
                    bb = PB.next()
                    mm_group(bb[:, :n], [(WB[:, kc, ch * 128:(ch + 1) * 128], hT[:, kc, off:off + n]) for kc in range(KC)], bb, [WB, hT])
                    cv_ = cv.next()
                    C.op("dve", lambda e, ch=ch: e.tensor_scalar(out=cv_[:, :n], in0=U[:, ch, uc - 1:uc - 1 + n], scalar1=cw[:, ch:ch + 1],
                                                                 scalar2=None, op0=ALU.mult), r=[U, SP_], w=[cv_])
                    for k in (1, 2):
                        C.op("dve", lambda e, ch=ch, k=k: e.scalar_tensor_tensor(
                            out=cv_[:, :n], in0=U[:, ch, uc - 1 + k:uc - 1 + k + n], scalar=cw[:, k * 4 + ch:k * 4 + ch + 1],
                            in1=cv_[:, :n], op0=ALU.mult, op1=ALU.add), r=[U, SP_, cv_], w=[cv_])
                    C.op("dve", lambda e, ch=ch: e.tensor_tensor(out=yc_[:, ch, :n], in0=bb[:, :n], in1=cv_[:, :n], op=ALU.mult),
                         r=[bb, cv_], w=[yc_], part=(ch > 0))
                for cc in range(KC):
                    cb = PB.next()
                    mm_group(cb[:, :n], [(WCO[:, ch, cc * 128:(cc + 1) * 128], yc_[:, ch, :n]) for ch in range(4)], cb, [WCO, yc_])
                    gb = PB.next()
                    mm_group(gb[:, :n], [(WGC[:, kc, cc * 128:(cc + 1) * 128], hT[:, kc, off:off + n]) for kc in range(KC)], gb, [WGC, hT])
                    g_ = gs.next()
                    C.op("act", lambda e: e.activation(out=g_[:, :n], in_=gb[:, :n], func=AF.Sigmoid), r=[gb], w=[g_])
                    t_ = tt.next()
                    C.op("dve", lambda e: e.tensor_tensor(out=t_[:, :n], in0=cb[:, :n], in1=g_[:, :n], op=ALU.mult), r=[cb, g_], w=[t_])
                    C.op("dve", lambda e, cc=cc: e.tensor_tensor(out=m_[:, cc, :n], in0=m_[:, cc, :n], in1=t_[:, :n], op=ALU.add),
                         r=[mc_[cc], t_], w=[mc_[cc]])
                C.dma("sp", MG[:, :, off:off + n], m_[:, :, :n], ds_mgo[i % 2], r=mc_, w=[b_MG[ti]])
            C.barrier()

    def stage_m3(l, b, tiles, P):
        with ExitStack() as s:
            dsw = C.dsem(sw=True)
            WGP = load_w(s, w_in[l, :, O_GT + 2048:O_GT + 3072], 1024, KC, dsw, "WGP")
            WPO = load_w(s, w_pool_out[l, :, :], 1024, 4, dsw, "WPO")
            WO = load_w(s, w_o[l, :, :], 1024, KC, dsw, "WO")
            PW = C.sb(s, [128, 4, 128], BF16, "PW")
            dspw = C.dsem(sw=True)
            for gI in range(4):
                C.dma("pool", PW[:, gI, :], pool_w[l, gI, :, :], dspw, r=[b_in], w=[PW], part=(gI > 0))
            et = C.sb(s, [128, 8], F32, "et")
            ive = cs("ive")
            psc = sp("pool_scale", l)
            mg = C.sb(s, [128, KC, 512], F32, "mg3")
            ds_mg = C.dsem()
            xt = C.sb(s, [128, KC, 512], F32, "xt3")
            xtc = [Buf() for _ in range(KC)]
            ds_xt = C.dsem()
            ds_xo = C.dsem()
            wa = C.sb(s, [128, 528], F32, "wa3")
            wb_ = C.sb(s, [128, 528], F32, "wb3")
            pl = Ring([C.sb(s, [128, 512], BF16, "pl") for _ in range(2)])
            yp = Ring([C.sb(s, [128, 4, 512], BF16, "yp") for _ in range(2)])
            gs = Ring([C.sb(s, [128, 512], F32, "gs3") for _ in range(2)])
            tt = Ring([C.sb(s, [128, 512], F32, "tt3") for _ in range(2)])
            mgb = C.sb(s, [128, KC, 512], BF16, "mgb")
            src = xin if l == 0 else XT
            for ti in tiles:
                off, n = TILES[ti]
                uc = ucol(off)
                r = 2 if ti == 0 else b
                C.dma("sp", mg[:, :, :n], MG[:, :, off:off + n], ds_mg, r=[b_MG[ti]], w=[mg])
                C.dma("sp", xt[:, :, :n], src[b, :, :, off:off + n], ds_xt, r=[b_in if l == 0 else b_XT[b][ti]], w=xtc)
                yp_ = yp.next()
                for gI, W in enumerate((2, 4, 8, 16)):
                    hw = W // 2
                    lo = uc - hw
                    ln = n + W - 2
                    C.op("dve", lambda e, gI=gI, lo=lo, ln=ln: e.tensor_tensor(out=wa[:, :ln], in0=P[:, gI, lo:lo + ln], in1=P[:, gI, lo + 1:lo + 1 + ln],
                                                                               op=ALU.add), r=[P], w=[wa])
                    cur, oth = wa, wb_
                    step = 2
                    while step < W:
                        ln2 = ln - step
                        C.op("dve", lambda e, cur=cur, oth=oth, ln2=ln2, step=step: e.tensor_tensor(
                            out=oth[:, :ln2], in0=cur[:, :ln2], in1=cur[:, step:step + ln2], op=ALU.add), r=[cur], w=[oth])
                        cur, oth = oth, cur
                        ln = ln2
                        step *= 2
                    assert ln == n
                    pl_ = pl.next()
                    C.op("dve", lambda e, cur=cur, gI=gI, W=W: e.scalar_tensor_tensor(
                        out=pl_[:, :n], in0=cur[:, :n], scalar=1.0 / W, in1=P[:, gI, uc:uc + n], op0=ALU.mult, op1=ALU.subtract),
                        r=[cur, P], w=[pl_])
                    edges = {0: [(0, 0), (n - 8, 1)], 1: [(0, 2)], 4: [(n - 8, 3)]}.get(ti, [])
                    for (e0, k) in edges:
                        io = (gI * 4 + k) * 8
                        C.op("dve", lambda e, cur=cur, e0=e0, io=io: e.tensor_tensor(out=et[:, 0:8], in0=cur[:, e0:e0 + 8], in1=ive[:, io:io + 8], op=ALU.mult),
                             r=[cur, CS], w=[et])
                        C.op("dve", lambda e, e0=e0, gI=gI: e.tensor_tensor(out=pl_[:, e0:e0 + 8], in0=et[:, 0:8], in1=P[:, gI, uc + e0:uc + e0 + 8], op=ALU.subtract),
                             r=[et, P], w=[pl_], part=True)
                    mb = PB.next()
                    mm_group(mb[:, :n], [(PW[:, gI, :], pl_[:, :n])], mb, [PW, pl_])
                    C.op("act", lambda e, gI=gI: e.activation(out=yp_[:, gI, :n], in_=mb[:, :n], func=AF.Identity, scale=psc[:, gI:gI + 1]),
                         r=[mb, SP_], w=[yp_], part=(gI > 0))
                for cc in range(KC):
                    pb = PB.next()
                    mm_group(pb[:, :n], [(WPO[:, ch, cc * 128:(cc + 1) * 128], yp_[:, ch, :n]) for ch in range(4)], pb, [WPO, yp_])
                    gb = PB.next()
                    mm_group(gb[:, :n], [(WGP[:, kc, cc * 128:(cc + 1) * 128], hT[:, kc, off:off + n]) for kc in range(KC)], gb, [WGP, hT])
                    g_ = gs.next()
                    C.op("act", lambda e: e.activation(out=g_[:, :n], in_=gb[:, :n], func=AF.Sigmoid), r=[gb], w=[g_])
                    t_ = tt.next()
                    C.op("dve", lambda e: e.tensor_tensor(out=t_[:, :n], in0=pb[:, :n], in1=g_[:, :n], op=ALU.mult), r=[pb, g_], w=[t_])
                    C.op("dve", lambda e, cc=cc: e.tensor_tensor(out=mgb[:, cc, :n], in0=mg[:, cc, :n], in1=t_[:, :n], op=ALU.add),
                         r=[mg, t_], w=[mgb], part=(cc > 0))
                for cc in range(KC):
                    yb = PB.next()
                    mm_group(yb[:, :n], [(WO[:, kc, cc * 128:(cc + 1) * 128], mgb[:, kc, :n]) for kc in range(KC)], yb, [WO, mgb])
                    C.op("dve", lambda e, cc=cc: e.scalar_tensor_tensor(
                        out=xt[:, cc, :n], in0=yb[:, :n], scalar=MOD[:, 16 + cc, r:r + 1], in1=xt[:, cc, :n], op0=ALU.mult, op1=ALU.add),
                        r=[yb, MOD, xtc[cc]], w=[xtc[cc]])
                C.dma("sp", XT[b, :, :, off:off + n], xt[:, :, :n], ds_xo, r=xtc, w=[b_XT[b][ti]])
            C.barrier()

    def stage_moe(l, b, tiles):
        last = (l == last_l)
        with ExitStack() as s:
            XS = C.sb(s, [128, KC, NT], F32, "XS")
            xsb = [[Buf() for _ in range(KC)] for _ in TILES]
            ds_xs = [C.dsem() for _ in TILES]
            COMBT = C.sb(s, [16, NT], F32, "COMBT")
            for ti in tiles:
                off, n = TILES[ti]
                C.dma("sp", XS[:, :, off:off + n], XT[b, :, :, off:off + n], ds_xs[ti], r=[b_XT[b][ti]], w=xsb[ti])
            wrl = C.sb(s, [128, KC, 20], F32, "wrl")
            C.dma("sp", wrl[:, :, :].rearrange("p k n -> p (k n)"), wr[:, l * KC * 20:(l + 1) * KC * 20], C.dsem(), r=[b_in], w=[wrl])
            brow = sp("b_r", l)
            with ExitStack() as s2:
                hf = C.sb(s2, [128, KC, 512], F32, "hf")
                sq = C.sb(s2, [128, KC, 512], BF16, "sq2")
                rstd = C.sb(s2, [128, 512], F32, "rstd2")
                tmp = Ring([C.sb(s2, [128, 512], F32, "ntmp2") for _ in range(2)])
                R = {k: C.sb(s2, shp, F32, "r_" + k) for k, shp in dict(
                    lg=[128, 4, 20], mx=[128, 4], eg=[128, 4, 4], se=[128, 4], gtop=[128, 4], ohg=[128, 4, 4], prod=[128, 4, 4, 4],
                    ing=[128, 4, 4], m1=[128, 4], oh1=[128, 4, 4], msk=[128, 4, 4], m2=[128, 4], oh2=[128, 4, 4], d21=[128, 4],
                    e2=[128, 4], den=[128, 4], w1=[128, 4], w2=[128, 4], loc=[128, 4, 4], tmp2=[128, 4, 4], comb=[128, 4, 16]).items()}
                V = lambda fn, r, w: C.op("dve", fn, r=r, w=w)
                A = lambda fn, r, w: C.op("act", fn, r=r, w=w)
                for ti in tiles:
                    off, n = TILES[ti]
                    norm_tile(s2, l, b, ti, XS[:, :, off:off + n], xsb[ti], GS2, 24, (sq, rstd, tmp), hf=hf)
                    c = n // 128
                    lb = PB.next()
                    for cj in range(c):
                        for kc in range(KC):
                            C.op("pe", lambda e, cj=cj, kc=kc: e.matmul(lb[:, cj * 20:(cj + 1) * 20], lhsT=hf[:, kc, cj * 128:(cj + 1) * 128],
                                                                      rhs=wrl[:, kc, :], start=(kc == 0), stop=(kc == KC - 1)),
                                 r=[hf, wrl], w=[lb], inc=(kc == KC - 1), part=(cj > 0 or kc > 0))
                    lg, mx, eg, se, gtop, ohg, prod, ing = (R[k] for k in ("lg", "mx", "eg", "se", "gtop", "ohg", "prod", "ing"))
                    m1, oh1, msk, m2, oh2, d21, e2, den, w1_, w2_, loc, tmp2, comb = (R[k] for k in (
                        "m1", "oh1", "msk", "m2", "oh2", "d21", "e2", "den", "w1", "w2", "loc", "tmp2", "comb"))
                    bc3 = lambda t_: t_[:, :c].unsqueeze(2).to_broadcast([128, c, 4])
                    V(lambda e: e.tensor_tensor(out=lg[:, :c, :], in0=lb[:, 0:c * 20].rearrange("p (c k) -> p c k", c=c),
                                                in1=brow.unsqueeze(1).to_broadcast([128, c, 20]), op=ALU.add), [lb, SP_], [lg])
                    lgg = lg[:, :c, 0:4]
                    V(lambda e: e.reduce_max(out=mx[:, :c], in_=lgg, axis=AX.X), [lg], [mx])
                    V(lambda e: e.tensor_tensor(out=eg[:, :c, :], in0=lgg, in1=bc3(mx), op=ALU.subtract), [lg, mx], [eg])
                    A(lambda e: e.activation(out=eg[:, :c, :], in_=eg[:, :c, :], func=AF.Exp), [eg], [eg])
                    V(lambda e: e.reduce_sum(out=se[:, :c], in_=eg[:, :c, :], axis=AX.X), [eg], [se])
                    V(lambda e: e.reciprocal(out=gtop[:, :c], in_=se[:, :c]), [se], [gtop])
                    V(lambda e: e.tensor_tensor(out=ohg[:, :c, :], in0=lgg, in1=bc3(mx), op=ALU.is_ge), [lg, mx], [ohg])
                    V(lambda e: e.tensor_tensor(out=prod[:, :c, :, :], in0=lg[:, :c, 4:20].rearrange("p c (g k) -> p c g k", g=4),
                                                in1=ohg[:, :c, :].unsqueeze(3).to_broadcast([128, c, 4, 4]), op=ALU.mult), [lg, ohg], [prod])
                    V(lambda e: e.tensor_tensor(out=ing[:, :c, :], in0=prod[:, :c, 0, :], in1=prod[:, :c, 1, :], op=ALU.add), [prod], [ing])
                    for gI in (2, 3):
                        V(lambda e, gI=gI: e.tensor_tensor(out=ing[:, :c, :], in0=ing[:, :c, :], in1=prod[:, :c, gI, :], op=ALU.add), [prod, ing], [ing])
                    V(lambda e: e.reduce_max(out=m1[:, :c], in_=ing[:, :c, :], axis=AX.X), [ing], [m1])
                    V(lambda e: e.tensor_tensor(out=oh1[:, :c, :], in0=ing[:, :c, :], in1=bc3(m1), op=ALU.is_ge), [ing, m1], [oh1])
                    V(lambda e: e.scalar_tensor_tensor(out=msk[:, :c, :], in0=oh1[:, :c, :], scalar=-1e30, in1=ing[:, :c, :], op0=ALU.mult, op1=ALU.add),
                      [oh1, ing], [msk])
                    V(lambda e: e.reduce_max(out=m2[:, :c], in_=msk[:, :c, :], axis=AX.X), [msk], [m2])
                    V(lambda e: e.tensor_tensor(out=oh2[:, :c, :], in0=msk[:, :c, :], in1=bc3(m2), op=ALU.is_ge), [msk, m2], [oh2])
                    V(lambda e: e.tensor_tensor(out=d21[:, :c], in0=m2[:, :c], in1=m1[:, :c], op=ALU.subtract), [m1, m2], [d21])
                    A(lambda e: e.activation(out=e2[:, :c], in_=d21[:, :c], func=AF.Exp), [d21], [e2])
                    V(lambda e: e.tensor_scalar(out=den[:, :c], in0=e2[:, :c], scalar1=1.0, scalar2=None, op0=ALU.add), [e2], [den])
                    V(lambda e: e.reciprocal(out=w1_[:, :c], in_=den[:, :c]), [den], [w1_])
                    V(lambda e: e.tensor_tensor(out=w1_[:, :c], in0=w1_[:, :c], in1=gtop[:, :c], op=ALU.mult), [w1_, gtop], [w1_])
                    V(lambda e: e.tensor_tensor(out=w2_[:, :c], in0=w1_[:, :c], in1=e2[:, :c], op=ALU.mult), [w1_, e2], [w2_])
                    V(lambda e: e.tensor_tensor(out=loc[:, :c, :], in0=oh1[:, :c, :], in1=bc3(w1_), op=ALU.mult), [oh1, w1_], [loc])
                    V(lambda e: e.tensor_tensor(out=tmp2[:, :c, :], in0=oh2[:, :c, :], in1=bc3(w2_), op=ALU.mult), [oh2, w2_], [tmp2])
                    V(lambda e: e.tensor_tensor(out=loc[:, :c, :], in0=loc[:, :c, :], in1=tmp2[:, :c, :], op=ALU.add), [loc, tmp2], [loc])
                    for gI in range(4):
                        C.op("dve", lambda e, gI=gI: e.tensor_tensor(out=comb[:, :c, 4 * gI:4 * gI + 4], in0=loc[:, :c, :],
                                                                     in1=ohg[:, :c, gI:gI + 1].to_broadcast([128, c, 4]), op=ALU.mult),
                             r=[loc, ohg], w=[comb], part=(gI > 0))
                    tb = PB.next()
                    for cj in range(c):
                        C.op("pe", lambda e, cj=cj: e.transpose(out=tb[0:16, cj * 128:(cj + 1) * 128], in_=comb[:, cj, :], identity=IDf),
                             r=[comb, CS], w=[tb], inc=(cj == c - 1), part=(cj > 0))
                    C.op("act", lambda e: e.copy(out=COMBT[:, off:off + n], in_=tb[0:16, 0:n]), r=[tb], w=[COMBT], part=True)
                C.barrier()
            with ExitStack() as s3:
                w1e = [C.sb(s3, [128, KC, 512], BF16, "w1e") for _ in range(2)]
                w3e = [C.sb(s3, [128, KC, 512], BF16, "w3e") for _ in range(2)]
                w2e = [C.sb(s3, [128, 4, 1024], BF16, "w2e") for _ in range(2)]
                ds_e = [[C.dsem(sw=True) for _ in range(3)] for _ in range(2)]
                sel = Ring([C.sb(s3, [16, 128], F32, "sel") for _ in range(2)])
                cbs = Ring([C.sb(s3, [128, 512], F32, "cbs") for _ in range(2)])
                s1 = Ring([C.sb(s3, [128, 512], F32, "s1") for _ in range(2)])
                s2_ = Ring([C.sb(s3, [128, 512], F32, "s2") for _ in range(2)])
                act = Ring([C.sb(s3, [128, 4, 512], BF16, "act") for _ in range(2)])

                def load_e(e_):
                    k = e_ % 2
                    v1 = w1[l, e_].rearrange("(kc p) n -> p kc n", p=128)
                    v3 = w3[l, e_].rearrange("(kc p) n -> p kc n", p=128)
                    v2 = w2[l, e_].rearrange("(kc p) n -> p kc n", p=128)
                    for kc in range(KC):
                        C.dma("pool", w1e[k][:, kc, :], v1[:, kc, :], ds_e[k][0], r=[b_in], w=[w1e[k]], part=(kc > 0))
                        C.dma("pool", w3e[k][:, kc, :], v3[:, kc, :], ds_e[k][1], r=[b_in], w=[w3e[k]], part=(kc > 0))
                    for fc in range(4):
                        C.dma("pool", w2e[k][:, fc, :], v2[:, fc, :], ds_e[k][2], r=[b_in], w=[w2e[k]], part=(fc > 0))

                load_e(0)
                for e_ in range(16):
                    if e_ + 1 < 16:
                        load_e(e_ + 1)
                    k = e_ % 2
                    se_ = sel.next()
                    C.op("dve", lambda e, e_=e_: e.tensor_copy(out=se_[:], in_=IDf[0:16, e_:e_ + 1].to_broadcast([16, 128])), r=[CS], w=[se_])
                    for ti in tiles:
                        off, n = TILES[ti]
                        r = 2 if ti == 0 else b
                        cb = PB.next()
                        mm_group(cb[:, :n], [(se_[:], COMBT[:, off:off + n])], cb, [se_, COMBT])
                        cb_ = cbs.next()
                        C.op("act", lambda e: e.copy(out=cb_[:, :n], in_=cb[:, :n]), r=[cb], w=[cb_])
                        a_ = act.next()
                        for fc in range(4):
                            z1 = PB.next()
                            mm_group(z1[:, :n], [(w1e[k][:, kc, fc * 128:(fc + 1) * 128], hT[:, kc, off:off + n]) for kc in range(KC)], z1, [w1e[k], hT])
                            z3 = PB.next()
                            mm_group(z3[:, :n], [(w3e[k][:, kc, fc * 128:(fc + 1) * 128], hT[:, kc, off:off + n]) for kc in range(KC)], z3, [w3e[k], hT])
                            a1 = s1.next()
                            C.op("act", lambda e: e.activation(out=a1[:, :n], in_=z1[:, :n], func=AF.Silu), r=[z1], w=[a1])
                            a2 = s2_.next()
                            C.op("dve", lambda e: e.tensor_tensor(out=a2[:, :n], in0=a1[:, :n], in1=cb_[:, :n], op=ALU.mult), r=[a1, cb_], w=[a2])
                            C.op("dve", lambda e, fc=fc: e.tensor_tensor(out=a_[:, fc, :n], in0=z3[:, :n], in1=a2[:, :n], op=ALU.mult),
                                 r=[z3, a2], w=[a_], part=(fc > 0))
                        for cc in range(KC):
                            yb = PB.next()
                            mm_group(yb[:, :n], [(w2e[k][:, fc, cc * 128:(cc + 1) * 128], a_[:, fc, :n]) for fc in range(4)], yb, [w2e[k], a_])
                            C.op("dve", lambda e, cc=cc: e.scalar_tensor_tensor(
                                out=XS[:, cc, off:off + n], in0=yb[:, :n], scalar=MOD[:, 40 + cc, r:r + 1], in1=XS[:, cc, off:off + n],
                                op0=ALU.mult, op1=ALU.add), r=[yb, MOD, xsb[ti][cc]], w=[xsb[ti][cc]])
                C.barrier()
            if not last:
                for ti in tiles:
                    off, n = TILES[ti]
                    C.dma("sp", XT[b, :, :, off:off + n], XS[:, :, off:off + n], ds_xs[ti], r=xsb[ti], w=[b_XT[b][ti]])
            else:
                with ExitStack() as s4:
                    sq = C.sb(s4, [128, KC, 512], BF16, "sq4")
                    rstd = C.sb(s4, [128, 512], F32, "rstd4")
                    tmp = Ring([C.sb(s4, [128, 512], F32, "ntmp4") for _ in range(2)])
                    ot = [C.sb(s4, [128, KC, 512], F32, "ot") for _ in range(2)]
                    ds_o = [C.dsem() for _ in range(2)]
                    fn_ = sp("final_norm")
                    FN = C.sb(s4, [128, KC, 3], F32, "FN")
                    for kc in range(KC):
                        C.op("dve", lambda e, kc=kc: e.tensor_copy(out=FN[:, kc, :], in_=fn_[:, kc:kc + 1].to_broadcast([128, 3])), r=[SP_], w=[FN], part=True)
                    for i, ti in enumerate(t for t in tiles if t > 0):
                        off, n = TILES[ti]
                        o_ = ot[i % 2]

                        def out_fn(kc, tb, n, o_=o_):
                            C.op("act", lambda e: e.copy(out=o_[:, kc, :n], in_=tb[:, :n]), r=[tb], w=[o_], part=True)
                        norm_tile(s4, l, b, ti, XS[:, :, off:off + n], xsb[ti], FN, 0, (sq, rstd, tmp), out_fn=out_fn)
                        C.dma("sp", outT[b, :, :, off - NCTX:off - NCTX + n], o_[:, :, :n], ds_o[i % 2], r=[o_], w=[b_out], part=True)
            C.barrier()

    stages = 0

    def done():
        nonlocal stages
        C.reset_dsems()
        stages += 1
        return stop_after is not None and stages >= stop_after

    def program():
        for l in range(n_layers):
            last = (l == last_l)
            layer_setup(l)
            C.reset_dsems()
            tiles_all = list(range(5))
            tiles_out = [1, 2, 3, 4] if last else tiles_all
            for b in range(NB):
                stage_norm1(l, b, tiles_all)
                if done(): return
                stage_ret(l, b)
                if done(): return
                stage_m1(l, b, tiles_out)
                if done(): return
                with ExitStack() as su:
                    U = C.sb(su, [128, 4, LT], BF16, "U")
                    P = C.sb(su, [128, 4, LT], BF16, "P")
                    stage_cp(l, b, tiles_out, U, P)
                    C.reset_dsems()
                    stage_m2(l, b, tiles_out, U)
                    if done(): return
                    stage_m3(l, b, tiles_out, P)
                if done(): return
                if last:
                    pass
                stage_moe(l, b, tiles_out)
                if done(): return

    program()
    C.barrier()
    if dbg:
        dh = dram("dbg_hT", [128, KC, NT], BF16, "ExternalOutput")
        C.dma("sp", dh[:, :, :], hT[:], ds_misc, r=[hT], w=[b_out])
    C.barrier()
    es.close()
    return nc


def _const_layout():
    off = {}
    o = 0
    for name, n in (("ident", 128), ("ones", 128), ("relp", 128), ("reln", 128), ("mf", 128), ("mb", 128),
                    ("iota1", 128), ("iotac", 128), ("jcol", 2), ("eps", 1), ("ive", 128)):
        off[name] = (o, n)
        o += n
    return off, o


CONST_OFF, CONST_W = _const_layout()


def _small_layout():
    off = {}
    o = 0
    for name, n in (("b_ada", DEPTH * 48), ("norm1", DEPTH * 8), ("norm2", DEPTH * 8), ("ret_decay", DEPTH * 8),
                    ("conv_w", DEPTH * 12), ("pool_scale", DEPTH * 4), ("b_r", DEPTH * 20), ("final_norm", 8)):
        off[name] = (o, n)
        o += n
    return off, o


SMALL_OFF, SMALL_W = _small_layout()


def _consts():
    c = np.zeros((128, CONST_W), np.float32)
    j = np.arange(128, dtype=np.float32)
    rel = j[None, :] - j[:, None]
    put = lambda name, v: c.__setitem__((slice(None), slice(CONST_OFF[name][0], CONST_OFF[name][0] + CONST_OFF[name][1])), v)
    put("ident", np.eye(128, dtype=np.float32))
    put("ones", np.ones((128, 128), np.float32))
    put("relp", np.maximum(rel, 0))
    put("reln", np.maximum(-rel, 0))
    put("mf", (rel >= 0).astype(np.float32))
    put("mb", (rel <= 0).astype(np.float32))
    put("iota1", np.broadcast_to(j[None, :] + 1.0, (128, 128)))
    put("iotac", np.broadcast_to(128.0 - j[None, :], (128, 128)))
    put("jcol", np.stack([j, 127.0 - j], axis=1))
    put("eps", np.full((128, 1), EPS, np.float32))
    pos = np.arange(SEQ)
    row = (pos // 64).astype(np.float32)
    col = (pos % 64).astype(np.float32)
    inv = (10000.0 ** (-np.arange(32, dtype=np.float32) / 32)).astype(np.float32)
    ang = np.concatenate([row[:, None] * inv[None, :], col[:, None] * inv[None, :]], axis=-1).astype(np.float32)
    cos, sin = np.cos(ang), np.sin(ang)
    sc = np.float32(128.0 ** -0.5)
    tab = np.stack([cos, sin, cos * sc, sin * sc], axis=0).reshape(4, 16, 128, 64).transpose(2, 0, 1, 3)
    ropet = np.ascontiguousarray(tab.reshape(128, 4 * 16 * 64)).astype(np.float32)
    iv = np.zeros((4, NT), np.float32)
    for gi, win in enumerate((2, 4, 8, 16)):
        for (o, L) in ((0, NCTX), (NCTX, SEQ)):
            t = np.arange(L)
            lo = np.clip(t - win // 2, 0, L)
            hi = np.clip(t + win - win // 2, 0, L)
            iv[gi, o:o + L] = 1.0 / (hi - lo)
    ive = np.zeros((4, 4, 8), np.float32)
    for gi in range(4):
        ive[gi, 0] = iv[gi, 0:8]
        ive[gi, 1] = iv[gi, NCTX - 8:NCTX]
        ive[gi, 2] = iv[gi, NCTX:NCTX + 8]
        ive[gi, 3] = iv[gi, NT - 8:NT]
    put("ive", np.broadcast_to(ive.reshape(1, 128), (128, 128)))
    return c, ropet


def _fm(v):
    return np.ascontiguousarray(v.reshape(-1, 128).T)


def _small(inp):
    s = np.zeros((128, SMALL_W), np.float32)

    def put(name, v):
        o, n = SMALL_OFF[name]
        assert v.shape == (128, n), (name, v.shape, n)
        s[:, o:o + n] = v
    put("b_ada", np.concatenate([_fm(inp["b_ada"][l]) for l in range(DEPTH)], axis=1))
    put("norm1", np.concatenate([_fm(inp["norm1"][l]) for l in range(DEPTH)], axis=1))
    put("norm2", np.concatenate([_fm(inp["norm2"][l]) for l in range(DEPTH)], axis=1))
    put("ret_decay", np.broadcast_to(inp["ret_decay"].reshape(1, DEPTH * 8), (128, DEPTH * 8)))
    cw = inp["conv_w"].reshape(DEPTH, 3, 4, 128).transpose(3, 0, 1, 2).reshape(128, DEPTH * 12)
    put("conv_w", cw)
    put("pool_scale", inp["pool_scale"].reshape(DEPTH, 4, 128).transpose(2, 0, 1).reshape(128, DEPTH * 4))
    br = np.concatenate([inp["b_rg"], inp["b_re"]], axis=1)
    put("b_r", np.broadcast_to(br.reshape(1, DEPTH * 20), (128, DEPTH * 20)))
    put("final_norm", _fm(inp["final_norm"]))
    return s


def _prep(inputs, NB, core):
    x, ctx = inputs["x"], inputs["ctx"]
    bs = slice(core * NB, (core + 1) * NB)
    seq = np.concatenate([ctx[bs], x[bs]], axis=1)
    xin = np.ascontiguousarray(seq.reshape(NB, NT, KC, 128).transpose(0, 3, 2, 1))
    crow = np.concatenate([inputs["c"][bs], inputs["c_ctx"][None, :]] if NB == 2 else
                          [inputs["c"][bs], inputs["c"][bs], inputs["c_ctx"][None, :]], axis=0)
    cT = np.ascontiguousarray(crow.reshape(3, KC, 128).transpose(2, 1, 0))
    return xin, cT


def _shared(inputs):
    c, ropet = _consts()
    wrc = np.concatenate([inputs["w_rg"], inputs["w_re"]], axis=2)
    wr = np.ascontiguousarray(wrc.reshape(DEPTH, KC, 128, 20).transpose(2, 0, 1, 3).reshape(128, DEPTH * KC * 20))
    d = {"consts": c, "ropet": ropet, "wr": wr, "smallp": _small(inputs)}
    for k in ("w_ada", "w_in", "w_ret_out", "w_conv_out", "w_pool_out", "w_o", "pool_w", "w1", "w3", "w2"):
        d[k] = np.ascontiguousarray(inputs[k], dtype=np.float32)
    return d


def kernel(**inputs):
    inputs = {k: np.asarray(v, dtype=np.float32) for k, v in inputs.items()}
    n = 8
    NB = 2
    nc = build(NB=NB)
    shared = _shared(inputs)
    in_maps = []
    for core in range(n):
        xin, cT = _prep(inputs, NB, core)
        m = dict(shared)
        m["xin"] = xin
        m["cT"] = cT
        in_maps.append(m)
    res = run_bass_kernel_spmd(nc, in_maps, core_ids=list(range(n)))
    outs = []
    for r in res.results:
        o = np.asarray(r["outT"])
        outs.append(o.transpose(0, 3, 2, 1).reshape(NB, SEQ, D))
    return np.ascontiguousarray(np.concatenate(outs, axis=0)).astype(np.float32)
```
